# Optimizing a Trainium2 kernel written in Bass

```python
import jax, jax.numpy as jnp
from jax import lax
import numpy as np

D_MODEL = 1024
BATCH = 16
SEQ = 4096
DEPTH = 2

GRID_W = 64
CTX_LEN = 256
N_MOD = 6
FNET_GROUPS = 8
FNET_GROUP_DIM = 64
FNET_WIDTH = FNET_GROUPS * FNET_GROUP_DIM
RET_HEADS = 8
RET_DK = 64
RET_DV = 64
RET_QK_WIDTH = RET_HEADS * RET_DK
RET_V_WIDTH = RET_HEADS * RET_DV
RET_CHUNK = 128
RET_DECAY_BASE = 5.0
IN_WIDTH = FNET_WIDTH + 2 * RET_QK_WIDTH + 2 * RET_V_WIDTH
IN_SPLITS = (FNET_WIDTH, FNET_WIDTH + RET_QK_WIDTH, FNET_WIDTH + 2 * RET_QK_WIDTH,
             FNET_WIDTH + 2 * RET_QK_WIDTH + RET_V_WIDTH)
MIX_WIDTH = FNET_WIDTH + RET_V_WIDTH
ROPE_BASE = 10000.0
N_EXPERTS = 32
TOP_K = 4
D_EXPERT = D_MODEL
SWIGLU_LIMIT = 7.0
SWIGLU_ALPHA = 1.702
MOE_BLOCK = 128
NORM_EPS = 1e-6

kernel_name = "hybrid_fnet_retention_moe_dit"


def rmsnorm(x, w):
    x32 = x.astype(jnp.float32)
    y = x32 * lax.rsqrt(jnp.mean(x32 * x32, axis=-1, keepdims=True) + NORM_EPS)
    return (y * w.astype(jnp.float32)).astype(x.dtype)


def modulate(h, shift, scale):
    return h * (1 + scale) + shift


def flip(t):
    return jnp.flip(t, axis=1)


def heads(t, d):
    return t.reshape(t.shape[0], t.shape[1], RET_HEADS, d)


def axial_rope_angles(rows):
    row = jnp.broadcast_to(jnp.arange(rows)[:, None], (rows, GRID_W)).reshape(-1).astype(jnp.float32)
    col = jnp.broadcast_to(jnp.arange(GRID_W)[None, :], (rows, GRID_W)).reshape(-1).astype(jnp.float32)
    n_freq = RET_DK // 4
    freqs = ROPE_BASE ** (-jnp.arange(n_freq, dtype=jnp.float32) / n_freq)
    return jnp.concatenate([row[:, None] * freqs, col[:, None] * freqs], axis=-1)


def apply_rope(t, ang):
    cos = jnp.cos(ang)[None, :, None, :]
    sin = jnp.sin(ang)[None, :, None, :]
    t1, t2 = jnp.split(t, 2, axis=-1)
    return jnp.concatenate([t1 * cos - t2 * sin, t1 * sin + t2 * cos], axis=-1).astype(t.dtype)


def fourier_mix(z):
    b, t, _ = z.shape
    zg = z.reshape(b, t, FNET_GROUPS, FNET_GROUP_DIM).astype(jnp.float32)
    y = jnp.fft.fft2(zg, axes=(1, 3), norm="ortho").real
    return y.reshape(b, t, FNET_WIDTH)


def retention_states(k, v, log_g, s0):
    b, t, h, dk = k.shape
    n = t // RET_CHUNK
    kc = k.reshape(b, n, RET_CHUNK, h, dk)
    vc = v.reshape(b, n, RET_CHUNK, h, v.shape[-1])
    j = jnp.arange(RET_CHUNK, dtype=jnp.float32)
    w_kv = jnp.exp((RET_CHUNK - 1 - j)[:, None] * log_g[None, :])
    chunk_kv = jnp.einsum('bnjhd,jh,bnjhe->nbhde', kc, w_kv, vc)
    g_chunk = jnp.exp(RET_CHUNK * log_g)[None, :, None, None]

    def step(s, kv):
        return g_chunk * s + kv, s

    s_final, s_before = lax.scan(step, s0, chunk_kv)
    return s_before, s_final


def retention_outputs(q, k, v, log_g, s_before):
    b, t, h, dk = q.shape
    n = t // RET_CHUNK
    qc = q.reshape(b, n, RET_CHUNK, h, dk)
    kc = k.reshape(b, n, RET_CHUNK, h, dk)
    vc = v.reshape(b, n, RET_CHUNK, h, v.shape[-1])
    pos = jnp.arange(RET_CHUNK)
    diff = pos[:, None] - pos[None, :]
    decay = jnp.where((diff >= 0)[None],
                      jnp.exp(jnp.maximum(diff, 0).astype(jnp.float32)[None] * log_g[:, None, None]),
                      0.0)
    scores = jnp.einsum('bnihd,bnjhd->bnhij', qc, kc) * decay
    o_intra = jnp.einsum('bnhij,bnjhe->bnihe', scores, vc)
    w_q = jnp.exp((pos.astype(jnp.float32) + 1.0)[:, None] * log_g[None, :])
    o_cross = jnp.einsum('bnihd,ih,nbhde->bnihe', qc, w_q, s_before)
    return (o_intra + o_cross).reshape(b, t, h, -1)


def mixer_merge(f_in, o_ret, g, ret_norm_w, w_out, dtype):
    b, t = o_ret.shape[:2]
    o = o_ret.astype(jnp.float32)
    mu = jnp.mean(o, axis=-1, keepdims=True)
    var = jnp.mean(jnp.square(o - mu), axis=-1, keepdims=True)
    on = ((o - mu) * lax.rsqrt(var + NORM_EPS)).reshape(b, t, RET_V_WIDTH)
    y = jax.nn.silu(g.astype(jnp.float32)) * on * ret_norm_w.astype(jnp.float32)
    cat = jnp.concatenate([fourier_mix(f_in), y], axis=-1).astype(dtype)
    return cat @ w_out


def moe_ffn(h, router_w, router_b, w1, b1, w2, b2):
    n_tok, d = h.shape
    n_assign = n_tok * TOP_K
    logits = (h @ router_w + router_b).astype(jnp.float32)
    top_vals, top_idx = lax.top_k(logits, TOP_K)
    gates = jax.nn.softmax(top_vals, axis=-1)
    flat_e = top_idx.reshape(-1)
    flat_tok = jnp.arange(n_assign) // TOP_K
    order = jnp.argsort(flat_e)
    sorted_e = flat_e[order]
    sorted_tok = flat_tok[order]
    sorted_gate = gates.reshape(-1)[order]
    group_sizes = jnp.bincount(flat_e, length=N_EXPERTS)
    padded_sizes = (group_sizes + MOE_BLOCK - 1) // MOE_BLOCK * MOE_BLOCK
    group_start = jnp.cumsum(group_sizes) - group_sizes
    padded_end = jnp.cumsum(padded_sizes)
    padded_start = padded_end - padded_sizes
    dest = padded_start[sorted_e] + (jnp.arange(n_assign) - group_start[sorted_e])
    n_blocks = -(-n_assign // MOE_BLOCK) + N_EXPERTS
    buf = jnp.zeros((n_blocks * MOE_BLOCK, d), h.dtype).at[dest].set(h[sorted_tok])
    block_e = jnp.minimum(jnp.searchsorted(padded_end, jnp.arange(n_blocks) * MOE_BLOCK, side='right'),
                          N_EXPERTS - 1)

    def expert_block(args):
        xb, e = args
        hb = xb @ w1[e] + b1[e]
        x_glu = jnp.minimum(hb[:, ::2], SWIGLU_LIMIT)
        x_lin = jnp.clip(hb[:, 1::2], -SWIGLU_LIMIT, SWIGLU_LIMIT)
        act = x_glu * jax.nn.sigmoid(SWIGLU_ALPHA * x_glu) * (x_lin + 1)
        return act @ w2[e] + b2[e]

    out = lax.map(expert_block, (buf.reshape(n_blocks, MOE_BLOCK, d), block_e)).reshape(-1, d)
    contrib = out[dest].astype(jnp.float32) * sorted_gate[:, None]
    return jax.ops.segment_sum(contrib, sorted_tok, num_segments=n_tok).astype(h.dtype)


def setup_inputs(seed: int = 0) -> dict:
    key = jax.random.key(seed)
    ks = jax.random.split(key, 19)
    f32 = jnp.float32
    nrm = lambda k, shape, s: jax.random.normal(k, shape, f32) * s
    return {
        "x": nrm(ks[0], (BATCH, SEQ, D_MODEL), 1.0),
        "c": nrm(ks[1], (BATCH, D_MODEL), 1.0),
        "ctx": nrm(ks[2], (BATCH, CTX_LEN, D_MODEL), 1.0),
        "c_ctx": nrm(ks[3], (D_MODEL,), 1.0),
        "mod_w": nrm(ks[4], (DEPTH, D_MODEL, N_MOD * D_MODEL), 0.5 * D_MODEL ** -0.5),
        "mod_b": nrm(ks[5], (DEPTH, N_MOD * D_MODEL), 0.01),
        "norm_w": 1.0 + nrm(ks[6], (DEPTH, 2, D_MODEL), 0.01),
        "w_in": nrm(ks[7], (DEPTH, D_MODEL, IN_WIDTH), D_MODEL ** -0.5),
        "w_out": nrm(ks[8], (DEPTH, MIX_WIDTH, D_MODEL), MIX_WIDTH ** -0.5),
        "ret_decay": RET_DECAY_BASE + jnp.arange(RET_HEADS, dtype=f32) + nrm(ks[9], (DEPTH, 2, RET_HEADS), 0.05),
        "ret_norm_w": 1.0 + nrm(ks[10], (DEPTH, RET_V_WIDTH), 0.01),
        "router_w": nrm(ks[11], (DEPTH, D_MODEL, N_EXPERTS), D_MODEL ** -0.5),
        "router_b": nrm(ks[12], (DEPTH, N_EXPERTS), 0.01),
        "expert_w1": nrm(ks[13], (DEPTH, N_EXPERTS, D_MODEL, 2 * D_EXPERT), D_MODEL ** -0.5),
        "expert_b1": nrm(ks[14], (DEPTH, N_EXPERTS, 2 * D_EXPERT), 0.01),
        "expert_w2": nrm(ks[15], (DEPTH, N_EXPERTS, D_EXPERT, D_MODEL), D_EXPERT ** -0.5),
        "expert_b2": nrm(ks[16], (DEPTH, N_EXPERTS, D_MODEL), 0.01),
        "final_norm_w": 1.0 + nrm(ks[17], (D_MODEL,), 0.01),
    }


def reference(x, c, ctx, c_ctx, mod_w, mod_b, norm_w, w_in, w_out, ret_decay, ret_norm_w,
              router_w, router_b, expert_w1, expert_b1, expert_w2, expert_b2, final_norm_w):
    b, n_lat, d = x.shape
    rows = n_lat // GRID_W
    ang = axial_rope_angles(rows)
    s0 = jnp.zeros((b, RET_HEADS, RET_DK, RET_DV), jnp.float32)
    q_scale = RET_DK ** -0.5
    for l in range(DEPTH):
        has_ctx_out = l < DEPTH - 1
        mod_x = jnp.split((jax.nn.silu(c) @ mod_w[l] + mod_b[l])[:, None, :], N_MOD, axis=-1)
        mod_c = jnp.split(jax.nn.silu(c_ctx) @ mod_w[l] + mod_b[l], N_MOD, axis=-1)
        log_g = jnp.log1p(-jnp.exp2(-ret_decay[l].astype(jnp.float32)))

        hx = modulate(rmsnorm(x, norm_w[l, 0]), mod_x[0], mod_x[1])
        hc = modulate(rmsnorm(ctx, norm_w[l, 0]), mod_c[0], mod_c[1])
        fx, qx, kx, vx, gx = jnp.split(hx @ w_in[l], IN_SPLITS, axis=-1)
        fc, qc, kc, vc, gc = jnp.split(hc @ w_in[l], IN_SPLITS, axis=-1)
        qx = apply_rope(heads(qx, RET_DK) * q_scale, ang)
        kx = apply_rope(heads(kx, RET_DK), ang)
        vx = heads(vx, RET_DV)
        qc, kc, vc = heads(qc, RET_DK) * q_scale, heads(kc, RET_DK), heads(vc, RET_DV)

        sb_cf, s_cf = retention_states(kc, vc, log_g[0], s0)
        sb_cb, s_cb = retention_states(flip(kc), flip(vc), log_g[1], s0)
        sb_xf, _ = retention_states(kx, vx, log_g[0], s_cf)
        sb_xb, _ = retention_states(flip(kx), flip(vx), log_g[1], s_cb)
        o_x = (retention_outputs(qx, kx, vx, log_g[0], sb_xf)
               + flip(retention_outputs(flip(qx), flip(kx), flip(vx), log_g[1], sb_xb)))
        x = x + mod_x[2] * mixer_merge(fx, o_x, gx, ret_norm_w[l], w_out[l], x.dtype)
        if has_ctx_out:
            o_c = (retention_outputs(qc, kc, vc, log_g[0], sb_cf)
                   + flip(retention_outputs(flip(qc), flip(kc), flip(vc), log_g[1], sb_cb)))
            ctx = ctx + mod_c[2] * mixer_merge(fc, o_c, gc, ret_norm_w[l], w_out[l], ctx.dtype)

        hx = modulate(rmsnorm(x, norm_w[l, 1]), mod_x[3], mod_x[4])
        moe_args = (router_w[l], router_b[l], expert_w1[l], expert_b1[l], expert_w2[l], expert_b2[l])
        if has_ctx_out:
            hc = modulate(rmsnorm(ctx, norm_w[l, 1]), mod_c[3], mod_c[4])
            n_ctx_tok = hc.shape[0] * hc.shape[1]
            y = moe_ffn(jnp.concatenate([hc.reshape(-1, d), hx.reshape(-1, d)], axis=0), *moe_args)
            ctx = ctx + mod_c[5] * y[:n_ctx_tok].reshape(ctx.shape)
            x = x + mod_x[5] * y[n_ctx_tok:].reshape(x.shape)
        else:
            x = x + mod_x[5] * moe_ffn(hx.reshape(-1, d), *moe_args).reshape(x.shape)
    return rmsnorm(x, final_norm_w)
```

```python
import numpy as np
import ml_dtypes
from contextlib import ExitStack
import concourse.bass as bass
import concourse.mybir as mybir
from concourse.bass_utils import run_bass_kernel_spmd

F32 = mybir.dt.float32
BF16 = mybir.dt.bfloat16
U32 = mybir.dt.uint32
I32 = mybir.dt.int32
ALU = mybir.AluOpType
AF = mybir.ActivationFunctionType
AX = mybir.AxisListType

D = 1024
T = 4096
TC = 256
NCX = T // 128
NCC = TC // 128
NCS = NCX + NCC
NE = 32
DEPTH = 2
EPS = 1e-6


class Tok:
    __slots__ = ("sem", "val", "key", "owner")

    def __init__(self, sem, val, key, owner):
        self.sem, self.val, self.key, self.owner = sem, val, key, owner


class Buf:
    __slots__ = ("name", "w", "r")

    def __init__(self, name=""):
        self.name = name
        self.w = None
        self.r = {}


class EngS:
    def __init__(self, name, e, sem):
        self.name, self.e, self.sem = name, e, sem
        self.count = 0
        self.waited = {}


class FW:
    def __init__(self, nc, es, n_dma_sems=12):
        self.nc = nc
        self.engs = {}
        for name, e in (("pe", nc.tensor), ("act", nc.scalar), ("dve", nc.vector),
                        ("pool", nc.gpsimd), ("sp", nc.sync)):
            sem = es.enter_context(nc.semaphore("sem_" + name))
            self.engs[name] = EngS(name, e, sem)
        self.dsems = {}
        for q in ("sp", "pool", "act", "pe"):
            lst = []
            for i in range(n_dma_sems):
                s = es.enter_context(nc.semaphore(f"dsem_{q}{i}"))
                lst.append([s, 0, None])
            self.dsems[q] = [lst, 0]

    def wait(self, E, tok):
        if tok is None:
            return
        if E.waited.get(tok.key, 0) >= tok.val:
            return
        E.e.wait_ge(tok.sem, tok.val)
        E.waited[tok.key] = tok.val

    def _deps(self, reads, writes):
        deps = []
        for b in reads:
            if b.w is not None:
                deps.append(b.w)
        for b in writes:
            if b.w is not None:
                deps.append(b.w)
            deps.extend(b.r.values())
        return deps

    def op(self, en, fn, reads=(), writes=()):
        E = self.engs[en]
        for tok in self._deps(reads, writes):
            if en == "pe" and tok.owner == "pe":
                continue
            self.wait(E, tok)
        ins = fn(E.e)
        E.count += 1
        ins.then_inc(E.sem, 1)
        tok = Tok(E.sem, E.count, "E" + en, en)
        E.waited[tok.key] = max(E.waited.get(tok.key, 0), 0)
        for b in reads:
            b.r[tok.key] = tok
        for b in writes:
            b.w = tok
            b.r = {}
        return tok

    def dma(self, q, fn, reads=(), writes=()):
        E = self.engs[q]
        for tok in self._deps(reads, writes):
            self.wait(E, tok)
        lst, idx = self.dsems[q]
        ent = lst[idx % len(lst)]
        self.dsems[q][1] = idx + 1
        if ent[2] is not None:
            self.wait(E, ent[2])
        ins = fn(E.e)
        ent[1] += 16
        ins.then_inc(ent[0], 16)
        tok = Tok(ent[0], ent[1], f"D{q}{idx % len(lst)}", "dma")
        ent[2] = tok
        for b in reads:
            b.r[tok.key] = tok
        for b in writes:
            b.w = tok
            b.r = {}
        return tok

    def all_toks(self):
        toks = []
        for E in self.engs.values():
            if E.count > 0:
                toks.append(Tok(E.sem, E.count, "E" + E.name, E.name))
        for q, (lst, _) in self.dsems.items():
            for ent in lst:
                if ent[2] is not None:
                    toks.append(ent[2])
        return toks

    def barrier(self, engines=("pe", "act", "dve", "pool", "sp")):
        toks = self.all_toks()
        for en in engines:
            E = self.engs[en]
            for t in toks:
                if t.owner == en:
                    continue
                self.wait(E, t)


_UID = [0]


class Ring:
    def __init__(self, nc, es, name, n, shape, dtype):
        self.tiles = []
        for i in range(n):
            _UID[0] += 1
            self.tiles.append(es.enter_context(nc.sbuf_tensor(f"{name}{i}_u{_UID[0]}", shape, dtype)))
        self.bufs = [Buf(f"{name}{i}") for i in range(n)]
        self.i = 0

    def next(self):
        k = self.i % len(self.tiles)
        self.i += 1
        return self.tiles[k], self.bufs[k]


def bc_mid(ap2, n):
    P, A = ap2.shape
    return ap2.unsqueeze(2).to_broadcast([P, A, n])


def build_program(cfg):
    NSEQ = cfg.get("nseq", 2)
    LAYERS = cfg.get("layers", DEPTH)
    DEBUG = cfg.get("debug", False)
    STOP_AFTER = cfg.get("stop_after", None)
    NTOKC = NSEQ * NCS
    PBS = cfg.get("pb_stop", 99)

    nc = bass.Bass("TRN2", target_bir_lowering=False)
    es = ExitStack()
    fw = FW(nc, es)
    op, dma = fw.op, fw.dma

    def din(name, shape, dt=F32):
        return nc.dram_tensor(name, list(shape), dt, kind="ExternalInput").ap()

    def dscr(name, shape, dt=F32):
        return nc.dram_tensor(name, list(shape), dt, kind="Internal").ap()

    x_in = din("x", [NSEQ, T, D])
    ctx_in = din("ctx", [NSEQ, TC, D])
    cT_d = din("cT", [128, 8, 3])
    modw_d = din("mod_w", [DEPTH, D, 6 * D])
    modbT_d = din("mod_bT", [128, DEPTH, 48])
    modb_d = din("mod_b", [DEPTH, 6 * D])
    nwT_d = din("nwT", [128, DEPTH, 2, 8])
    nw1bc_d = din("nw1_bc", [128, DEPTH, D])
    wfT_d = din("wfT", [DEPTH, 512, D])
    wqkvg_d = din("w_qkvg", [DEPTH, D, 2048])
    wout_d = din("w_out", [DEPTH, D, D])
    rdbc_d = din("rd_bc", [128, DEPTH, 16])
    rnwbc_d = din("rnw_bc", [128, DEPTH, 512])
    rw_d = din("router_w", [DEPTH, D, NE])
    rbbc_d = din("rb_bc", [128, DEPTH, NE])
    fnwbc_d = din("fnw_bc", [128, D])
    dft_d = din("dft", [16, 4, 128, 2 * 8 * 256], BF16)
    dftc_d = din("dftc", [128, 2 * 2 * 256], BF16)
    bd64_d = din("bd64", [128, 2, 128])
    rope_d = din("rope", [128, 2, NCX, 32])
    tri_d = din("tri", [128, 4, 128])
    pcol_d = din("pcol", [128, 4])
    identb_d = din("identb", [128, 128], BF16)
    identf_d = din("identf", [128, 128])

    CAPMAX = 2048 * NSEQ + 512
    CAPB = CAPMAX // 512
    w1g_d = din("w1g", [DEPTH, NE * 128 * 4, 2048])
    w1l_d = din("w1l", [DEPTH, NE * 128 * 4, 2048])
    w2_d = din("w2h", [DEPTH, NE * 128 * 4, 2048])
    b1T_d = din("b1T", [DEPTH, NE * 128, 16])
    b2_d = din("b2", [DEPTH, NE, D])
    ustr_d = din("ustrict", [128, 128], BF16)
    rows_d = din("rowc", [128, 4, 128])
    NSB_MAX = (NTOKC * 128 * 4) // 512 + NE
    hbuf_d = dscr("hbuf", [NSB_MAX * 512, D], BF16)
    obuf_d = dscr("obuf", [NSB_MAX * 512, D])
    h2d_d = dscr("h2d", [NTOKC * 128, D], BF16)
    out_d = nc.dram_tensor("out", [NSEQ, T, D], F32, kind="ExternalOutput").ap()
    if DEBUG:
        dbg_x = nc.dram_tensor("dbg_x", [NTOKC * 128, D], F32, kind="ExternalOutput").ap()
        dbg_lg = nc.dram_tensor("dbg_lg", [NTOKC * 128, NE], F32, kind="ExternalOutput").ap()
        dbg_x2 = nc.dram_tensor("dbg_x2", [NTOKC * 128, D], F32, kind="ExternalOutput").ap()
        dbg_sched = nc.dram_tensor("dbg_sched", [128, 512], F32, kind="ExternalOutput").ap()

    spill_d = dscr("spill", [NTOKC, 128, 3072], BF16)
    zs_d = dscr("zs", [NSEQ, 128, NCX, 1024], BF16)
    zc_d = dscr("zc", [NSEQ, 128, NCC, 1024], BF16)
    yts_d = dscr("yts", [NTOKC, 128, 512], BF16)
    xmid_d = dscr("xmid", [NTOKC * 128, D])
    xnext_d = dscr("xnext", [NTOKC * 128, D])
    modrow_d = dscr("modrow", [3, 4 * D])

    def sb(name, shape, dt=F32, scope=es):
        _UID[0] += 1
        return scope.enter_context(nc.sbuf_tensor(f"{name}_u{_UID[0]}", list(shape), dt))

    identb = sb("identb", [128, 128], BF16)
    identf = sb("identf", [128, 128])
    tri = sb("tri", [128, 4, 128])
    pcol = sb("pcol", [128, 4])
    bd64 = sb("bd64", [128, 2, 128])
    epst = sb("epst", [128, 1])
    cT = sb("cT", [128, 8, 3])
    B_const = Buf("const")
    dma("sp", lambda e: e.dma_start(out=identb[:], in_=identb_d), writes=[B_const])
    dma("sp", lambda e: e.dma_start(out=identf[:], in_=identf_d), writes=[B_const])
    dma("sp", lambda e: e.dma_start(out=tri[:], in_=tri_d), writes=[B_const])
    dma("sp", lambda e: e.dma_start(out=pcol[:], in_=pcol_d), writes=[B_const])
    dma("sp", lambda e: e.dma_start(out=bd64[:], in_=bd64_d), writes=[B_const])
    dma("sp", lambda e: e.dma_start(out=cT[:], in_=cT_d), writes=[B_const])
    op("dve", lambda e: e.memset(epst[:], EPS), writes=[B_const])
    ustr = sb("ustr", [128, 128], BF16)
    onesb = sb("onesb", [128, 128], BF16)
    rowc = sb("rowc", [128, 4, 128])
    dma("sp", lambda e: e.dma_start(out=ustr[:], in_=ustr_d), writes=[B_const])
    dma("sp", lambda e: e.dma_start(out=rowc[:], in_=rows_d), writes=[B_const])
    op("dve", lambda e: e.memset(onesb[:], 1.0), writes=[B_const])
    siluT = sb("siluT", [128, 8, 3])
    op("act", lambda e: e.activation(out=siluT[:], in_=cT[:], func=AF.Silu), reads=[B_const], writes=[B_const])

    ps = es.enter_context(nc.psum_tensor("ps", [128, 8, 512], F32))
    PB = [Buf(f"psum{i}") for i in range(8)]

    def psb16(bank):
        return ps[:, bank, :].bitcast(BF16)

    def chunk_src(l, s, c):
        if l == 0:
            if c < NCC:
                return ctx_in[s, c * 128:(c + 1) * 128, :]
            return x_in[s, (c - NCC) * 128:(c - NCC + 1) * 128, :]
        ci = s * NCS + c
        return xnext_d[ci * 128:(ci + 1) * 128, :]

    for l in range(LAYERS):
        has_ctx_out = l < DEPTH - 1
        lt = ExitStack()
        Gd = sb("Gd", [128, NTOKC, NE], scope=lt)
        destk = sb("destk", [128, NTOKC, 4], I32, scope=lt)
        gatek = sb("gatek", [128, NTOKC, 4], scope=lt)
        basecap = sb("basecap", [128, NE], scope=lt)
        poskf = sb("poskf", [128, NTOKC, 4], scope=lt)
        ekf = sb("ekf", [128, NTOKC, 4], scope=lt)
        Brt_ = Buf("route")
        op("dve", lambda e: e.memset(basecap[:], 0.0), writes=[Brt_])
        lchunks = [(s_, c_) for s_ in range(NSEQ) for c_ in (range(NCS) if has_ctx_out else range(NCC, NCS))]
        NSB = (len(lchunks) * 128 * 4) // 512 + NE
        ls = ExitStack()
        logg = sb("logg", [128, 16], scope=ls)
        rdt = sb("rdt", [128, 16], scope=ls)
        DT8 = sb("DT8", [128, 8, 128], scope=ls)
        wk = sb("wk", [128, 8, 2], scope=ls)
        wq = sb("wq", [128, 8, 2], scope=ls)
        G8 = sb("G8", [128, 8], scope=ls)
        Gbc = sb("Gbc", [128, 8, 64], scope=ls)
        modF = sb("modF", [128, 16, 3], scope=ls)
        A1 = sb("A1", [128, 8, 3], scope=ls)
        wz = sb("wz", [128, 8, 1024], BF16, scope=ls)
        rnwbc = sb("rnwbc", [128, 512], scope=ls)
        nw1bc = sb("nw1bc", [128, D], scope=ls)
        rbbc = sb("rbbc", [128, NE], scope=ls)
        rw = sb("rw", [128, 8, NE], scope=ls)
        nwT = sb("nwT", [128, 2, 8], scope=ls)
        mbT = sb("mbT", [128, 48], scope=ls)
        BL = Buf("layerconst")
        dma("sp", lambda e: e.dma_start(out=rdt[:], in_=rdbc_d[:, l, :]), writes=[BL])
        dma("sp", lambda e: e.dma_start(out=rnwbc[:], in_=rnwbc_d[:, l, :]), writes=[BL])
        dma("sp", lambda e: e.dma_start(out=nw1bc[:], in_=nw1bc_d[:, l, :]), writes=[BL])
        dma("sp", lambda e: e.dma_start(out=rbbc[:], in_=rbbc_d[:, l, :]), writes=[BL])
        dma("sp", lambda e: e.dma_start(out=rw[:], in_=rw_d[l].rearrange("(k p) n -> p k n", p=128)), writes=[BL])
        dma("sp", lambda e: e.dma_start(out=nwT[:], in_=nwT_d[:, l, :, :]), writes=[BL])
        dma("sp", lambda e: e.dma_start(out=mbT[:], in_=modbT_d[:, l, :]), writes=[BL])
        op("act", lambda e: e.activation(out=logg[:], in_=rdt[:], func=AF.Exp, scale=-float(np.log(2.0))), reads=[BL], writes=[BL])
        op("dve", lambda e: e.tensor_scalar(logg[:], logg[:], -1.0, 1.0, op0=ALU.mult, op1=ALU.add), reads=[BL], writes=[BL])
        op("act", lambda e: e.activation(out=logg[:], in_=logg[:], func=AF.Ln), reads=[BL], writes=[BL])
        with ExitStack() as ss:
            tmpa = sb("tmpa", [128, 128], scope=ss)
            tmpb = sb("tmpb", [128, 128], scope=ss)
            Bt = Buf("tmpab")
            for h in range(8):
                op("act", lambda e: e.activation(out=tmpa[:], in_=tri[:, 0, :], func=AF.Exp, scale=logg[:, h:h + 1]), reads=[BL, B_const], writes=[Bt])
                op("dve", lambda e: e.tensor_tensor(out=tmpa[:], in0=tmpa[:], in1=tri[:, 2, :], op=ALU.mult), reads=[Bt], writes=[Bt])
                op("act", lambda e: e.activation(out=tmpb[:], in_=tri[:, 1, :], func=AF.Exp, scale=logg[:, 8 + h:9 + h]), reads=[BL, Bt], writes=[Bt])
                op("dve", lambda e: e.tensor_tensor(out=tmpb[:], in0=tmpb[:], in1=tri[:, 3, :], op=ALU.mult), reads=[Bt], writes=[Bt])
                op("dve", lambda e: e.tensor_tensor(out=DT8[:, h, :], in0=tmpa[:], in1=tmpb[:], op=ALU.add), reads=[Bt], writes=[BL])
            op("act", lambda e: e.activation(out=wk[:, :, 0], in_=logg[:, 0:8], func=AF.Exp, scale=pcol[:, 0:1]), reads=[BL], writes=[BL])
            op("act", lambda e: e.activation(out=wk[:, :, 1], in_=logg[:, 8:16], func=AF.Exp, scale=pcol[:, 2:3]), reads=[BL], writes=[BL])
            op("act", lambda e: e.activation(out=wq[:, :, 0], in_=logg[:, 0:8], func=AF.Exp, scale=pcol[:, 1:2]), reads=[BL], writes=[BL])
            op("act", lambda e: e.activation(out=wq[:, :, 1], in_=logg[:, 8:16], func=AF.Exp, scale=pcol[:, 3:4]), reads=[BL], writes=[BL])
            op("dve", lambda e: e.tensor_scalar(wq[:], wq[:], 0.125, None, op0=ALU.mult), reads=[BL], writes=[BL])
            op("act", lambda e: e.activation(out=G8[0:64, :], in_=logg[0:64, 0:8], func=AF.Exp, scale=128.0), reads=[BL], writes=[BL])
            op("act", lambda e: e.activation(out=G8[64:128, :], in_=logg[64:128, 8:16], func=AF.Exp, scale=128.0), reads=[BL], writes=[BL])
            op("dve", lambda e: e.tensor_copy(out=Gbc[:], in_=bc_mid(G8[:], 64)), reads=[BL], writes=[BL])
            fw.barrier()
        with ExitStack() as ss:
            mwr = Ring(nc, ss, "mw", 2, [128, 8, 512], F32)
            mbrow = sb("mbrow", [3, 4 * D], scope=ss)
            mrow = sb("mrow", [3, 4 * D], scope=ss)
            Bmb = Buf("mbrow")
            for r in range(3):
                dma("sp", lambda e: e.dma_start(out=mbrow[r:r + 1, :], in_=modb_d[l:l + 1, 2 * D:6 * D]), writes=[Bmb])
            psM = ps[:, 0, 0:64].rearrange("p (a b) -> p a b", b=4)
            for cb in range(12):
                m, half = cb // 2, cb % 2
                mw, mwb = mwr.next()
                dma("sp", lambda e: e.dma_start(out=mw[:], in_=modw_d[l, :, cb * 512:(cb + 1) * 512].rearrange("(k p) n -> p k n", p=128)), writes=[mwb])
                if m in (0, 1):
                    for jj in range(4):
                        idx = m * 8 + half * 4 + jj
                        for k in range(8):
                            op("pe", lambda e: e.matmul(psM[:, idx, 0:3], lhsT=mw[:, k, jj * 128:(jj + 1) * 128], rhs=siluT[:, k, :],
                                                        start=(k == 0), stop=(k == 7)), reads=[mwb, B_const], writes=[PB[0]])
                else:
                    for k in range(8):
                        op("pe", lambda e: e.matmul(ps[0:3, 1, :], lhsT=siluT[:, k, :], rhs=mw[:, k, :],
                                                    start=(k == 0), stop=(k == 7)), reads=[mwb, B_const], writes=[PB[1]])
                    c0 = (m - 2) * D + half * 512
                    op("dve", lambda e: e.tensor_tensor(out=mrow[0:3, c0:c0 + 512], in0=ps[0:3, 1, :], in1=mbrow[0:3, c0:c0 + 512], op=ALU.add),
                       reads=[PB[1], Bmb], writes=[Bmb])
            op("dve", lambda e: e.tensor_tensor(out=modF[:], in0=psM[:, :, 0:3], in1=bc_mid(mbT[:, 0:16], 3), op=ALU.add), reads=[PB[0], BL], writes=[BL])
            op("dve", lambda e: e.scalar_tensor_tensor(out=A1[:], in0=modF[:, 8:16, :], scalar=1.0, in1=bc_mid(nwT[:, 0, :], 3), op0=ALU.add, op1=ALU.mult),
               reads=[BL], writes=[BL])
            dma("sp", lambda e: e.dma_start(out=modrow_d[:, :], in_=mrow[0:3, :]), reads=[Bmb], writes=[BL])
            fw.barrier()
        with ExitStack() as ss:
            wft = sb("wft", [128, 4, D], scope=ss)
            Bw = Buf("wft")
            dma("sp", lambda e: e.dma_start(out=wft[:], in_=wfT_d[l].rearrange("(g p) d -> p g d", p=128)), writes=[Bw])
            for dk in range(8):
                for cs in range(2):
                    for gc in range(4):
                        op("pe", lambda e: e.matmul(ps[:, 2 + cs, gc * 128:(gc + 1) * 128], lhsT=wft[:, gc, dk * 128:(dk + 1) * 128], rhs=bd64[:, cs, :],
                                                    start=True, stop=True), reads=[Bw, B_const], writes=[PB[2 + cs]])
                    op("act", lambda e: e.activation(out=wz[:, dk, cs * 512:(cs + 1) * 512], in_=ps[:, 2 + cs, :], func=AF.Copy), reads=[PB[2 + cs]], writes=[BL])
            fw.barrier()

        if STOP_AFTER == "setup":
            ls.close()
            lt.close()
            break
        for s in range(NSEQ):
            sq = ExitStack()
            kvs = sb("kvs", [128, NCS, 512], BF16, scope=sq)
            Bkv = Buf("kvs")
            with ExitStack() as pa:
                wqk = sb("wqk", [128, 8, 2048], BF16, scope=pa)
                Bwqk = Buf("wqk")
                for k in range(8):
                    dma("pool", lambda e: e.dma_start(out=wqk[:, k, :], in_=wqkvg_d[l, k * 128:(k + 1) * 128, :]), writes=[Bwqk])
                ropet = sb("ropet", [128, 2, NCX, 32], scope=pa)
                dma("sp", lambda e: e.dma_start(out=ropet[:], in_=rope_d), writes=[Bwqk])
                xr = Ring(nc, pa, "xa", 2, [128, D], F32)
                junk = sb("junkA", [128, D], BF16, scope=pa)
                Bj = Buf("junkA")
                ssq = sb("ssqA", [128, 4], scope=pa)
                xnr = Ring(nc, pa, "xn", 2, [128, D], BF16)
                hTr = Ring(nc, pa, "hT", 2, [128, 8, 128], BF16)
                qkr = Ring(nc, pa, "qkrot", 2, [128, 16, 64], BF16)
                rtmp = sb("rtmp", [128, 4, 16, 32], scope=pa)
                Brt = Buf("rtmp")
                kwr = Ring(nc, pa, "kw", 2, [128, 8, 2, 64], BF16)
                qcr = Ring(nc, pa, "qc", 2, [128, 8, 2, 64], BF16)
                sgt = sb("sgt", [128, 512], scope=pa)
                Bsg = Buf("sgt")
                recr = Ring(nc, pa, "recA", 2, [128, 3072], BF16)
                zrr = Ring(nc, pa, "zrec", 2, [128, 1024], BF16)
                for c in range(NCS):
                    ci = s * NCS + c
                    is_ctx = c < NCC
                    col = 2 if is_ctx else s
                    xt, xb = xr.next()
                    dma("sp", lambda e: e.dma_start(out=xt[:], in_=chunk_src(l, s, c)), writes=[xb])
                    op("dve", lambda e: e.scalar_tensor_tensor(out=junk[:], in0=xt[:], scalar=1.0, in1=xt[:], op0=ALU.mult, op1=ALU.mult, accum_out=ssq[:, 0:1]),
                       reads=[xb], writes=[Bj])
                    op("act", lambda e: e.activation(out=ssq[:, 1:2], in_=ssq[:, 0:1], func=AF.Sqrt, scale=1.0 / D, bias=epst[:, 0:1]), reads=[Bj], writes=[Bj])
                    op("dve", lambda e: e.reciprocal(out=ssq[:, 2:3], in_=ssq[:, 1:2]), reads=[Bj], writes=[Bj])
                    xn, xnb = xnr.next()
                    op("dve", lambda e: e.tensor_scalar(xn[:], xt[:], ssq[:, 2:3], None, op0=ALU.mult), reads=[xb, Bj], writes=[xnb])
                    pT = psb16(0)
                    for k in range(8):
                        op("pe", lambda e: e.transpose(out=pT[:, k * 128:(k + 1) * 128], in_=xn[:, k * 128:(k + 1) * 128], identity=identb[:]),
                           reads=[xnb, B_const], writes=[PB[0]])
                    hT, hTb = hTr.next()
                    for k in range(8):
                        op("act", lambda e: e.activation(out=hT[:, k, :], in_=pT[:, k * 128:(k + 1) * 128], func=AF.Identity,
                                                         scale=A1[:, k, col:col + 1], bias=modF[:, k, col:col + 1]), reads=[PB[0], BL], writes=[hTb])
                    for grp in range(6):
                        for k in range(8):
                            rhs = wz[:, k, grp * 512:(grp + 1) * 512] if grp < 2 else wqk[:, k, (grp - 2) * 512:(grp - 1) * 512]
                            op("pe", lambda e: e.matmul(ps[:, 1 + grp, :], lhsT=hT[:, k, :], rhs=rhs, start=(k == 0), stop=(k == 7)),
                               reads=[hTb, BL, Bwqk], writes=[PB[1 + grp]])
                    rec, recb = recr.next()
                    if (not is_ctx) or has_ctx_out:
                        zr, zrb = zrr.next()
                        op("act", lambda e: e.activation(out=zr[:, 0:512], in_=ps[:, 1, :], func=AF.Copy), reads=[PB[1]], writes=[zrb])
                        op("act", lambda e: e.activation(out=zr[:, 512:1024], in_=ps[:, 2, :], func=AF.Copy), reads=[PB[2]], writes=[zrb])
                        if is_ctx:
                            dma("sp", lambda e: e.dma_start(out=zc_d[s, :, c, :], in_=zr[:]), reads=[zrb])
                        else:
                            dma("sp", lambda e: e.dma_start(out=zs_d[s, :, c - NCC, :], in_=zr[:]), reads=[zrb])
                    qk, qkb = qkr.next()
                    psqk = ps[:, 3:5, :].rearrange("p a (h d) -> p (a h) d", d=64)
                    if is_ctx:
                        op("act", lambda e: e.activation(out=qk[:], in_=psqk, func=AF.Copy), reads=[PB[3], PB[4]], writes=[qkb])
                    else:
                        cx = c - NCC
                        cosb = ropet[:, 0, cx, :].unsqueeze(1).to_broadcast([128, 16, 32])
                        sinb = ropet[:, 1, cx, :].unsqueeze(1).to_broadcast([128, 16, 32])
                        t1, t2 = psqk[:, :, 0:32], psqk[:, :, 32:64]
                        op("dve", lambda e: e.tensor_tensor(out=rtmp[:, 0], in0=t1, in1=cosb, op=ALU.mult), reads=[PB[3], PB[4], Bwqk], writes=[Brt])
                        op("dve", lambda e: e.tensor_tensor(out=rtmp[:, 1], in0=t2, in1=sinb, op=ALU.mult), reads=[PB[3], PB[4], Bwqk], writes=[Brt])
                        op("dve", lambda e: e.tensor_tensor(out=rtmp[:, 2], in0=t1, in1=sinb, op=ALU.mult), reads=[PB[3], PB[4], Bwqk], writes=[Brt])
                        op("dve", lambda e: e.tensor_tensor(out=rtmp[:, 3], in0=t2, in1=cosb, op=ALU.mult), reads=[PB[3], PB[4], Bwqk], writes=[Brt])
                        op("dve", lambda e: e.tensor_tensor(out=qk[:, :, 0:32], in0=rtmp[:, 0], in1=rtmp[:, 1], op=ALU.subtract), reads=[Brt], writes=[qkb])
                        op("dve", lambda e: e.tensor_tensor(out=qk[:, :, 32:64], in0=rtmp[:, 2], in1=rtmp[:, 3], op=ALU.add), reads=[Brt], writes=[qkb])
                    kw, kwb = kwr.next()
                    qc, qcb = qcr.next()
                    op("dve", lambda e: e.tensor_tensor(out=kw[:], in0=qk[:, 8:16, :].unsqueeze(2).to_broadcast([128, 8, 2, 64]),
                                                        in1=wk[:].unsqueeze(3).to_broadcast([128, 8, 2, 64]), op=ALU.mult), reads=[qkb, BL], writes=[kwb])
                    op("dve", lambda e: e.tensor_tensor(out=qc[:], in0=qk[:, 0:8, :].unsqueeze(2).to_broadcast([128, 8, 2, 64]),
                                                        in1=wq[:].unsqueeze(3).to_broadcast([128, 8, 2, 64]), op=ALU.mult), reads=[qkb, BL], writes=[qcb])
                    op("act", lambda e: e.activation(out=rec[:, 2048:2560], in_=ps[:, 5, :], func=AF.Copy), reads=[PB[5]], writes=[recb])
                    op("act", lambda e: e.activation(out=sgt[:], in_=ps[:, 6, :], func=AF.Silu), reads=[PB[6]], writes=[Bsg])
                    op("dve", lambda e: e.tensor_tensor(out=rec[:, 2560:3072], in0=sgt[:], in1=rnwbc[:], op=ALU.mult), reads=[Bsg, BL], writes=[recb])
                    for h in range(8):
                        op("pe", lambda e: e.matmul(ps[:, 0, h * 64:(h + 1) * 64], lhsT=kw[:, h].rearrange("p a d -> p (a d)"), rhs=rec[:, 2048 + h * 64:2048 + (h + 1) * 64],
                                                    start=True, stop=True), reads=[kwb, recb], writes=[PB[0]])
                    op("act", lambda e: e.activation(out=kvs[:, c, :], in_=ps[:, 0, :], func=AF.Copy), reads=[PB[0]], writes=[Bkv])
                    p7 = psb16(7)
                    for j in range(8):
                        op("pe", lambda e: e.transpose(out=p7[:, j * 128:(j + 1) * 128], in_=qk[:, 2 * j:2 * j + 2, :].rearrange("p a d -> p (a d)"), identity=identb[:]),
                           reads=[qkb, B_const], writes=[PB[7]])
                    op("act", lambda e: e.activation(out=rec[:, 0:1024], in_=p7[:, :], func=AF.Copy), reads=[PB[7]], writes=[recb])
                    for h in range(8):
                        op("pe", lambda e: e.transpose(out=p7[:, h * 128:(h + 1) * 128], in_=qc[:, h].rearrange("p a d -> p (a d)"), identity=identb[:]),
                           reads=[qcb, B_const], writes=[PB[7]])
                    op("act", lambda e: e.activation(out=rec[:, 1024:2048], in_=p7[:, :], func=AF.Copy), reads=[PB[7]], writes=[recb])
                    dma("sp", lambda e: e.dma_start(out=spill_d[ci], in_=rec[:]), reads=[recb])
                fw.barrier()
            if STOP_AFTER == "passA":
                sq.close()
                break
            with ExitStack() as sc:
                St = sb("St", [128, 512], scope=sc)
                tmpS = sb("tmpS", [128, 512], scope=sc)
                Bs = Buf("scan")
                op("dve", lambda e: e.memset(St[:], 0.0), writes=[Bs])
                Gf = Gbc[:].rearrange("p h d -> p (h d)")
                order_f = list(range(NCS))
                order_b = [1, 0] + list(range(NCS - 1, NCC - 1, -1))
                for (lo, hi, order) in ((0, 64, order_f), (64, 128, order_b)):
                    for c in order:
                        op("dve", lambda e: e.tensor_copy(out=tmpS[lo:hi, :], in_=kvs[lo:hi, c, :]), reads=[Bkv, Bs], writes=[Bs])
                        op("dve", lambda e: e.tensor_copy(out=kvs[lo:hi, c, :], in_=St[lo:hi, :]), reads=[Bs], writes=[Bkv])
                        op("dve", lambda e: e.tensor_tensor(out=St[lo:hi, :], in0=St[lo:hi, :], in1=Gf[lo:hi, :], op=ALU.mult), reads=[Bs, BL], writes=[Bs])
                        op("dve", lambda e: e.tensor_tensor(out=St[lo:hi, :], in0=St[lo:hi, :], in1=tmpS[lo:hi, :], op=ALU.add), reads=[Bs], writes=[Bs])
                fw.barrier()
            if STOP_AFTER == "scan":
                sq.close()
                break
            with ExitStack() as fo:
                Zt = sb("Zt", [128, NCX, 1024], BF16, scope=fo)
                Bz = Buf("Zt")
                for q4 in range(4):
                    dma("sp", lambda e: e.dma_start(out=Zt[:, q4 * 8:(q4 + 1) * 8, :], in_=zs_d[s, :, q4 * 8:(q4 + 1) * 8, :]), writes=[Bz])
                tabr = Ring(nc, fo, "dtab", 4, [128, 2, 8, 256], BF16)
                ystr = Ring(nc, fo, "yst", 2, [128, 2, 4, 128], BF16)
                for kb in range(16):
                    bank0 = 4 * (kb % 2)
                    for tq in range(4):
                        tab, tabb = tabr.next()
                        dma("sp", lambda e: e.dma_start(out=tab[:].rearrange("p a b c -> p (a b c)"), in_=dft_d[kb, tq]), writes=[tabb])
                        for n in range(4):
                            for tcc in range(8):
                                tt = tq * 8 + tcc
                                op("pe", lambda e: e.matmul(ps[:, bank0 + n, 0:256], lhsT=Zt[:, tt, n * 128:(n + 1) * 128], rhs=tab[:, 0, tcc, :],
                                                            start=(tq == 0 and tcc == 0), stop=False), reads=[Bz, tabb], writes=[PB[bank0 + n]])
                                op("pe", lambda e: e.matmul(ps[:, bank0 + n, 0:256], lhsT=Zt[:, tt, 512 + n * 128:512 + (n + 1) * 128], rhs=tab[:, 1, tcc, :],
                                                            start=False, stop=(tq == 3 and tcc == 7)), reads=[Bz, tabb], writes=[PB[bank0 + n]])
                    yst, ystb = ystr.next()
                    for n in range(4):
                        op("act", lambda e: e.activation(out=yst[:, :, n, :], in_=ps[:, bank0 + n, 0:256].rearrange("p (a b) -> p a b", b=128), func=AF.Copy),
                           reads=[PB[bank0 + n]], writes=[ystb])
                    ci0 = s * NCS + NCC + 2 * kb
                    dma("sp", lambda e: e.dma_start(out=yts_d[ci0:ci0 + 2].rearrange("c p f -> p c f"), in_=yst[:].rearrange("p a n t -> p a (n t)")), reads=[ystb])
                if has_ctx_out:
                    Zc = sb("Zc", [128, NCC, 1024], BF16, scope=fo)
                    tabc = sb("tabc", [128, 2, 2, 256], BF16, scope=fo)
                    Bzc = Buf("Zc")
                    dma("sp", lambda e: e.dma_start(out=Zc[:], in_=zc_d[s]), writes=[Bzc])
                    dma("sp", lambda e: e.dma_start(out=tabc[:].rearrange("p a b c -> p (a b c)"), in_=dftc_d), writes=[Bzc])
                    yst, ystb = ystr.next()
                    for n in range(4):
                        for tt in range(2):
                            op("pe", lambda e: e.matmul(ps[:, n, 0:256], lhsT=Zc[:, tt, n * 128:(n + 1) * 128], rhs=tabc[:, 0, tt, :], start=(tt == 0), stop=False),
                               reads=[Bzc], writes=[PB[n]])
                            op("pe", lambda e: e.matmul(ps[:, n, 0:256], lhsT=Zc[:, tt, 512 + n * 128:512 + (n + 1) * 128], rhs=tabc[:, 1, tt, :], start=False, stop=(tt == 1)),
                               reads=[Bzc], writes=[PB[n]])
                        op("act", lambda e: e.activation(out=yst[:, :, n, :], in_=ps[:, n, 0:256].rearrange("p (a b) -> p a b", b=128), func=AF.Copy),
                           reads=[PB[n]], writes=[ystb])
                    ci0 = s * NCS
                    dma("sp", lambda e: e.dma_start(out=yts_d[ci0:ci0 + 2].rearrange("c p f -> p c f"), in_=yst[:].rearrange("p a n t -> p a (n t)")), reads=[ystb])
                fw.barrier()
            if STOP_AFTER == "fourier":
                sq.close()
                break
            with ExitStack() as pb:
                wout = sb("wout", [128, 8, D], BF16, scope=pb)
                Bwo = Buf("wout")
                for k in range(8):
                    dma("pool", lambda e: e.dma_start(out=wout[:, k, :], in_=wout_d[l, k * 128:(k + 1) * 128, :]), writes=[Bwo])
                bcs = {}
                Bbc = Buf("bc")
                for (nm, col) in (("x", s), ("c", 2)):
                    if nm == "c" and not has_ctx_out:
                        continue
                    G1 = sb(f"G1bc{nm}", [128, D], scope=pb)
                    A2 = sb(f"A2bc{nm}", [128, D], scope=pb)
                    B2 = sb(f"B2bc{nm}", [128, D], scope=pb)
                    dma("sp", lambda e: e.dma_start(out=G1[:], in_=modrow_d[col:col + 1, 0:D].partition_broadcast(128)), writes=[Bbc])
                    dma("sp", lambda e: e.dma_start(out=B2[:], in_=modrow_d[col:col + 1, D:2 * D].partition_broadcast(128)), writes=[Bbc])
                    dma("sp", lambda e: e.dma_start(out=A2[:], in_=modrow_d[col:col + 1, 2 * D:3 * D].partition_broadcast(128)), writes=[Bbc])
                    op("dve", lambda e: e.scalar_tensor_tensor(out=A2[:], in0=A2[:], scalar=1.0, in1=nw1bc[:], op0=ALU.add, op1=ALU.mult), reads=[Bbc, BL], writes=[Bbc])
                    bcs[nm] = (G1, A2, B2)
                recr = Ring(nc, pb, "recB", 2, [128, 3072], BF16)
                catr = Ring(nc, pb, "catT", 2, [128, 8, 128], BF16)
                xr = Ring(nc, pb, "xb", 2, [128, D], F32)
                PTr = Ring(nc, pb, "PT", 2, [128, 8, 128], BF16)
                gn = sb("gn", [128, 3, 8, 64], scope=pb)
                gs = sb("gs", [128, 4, 8], scope=pb)
                Bgn = Buf("gn")
                yr = Ring(nc, pb, "yb", 2, [128, 512], BF16)
                x1r = Ring(nc, pb, "x1", 2, [128, D], F32)
                junk = sb("junkB", [128, D], BF16, scope=pb)
                ssq = sb("ssqB", [128, 4], scope=pb)
                Bj = Buf("junkB")
                h2r = Ring(nc, pb, "h2", 2, [128, D], F32)
                h2Tr = Ring(nc, pb, "h2T", 2, [128, 8, 128], F32)
                lgr = Ring(nc, pb, "lg", 2, [128, NE], F32)
                rtr = Ring(nc, pb, "rt", 2, [128, 4, NE], F32)
                smr = Ring(nc, pb, "sm", 2, [128, 16], F32)
                mbr = Ring(nc, pb, "mb", 2, [128, NE], BF16)
                h2br = Ring(nc, pb, "h2b", 2, [128, D], BF16)
                chunks = list(range(NCS)) if has_ctx_out else list(range(NCC, NCS))
                for c in chunks:
                    ci = s * NCS + c
                    is_ctx = c < NCC
                    G1, A2, B2 = bcs["c" if is_ctx else "x"]
                    rec, recb = recr.next()
                    dma("sp", lambda e: e.dma_start(out=rec[:], in_=spill_d[ci]), writes=[recb])
                    cat, catb = catr.next()
                    dma("sp", lambda e: e.dma_start(out=cat[:, 0:4, :].rearrange("p a t -> p (a t)"), in_=yts_d[ci]), writes=[catb])
                    xt, xb = xr.next()
                    dma("sp", lambda e: e.dma_start(out=xt[:], in_=chunk_src(l, s, c)), writes=[xb])
                    if PBS <= 1:
                        continue
                    for h in range(8):
                        hp, hh = h // 2, h % 2
                        op("pe", lambda e: e.matmul(ps[:, hh, hp * 128:(hp + 1) * 128],
                                                    lhsT=rec[hh * 64:(hh + 1) * 64, 512 + hp * 128:512 + (hp + 1) * 128],
                                                    rhs=rec[hh * 64:(hh + 1) * 64, hp * 128:(hp + 1) * 128], start=True, stop=True),
                           reads=[recb], writes=[PB[hh]])
                    PT, PTb = PTr.next()
                    for g2 in range(2):
                        op("dve", lambda e: e.tensor_tensor(out=PT[:].rearrange("p (a b) t -> p a b t", b=2)[:, :, g2, :], in0=ps[:, g2, :].rearrange("p (h t) -> p h t", t=128),
                                                            in1=DT8[:].rearrange("p (a b) t -> p a b t", b=2)[:, :, g2, :], op=ALU.mult), reads=[PB[g2], BL], writes=[PTb])
                    if PBS <= 2:
                        continue
                    for h in range(8):
                        op("pe", lambda e: e.matmul(ps[:, 2, h * 64:(h + 1) * 64], lhsT=PT[:, h, :], rhs=rec[:, 2048 + h * 64:2048 + (h + 1) * 64], start=True, stop=False),
                           reads=[PTb, recb], writes=[PB[2]])
                        op("pe", lambda e: e.matmul(ps[:, 2, h * 64:(h + 1) * 64], lhsT=rec[:, 1024 + h * 128:1024 + (h + 1) * 128], rhs=kvs[:, c, h * 64:(h + 1) * 64], start=False, stop=True),
                           reads=[recb, Bkv], writes=[PB[2]])
                    if PBS <= 3:
                        continue
                    po = ps[:, 2, :].rearrange("p (h d) -> p h d", d=64)
                    op("dve", lambda e: e.tensor_reduce(out=gs[:, 0, :], in_=po, axis=AX.X, op=ALU.add), reads=[PB[2]], writes=[Bgn])
                    op("dve", lambda e: e.tensor_scalar(gs[:, 1, :], gs[:, 0, :], -1.0 / 64, None, op0=ALU.mult), reads=[Bgn], writes=[Bgn])
                    op("dve", lambda e: e.tensor_tensor(out=gn[:, 0], in0=po, in1=bc_mid(gs[:, 1, :], 64), op=ALU.add), reads=[PB[2], Bgn], writes=[Bgn])
                    op("dve", lambda e: e.tensor_tensor(out=gn[:, 1], in0=gn[:, 0], in1=gn[:, 0], op=ALU.mult), reads=[Bgn], writes=[Bgn])
                    op("dve", lambda e: e.tensor_reduce(out=gs[:, 2, :], in_=gn[:, 1], axis=AX.X, op=ALU.add), reads=[Bgn], writes=[Bgn])
                    op("act", lambda e: e.activation(out=gs[:, 3, :], in_=gs[:, 2, :], func=AF.Sqrt, scale=1.0 / 64, bias=epst[:, 0:1]), reads=[Bgn], writes=[Bgn])
                    op("dve", lambda e: e.reciprocal(out=gs[:, 2, :], in_=gs[:, 3, :]), reads=[Bgn], writes=[Bgn])
                    op("dve", lambda e: e.tensor_tensor(out=gn[:, 2], in0=gn[:, 0], in1=bc_mid(gs[:, 2, :], 64), op=ALU.mult), reads=[Bgn], writes=[Bgn])
                    yb_, ybb = yr.next()
                    op("dve", lambda e: e.tensor_tensor(out=yb_[:], in0=gn[:, 2].rearrange("p h d -> p (h d)"), in1=rec[:, 2560:3072], op=ALU.mult), reads=[Bgn, recb], writes=[ybb])
                    if PBS <= 4:
                        continue
                    p3 = psb16(3)
                    for j in range(4):
                        op("pe", lambda e: e.transpose(out=p3[:, j * 128:(j + 1) * 128], in_=yb_[:, j * 128:(j + 1) * 128], identity=identb[:]), reads=[ybb, B_const], writes=[PB[3]])
                    op("act", lambda e: e.activation(out=cat[:, 4:8, :].rearrange("p a t -> p (a t)"), in_=p3[:, 0:512], func=AF.Copy), reads=[PB[3]], writes=[catb])
                    if PBS <= 5:
                        continue
                    for hf in range(2):
                        for m in range(8):
                            op("pe", lambda e: e.matmul(ps[:, 4 + hf, :], lhsT=cat[:, m, :], rhs=wout[:, m, hf * 512:(hf + 1) * 512], start=(m == 0), stop=(m == 7)),
                               reads=[catb, Bwo], writes=[PB[4 + hf]])
                    x1, x1b = x1r.next()
                    op("dve", lambda e: e.tensor_tensor(out=x1[:], in0=ps[:, 4:6, :].rearrange("p a n -> p (a n)"), in1=G1[:], op=ALU.mult), reads=[PB[4], PB[5], Bbc], writes=[x1b])
                    op("dve", lambda e: e.tensor_tensor(out=x1[:], in0=x1[:], in1=xt[:], op=ALU.add), reads=[x1b, xb], writes=[x1b])
                    dma("sp", lambda e: e.dma_start(out=xmid_d[ci * 128:(ci + 1) * 128, :], in_=x1[:]), reads=[x1b])
                    if DEBUG:
                        dma("sp", lambda e: e.dma_start(out=dbg_x[ci * 128:(ci + 1) * 128, :], in_=x1[:]), reads=[x1b])
                    if PBS <= 6:
                        continue
                    op("dve", lambda e: e.scalar_tensor_tensor(out=junk[:], in0=x1[:], scalar=1.0, in1=x1[:], op0=ALU.mult, op1=ALU.mult, accum_out=ssq[:, 0:1]),
                       reads=[x1b], writes=[Bj])
                    op("act", lambda e: e.activation(out=ssq[:, 1:2], in_=ssq[:, 0:1], func=AF.Sqrt, scale=1.0 / D, bias=epst[:, 0:1]), reads=[Bj], writes=[Bj])
                    op("dve", lambda e: e.reciprocal(out=ssq[:, 2:3], in_=ssq[:, 1:2]), reads=[Bj], writes=[Bj])
                    h2, h2b = h2r.next()
                    op("dve", lambda e: e.scalar_tensor_tensor(out=h2[:], in0=x1[:], scalar=ssq[:, 2:3], in1=A2[:], op0=ALU.mult, op1=ALU.mult), reads=[x1b, Bj, Bbc], writes=[h2b])
                    op("dve", lambda e: e.tensor_tensor(out=h2[:], in0=h2[:], in1=B2[:], op=ALU.add), reads=[h2b, Bbc], writes=[h2b])
                    if PBS <= 7:
                        continue
                    for k in range(8):
                        op("pe", lambda e: e.transpose(out=ps[:, 6 + k // 4, (k % 4) * 128:(k % 4 + 1) * 128], in_=h2[:, k * 128:(k + 1) * 128], identity=identf[:]),
                           reads=[h2b, B_const], writes=[PB[6 + k // 4]])
                    h2T, h2Tb = h2Tr.next()
                    op("act", lambda e: e.activation(out=h2T[:].rearrange("p k t -> p (k t)"), in_=ps[:, 6:8, :].rearrange("p a n -> p (a n)"), func=AF.Copy),
                       reads=[PB[6], PB[7]], writes=[h2Tb])
                    for k in range(8):
                        op("pe", lambda e: e.matmul(ps[:, 3, 0:NE], lhsT=h2T[:, k, :], rhs=rw[:, k, :], start=(k == 0), stop=(k == 7)), reads=[h2Tb, BL], writes=[PB[3]])
                    lg, lgb = lgr.next()
                    op("dve", lambda e: e.tensor_tensor(out=lg[:], in0=ps[:, 3, 0:NE], in1=rbbc[:], op=ALU.add), reads=[PB[3], BL], writes=[lgb])
                    if DEBUG:
                        dma("sp", lambda e: e.dma_start(out=dbg_lg[ci * 128:(ci + 1) * 128, :], in_=lg[:]), reads=[lgb])
                    if PBS <= 8:
                        continue
                    rt, rtb = rtr.next()
                    sm, smb = smr.next()
                    op("dve", lambda e: e.max(out=sm[:, 0:8], in_=lg[:]), reads=[lgb], writes=[smb])
                    op("dve", lambda e: e.tensor_scalar(rt[:, 0, :], lg[:], sm[:, 3:4], None, op0=ALU.is_ge), reads=[lgb, smb], writes=[rtb])
                    op("dve", lambda e: e.tensor_scalar(sm[:, 8:9], sm[:, 0:1], -1.0, None, op0=ALU.mult), reads=[smb], writes=[smb])
                    op("act", lambda e: e.activation(out=rt[:, 1, :], in_=lg[:], func=AF.Exp, bias=sm[:, 8:9], scale=1.0), reads=[lgb, smb], writes=[rtb])
                    op("dve", lambda e: e.scalar_tensor_tensor(out=rt[:, 1, :], in0=rt[:, 1, :], scalar=1.0, in1=rt[:, 0, :], op0=ALU.mult, op1=ALU.mult, accum_out=sm[:, 9:10]),
                       reads=[rtb], writes=[rtb, smb])
                    op("dve", lambda e: e.reciprocal(out=sm[:, 10:11], in_=sm[:, 9:10]), reads=[smb], writes=[smb])
                    op("dve", lambda e: e.tensor_scalar(Gd[:, ci, :], rt[:, 1, :], sm[:, 10:11], None, op0=ALU.mult), reads=[rtb, smb], writes=[Brt_])
                    mb_, mbb = mbr.next()
                    op("dve", lambda e: e.tensor_copy(out=mb_[:], in_=rt[:, 0, :]), reads=[rtb], writes=[mbb])
                    op("pe", lambda e: e.matmul(ps[:, 3, 32:64], lhsT=ustr[:], rhs=mb_[:], start=True, stop=True), reads=[mbb, B_const], writes=[PB[3]])
                    op("pe", lambda e: e.matmul(ps[:, 3, 64:96], lhsT=onesb[:], rhs=mb_[:], start=True, stop=True), reads=[mbb, B_const], writes=[PB[3]])
                    op("dve", lambda e: e.tensor_tensor(out=rt[:, 2, :], in0=ps[:, 3, 32:64], in1=basecap[:], op=ALU.add), reads=[PB[3], Brt_], writes=[rtb])
                    op("dve", lambda e: e.tensor_tensor(out=basecap[:], in0=ps[:, 3, 64:96], in1=basecap[:], op=ALU.add), reads=[PB[3], Brt_], writes=[Brt_])
                    for k4 in range(4):
                        op("dve", lambda e: e.scalar_tensor_tensor(out=rt[:, 3, :], in0=lg[:], scalar=sm[:, k4:k4 + 1], in1=rt[:, 2, :], op0=ALU.is_equal, op1=ALU.mult,
                                                                   accum_out=poskf[:, ci, k4:k4 + 1]), reads=[lgb, smb, rtb, Brt_], writes=[rtb, Brt_])
                        op("dve", lambda e: e.scalar_tensor_tensor(out=rt[:, 3, :], in0=lg[:], scalar=sm[:, k4:k4 + 1], in1=rowc[:, 1, 0:NE], op0=ALU.is_equal, op1=ALU.mult,
                                                                   accum_out=ekf[:, ci, k4:k4 + 1]), reads=[lgb, smb, rtb, Brt_], writes=[rtb, Brt_])
                        op("dve", lambda e: e.scalar_tensor_tensor(out=rt[:, 3, :], in0=lg[:], scalar=sm[:, k4:k4 + 1], in1=Gd[:, ci, :], op0=ALU.is_equal, op1=ALU.mult,
                                                                   accum_out=gatek[:, ci, k4:k4 + 1]), reads=[lgb, smb, rtb, Brt_], writes=[rtb, Brt_])
                    h2b_, h2bb = h2br.next()
                    op("act", lambda e: e.activation(out=h2b_[:], in_=h2[:], func=AF.Copy), reads=[h2b], writes=[h2bb])
                    dma("sp", lambda e: e.dma_start(out=h2d_d[ci * 128:(ci + 1) * 128, :], in_=h2b_[:]), reads=[h2bb])
                fw.barrier()
            sq.close()
        ls.close()
        if STOP_AFTER in ("passA", "scan", "fourier", "mixer0"):
            lt.close()
            break

        idxW = sb("idxW", [128, NSB, 4], I32, scope=lt)
        idxB = sb("idxB", [128, NSB], I32, scope=lt)
        Bsch = Buf("sched")
        with ExitStack() as sc:
            cnt = sb("cnt", [128, NE], scope=sc)
            nblk = sb("nblk", [128, NE], scope=sc)
            cum = sb("cum", [128, NE], scope=sc)
            cumex = sb("cumex", [128, NE], scope=sc)
            ones32 = sb("ones32", [128, NE], scope=sc)
            cmp17 = sb("cmp17", [128, NE, 17], scope=sc)
            cmpb = sb("cmpb", [128, NSB, NE], scope=sc)
            ebt = sb("ebt", [128, 4, NSB], scope=sc)
            idf = sb("idf", [128, 1, NSB, 4], scope=sc)
            cmpd = sb("cmpd", [128, NTOKC * 4, NE], scope=sc)
            destf = sb("destf", [128, NTOKC * 4], scope=sc)
            bvals = rowc[:, 3, 0:NSB]
            op("dve", lambda e: e.tensor_copy(out=cnt[:], in_=basecap[:]), reads=[Brt_, B_const], writes=[Bsch])
            op("dve", lambda e: e.tensor_tensor(out=cmp17[:], in0=bc_mid(cnt[:], 17), in1=rowc[:, 2, 0:17].unsqueeze(1).to_broadcast([128, NE, 17]), op=ALU.is_gt), reads=[Bsch], writes=[Bsch])
            op("dve", lambda e: e.tensor_reduce(out=nblk[:], in_=cmp17[:], axis=AX.X, op=ALU.add), reads=[Bsch], writes=[Bsch])
            op("dve", lambda e: e.memset(ones32[:], 1.0), writes=[Bsch])
            op("dve", lambda e: e.tensor_tensor_scan(out=cum[:], data0=ones32[:], data1=nblk[:], initial=0.0, op0=ALU.mult, op1=ALU.add), reads=[Bsch], writes=[Bsch])
            op("dve", lambda e: e.tensor_tensor(out=cumex[:], in0=cum[:], in1=nblk[:], op=ALU.subtract), reads=[Bsch], writes=[Bsch])
            op("dve", lambda e: e.tensor_tensor(out=cmpb[:], in0=cum[:].unsqueeze(1).to_broadcast([128, NSB, NE]), in1=bc_mid(bvals, NE), op=ALU.is_le), reads=[Bsch], writes=[Bsch])
            op("dve", lambda e: e.tensor_reduce(out=ebt[:, 0, :], in_=cmpb[:], axis=AX.X, op=ALU.add), reads=[Bsch], writes=[Bsch])
            op("dve", lambda e: e.tensor_scalar(ebt[:, 0, :], ebt[:, 0, :], float(NE - 1), None, op0=ALU.min), reads=[Bsch], writes=[Bsch])
            op("dve", lambda e: e.tensor_scalar(ebt[:, 3, :], ebt[:, 0, :], 128.0, pcol[:, 2:3], op0=ALU.mult, op1=ALU.add), reads=[Bsch], writes=[Bsch])
            if l > 0:
                op("dve", lambda e: e.tensor_scalar(ebt[:, 3, :], ebt[:, 3, :], float(l * NE * 128), None, op0=ALU.add), reads=[Bsch], writes=[Bsch])
            op("dve", lambda e: e.tensor_copy(out=idxB[:], in_=ebt[:, 3, :]), reads=[Bsch], writes=[Bsch])
            for q in range(4):
                op("dve", lambda e: e.tensor_scalar(idf[:, 0, :, q], ebt[:, 3, :], 4.0, float(q), op0=ALU.mult, op1=ALU.add), reads=[Bsch], writes=[Bsch])
            op("dve", lambda e: e.tensor_copy(out=idxW[:], in_=idf[:, 0]), reads=[Bsch], writes=[Bsch])
            op("dve", lambda e: e.tensor_scalar(cumex[:], cumex[:], 512.0, None, op0=ALU.mult), reads=[Bsch], writes=[Bsch])
            NA = NTOKC * 4
            ekflat = ekf[:].rearrange("p c k -> p (c k)")
            op("dve", lambda e: e.tensor_tensor(out=cmpd[:], in0=rowc[:, 1, 0:NE].unsqueeze(1).to_broadcast([128, NA, NE]), in1=bc_mid(ekflat, NE), op=ALU.is_equal), reads=[Bsch, Brt_], writes=[Bsch])
            op("dve", lambda e: e.tensor_tensor(out=cmpd[:], in0=cmpd[:], in1=cumex[:].unsqueeze(1).to_broadcast([128, NA, NE]), op=ALU.mult), reads=[Bsch], writes=[Bsch])
            op("dve", lambda e: e.tensor_reduce(out=destf[:], in_=cmpd[:], axis=AX.X, op=ALU.add), reads=[Bsch], writes=[Bsch])
            op("dve", lambda e: e.tensor_tensor(out=destf[:], in0=destf[:], in1=poskf[:].rearrange("p c k -> p (c k)"), op=ALU.add), reads=[Bsch, Brt_], writes=[Bsch])
            op("dve", lambda e: e.tensor_copy(out=destk[:].rearrange("p c k -> p (c k)"), in_=destf[:]), reads=[Bsch], writes=[Brt_])
            if DEBUG:
                dma("sp", lambda e: e.dma_start(out=dbg_sched[:, 0:NE], in_=cnt[:]), reads=[Bsch])
                dma("sp", lambda e: e.dma_start(out=dbg_sched[:, NE:NE + NSB], in_=ebt[:, 0, :]), reads=[Bsch])
                dma("sp", lambda e: e.dma_start(out=dbg_sched[:, NE + NSB:NE + NSB + 64], in_=destf[:, 0:64]), reads=[Bsch])
            fw.barrier()
        with ExitStack() as dp:
            hdr = Ring(nc, dp, "h2ld", 3, [128, D], BF16)
            for (s_, c_) in lchunks:
                ci = s_ * NCS + c_
                hd, hdb = hdr.next()
                dma("sp", lambda e: e.dma_start(out=hd[:], in_=h2d_d[ci * 128:(ci + 1) * 128, :]), writes=[hdb])
                for k4 in range(4):
                    dma("pool", lambda e: e.indirect_dma_start(out=hbuf_d[:, :], out_offset=bass.IndirectOffsetOnAxis(ap=destk[:, ci, k4:k4 + 1], axis=0),
                                                               in_=hd[:, :], in_offset=None), reads=[hdb, Brt_])
            fw.barrier()
        IOA = bass.IndirectOffsetOnAxis
        with ExitStack() as mo:
            wr = Ring(nc, mo, "wexp", 2, [128, 3, 4, 2048], BF16)
            b1r = Ring(nc, mo, "b1t", 2, [128, 24], F32)
            hrr = Ring(nc, mo, "hrows", 2, [128, 4, D], BF16)
            hTr = Ring(nc, mo, "hTm", 2, [128, 8, 512], BF16)
            atr = Ring(nc, mo, "actT", 2, [128, 8, 512], BF16)
            xgr = Ring(nc, mo, "xg", 2, [128, 4, 512], F32)
            osr = Ring(nc, mo, "ostage", 2, [128, D], F32)
            wsrc = (w1g_d, w1l_d, w2_d)
            for b in range(NSB):
                W, Wb = wr.next()
                for m in range(3):
                    for q in range(4):
                        dma("pool", lambda e: e.indirect_dma_start(out=W[:, m, q, :], out_offset=None, in_=wsrc[m].rearrange("l r f -> (l r) f"), in_offset=IOA(ap=idxW[:, b, q:q + 1], axis=0)),
                            reads=[Bsch], writes=[Wb])
                b1t, b1b = b1r.next()
                dma("pool", lambda e: e.indirect_dma_start(out=b1t[:, 0:16], out_offset=None, in_=b1T_d.rearrange("l r f -> (l r) f"), in_offset=IOA(ap=idxB[:, b:b + 1], axis=0)), reads=[Bsch], writes=[b1b])
                op("dve", lambda e: e.tensor_scalar(b1t[:, 16:24], b1t[:, 8:16], 1.0, None, op0=ALU.add), reads=[b1b], writes=[b1b])
                hr, hrb = hrr.next()
                dma("sp", lambda e: e.dma_start(out=hr[:], in_=hbuf_d[b * 512:(b + 1) * 512, :].rearrange("(g p) d -> p g d", p=128)), writes=[hrb])
                hT, hTb = hTr.next()
                for g in range(4):
                    pt = psb16(g % 2)
                    for k in range(8):
                        op("pe", lambda e: e.transpose(out=pt[:, k * 128:(k + 1) * 128], in_=hr[:, g, k * 128:(k + 1) * 128], identity=identb[:]), reads=[hrb, B_const], writes=[PB[g % 2]])
                    op("act" if g % 2 == 0 else "dve",
                       (lambda e: e.activation(out=hT[:, :, g * 128:(g + 1) * 128], in_=pt.rearrange("p (k t) -> p k t", t=128), func=AF.Copy)) if g % 2 == 0 else
                       (lambda e: e.tensor_copy(out=hT[:, :, g * 128:(g + 1) * 128], in_=pt.rearrange("p (k t) -> p k t", t=128))),
                       reads=[PB[g % 2]], writes=[hTb])
                aT, aTb = atr.next()
                Wv = W[:].rearrange("p m q (k f) -> p m (q k) f", f=1024)
                for fc in range(8):
                    bg, bl = 2 + (fc % 2) * 2, 3 + (fc % 2) * 2
                    for k in range(8):
                        op("pe", lambda e: e.matmul(ps[:, bg, :], lhsT=Wv[:, 0, k, fc * 128:(fc + 1) * 128], rhs=hT[:, k, :], start=(k == 0), stop=(k == 7)), reads=[Wb, hTb], writes=[PB[bg]])
                    for k in range(8):
                        op("pe", lambda e: e.matmul(ps[:, bl, :], lhsT=Wv[:, 1, k, fc * 128:(fc + 1) * 128], rhs=hT[:, k, :], start=(k == 0), stop=(k == 7)), reads=[Wb, hTb], writes=[PB[bl]])
                    xg, xgb = xgr.next()
                    op("dve", lambda e: e.tensor_scalar(xg[:, 0, :], ps[:, bg, :], b1t[:, fc:fc + 1], 7.0, op0=ALU.add, op1=ALU.min), reads=[PB[bg], b1b], writes=[xgb])
                    op("act", lambda e: e.activation(out=xg[:, 1, :], in_=xg[:, 0, :], func=AF.Sigmoid, scale=1.702), reads=[xgb], writes=[xgb])
                    op("dve", lambda e: e.tensor_scalar(xg[:, 2, :], ps[:, bl, :], b1t[:, 16 + fc:17 + fc], 8.0, op0=ALU.add, op1=ALU.min), reads=[PB[bl], b1b], writes=[xgb])
                    op("dve", lambda e: e.scalar_tensor_tensor(out=xg[:, 3, :], in0=xg[:, 2, :], scalar=-6.0, in1=xg[:, 0, :], op0=ALU.max, op1=ALU.mult), reads=[xgb], writes=[xgb])
                    op("dve", lambda e: e.tensor_tensor(out=aT[:, fc, :], in0=xg[:, 3, :], in1=xg[:, 1, :], op=ALU.mult), reads=[xgb], writes=[aTb])
                for g in range(4):
                    for hf in range(2):
                        for fc in range(8):
                            op("pe", lambda e: e.matmul(ps[:, 6 + hf, :], lhsT=aT[:, fc, g * 128:(g + 1) * 128], rhs=Wv[:, 2, fc, hf * 512:(hf + 1) * 512], start=(fc == 0), stop=(fc == 7)),
                               reads=[aTb, Wb], writes=[PB[6 + hf]])
                    ost, osb = osr.next()
                    op("act", lambda e: e.activation(out=ost[:], in_=ps[:, 6:8, :].rearrange("p a n -> p (a n)"), func=AF.Copy), reads=[PB[6], PB[7]], writes=[osb])
                    dma("sp", lambda e: e.dma_start(out=obuf_d[b * 512 + g * 128:b * 512 + (g + 1) * 128, :], in_=ost[:]), reads=[osb])
            fw.barrier()
        last = (l == DEPTH - 1)
        with ExitStack() as cb:
            b2t = sb("b2t", [NE, D], scope=cb)
            Bc = Buf("cmbconst")
            dma("sp", lambda e: e.dma_start(out=b2t[:], in_=b2_d[l]), writes=[Bc])
            G2 = {}
            for col in list(range(NSEQ)) + ([2] if has_ctx_out else []):
                G2[col] = sb(f"G2bc{col}", [128, D], scope=cb)
                dma("sp", lambda e: e.dma_start(out=G2[col][:], in_=modrow_d[col:col + 1, 3 * D:4 * D].partition_broadcast(128)), writes=[Bc])
            if last:
                fnw = sb("fnw", [128, D], scope=cb)
                dma("sp", lambda e: e.dma_start(out=fnw[:], in_=fnwbc_d), writes=[Bc])
            x1r = Ring(nc, cb, "x1c", 2, [128, D], F32)
            rwr = Ring(nc, cb, "orow", 2, [128, 4, D], F32)
            GTr = Ring(nc, cb, "GT", 2, [NE, 128], F32)
            accr = Ring(nc, cb, "acc", 2, [128, D], F32)
            junk = sb("junkC", [128, D], BF16, scope=cb)
            ssq = sb("ssqC", [128, 4], scope=cb)
            Bj = Buf("junkC")
            for (s, c) in lchunks:
                ci = s * NCS + c
                is_ctx = c < NCC
                col = 2 if is_ctx else s
                x1, x1b = x1r.next()
                dma("sp", lambda e: e.dma_start(out=x1[:], in_=xmid_d[ci * 128:(ci + 1) * 128, :]), writes=[x1b])
                orow, orb = rwr.next()
                for k4 in range(4):
                    dma("pool", lambda e: e.indirect_dma_start(out=orow[:, k4, :], out_offset=None, in_=obuf_d[:, :], in_offset=IOA(ap=destk[:, ci, k4:k4 + 1], axis=0)),
                        reads=[Brt_], writes=[orb])
                op("pe", lambda e: e.transpose(out=ps[0:NE, 0, 0:128], in_=Gd[:, ci, :], identity=identf[:]), reads=[Brt_, B_const], writes=[PB[0]])
                GT, GTb = GTr.next()
                op("act", lambda e: e.activation(out=GT[:], in_=ps[0:NE, 0, 0:128], func=AF.Copy), reads=[PB[0]], writes=[GTb])
                for hf in range(2):
                    op("pe", lambda e: e.matmul(ps[:, 1 + hf, :], lhsT=GT[:], rhs=b2t[:, hf * 512:(hf + 1) * 512], start=True, stop=True), reads=[GTb, Bc], writes=[PB[1 + hf]])
                acc, accb = accr.next()
                op("dve", lambda e: e.scalar_tensor_tensor(out=acc[:], in0=orow[:, 0, :], scalar=gatek[:, ci, 0:1], in1=ps[:, 1:3, :].rearrange("p a n -> p (a n)"), op0=ALU.mult, op1=ALU.add),
                   reads=[orb, Brt_, PB[1], PB[2]], writes=[accb])
                for k4 in range(1, 4):
                    op("dve", lambda e: e.scalar_tensor_tensor(out=acc[:], in0=orow[:, k4, :], scalar=gatek[:, ci, k4:k4 + 1], in1=acc[:], op0=ALU.mult, op1=ALU.add),
                       reads=[orb, Brt_, accb], writes=[accb])
                op("dve", lambda e: e.tensor_tensor(out=acc[:], in0=acc[:], in1=G2[col][:], op=ALU.mult), reads=[accb, Bc], writes=[accb])
                op("dve", lambda e: e.tensor_tensor(out=acc[:], in0=acc[:], in1=x1[:], op=ALU.add), reads=[accb, x1b], writes=[accb])
                if DEBUG:
                    dma("sp", lambda e: e.dma_start(out=dbg_x2[ci * 128:(ci + 1) * 128, :], in_=acc[:]), reads=[accb])
                if last:
                    op("dve", lambda e: e.scalar_tensor_tensor(out=junk[:], in0=acc[:], scalar=1.0, in1=acc[:], op0=ALU.mult, op1=ALU.mult, accum_out=ssq[:, 0:1]), reads=[accb], writes=[Bj])
                    op("act", lambda e: e.activation(out=ssq[:, 1:2], in_=ssq[:, 0:1], func=AF.Sqrt, scale=1.0 / D, bias=epst[:, 0:1]), reads=[Bj], writes=[Bj])
                    op("dve", lambda e: e.reciprocal(out=ssq[:, 2:3], in_=ssq[:, 1:2]), reads=[Bj], writes=[Bj])
                    op("dve", lambda e: e.scalar_tensor_tensor(out=acc[:], in0=acc[:], scalar=ssq[:, 2:3], in1=fnw[:], op0=ALU.mult, op1=ALU.mult), reads=[accb, Bj, Bc], writes=[accb])
                    dma("sp", lambda e: e.dma_start(out=out_d[s, (c - NCC) * 128:(c - NCC + 1) * 128, :], in_=acc[:]), reads=[accb])
                else:
                    dma("sp", lambda e: e.dma_start(out=xnext_d[ci * 128:(ci + 1) * 128, :], in_=acc[:]), reads=[accb])
            fw.barrier()
        lt.close()
        if STOP_AFTER is not None:
            break

    fw.barrier(engines=("sp",))
    es.close()
    return nc


def make_constants(nseq=2):
    bf = ml_dtypes.bfloat16
    k = np.arange(T, dtype=np.float64)
    ang = 2 * np.pi * np.outer(k, k) / T
    C = (np.cos(ang) / 64.0)
    S = (-np.sin(ang) / 64.0)
    def lay(M):
        M = M.reshape(4, 8, 128, 16, 256)
        return M.transpose(3, 0, 2, 1, 4)
    dft = np.stack([lay(C), lay(S)], axis=3)
    dft = np.ascontiguousarray(dft.reshape(16, 4, 128, 2 * 8 * 256)).astype(bf)
    kc = np.arange(TC, dtype=np.float64)
    angc = 2 * np.pi * np.outer(kc, kc) / TC
    Cc = (np.cos(angc) / 16.0).reshape(2, 128, 256).transpose(1, 0, 2)
    Sc = (-np.sin(angc) / 16.0).reshape(2, 128, 256).transpose(1, 0, 2)
    dftc = np.ascontiguousarray(np.stack([Cc, Sc], axis=1).reshape(128, 2 * 2 * 256)).astype(bf)
    m = np.arange(64, dtype=np.float64)
    a64 = 2 * np.pi * np.outer(m, m) / 64
    bd = np.zeros((128, 2, 128), np.float32)
    for g in range(2):
        bd[g * 64:(g + 1) * 64, 0, g * 64:(g + 1) * 64] = np.cos(a64) / 8.0
        bd[g * 64:(g + 1) * 64, 1, g * 64:(g + 1) * 64] = np.sin(a64) / 8.0
    t = np.arange(T)
    row = (t // 64).astype(np.float32)
    colp = (t % 64).astype(np.float32)
    freqs = (np.float32(10000.0) ** (-np.arange(16, dtype=np.float32) / np.float32(16))).astype(np.float32)
    angr = np.concatenate([row[:, None] * freqs, colp[:, None] * freqs], -1).astype(np.float32)
    cs = np.cos(angr).reshape(NCX, 128, 32).transpose(1, 0, 2)
    sn = np.sin(angr).reshape(NCX, 128, 32).transpose(1, 0, 2)
    rope = np.ascontiguousarray(np.stack([cs, sn], axis=1)).astype(np.float32)
    j = np.arange(128)[:, None]
    i = np.arange(128)[None, :]
    tri = np.stack([np.maximum(i - j, 0), np.maximum(j - i, 0), (i >= j) * 0.125, (j >= i) * 0.125], axis=1).astype(np.float32)
    p = np.arange(128, dtype=np.float32)
    pcol = np.stack([127 - p, p + 1, p, 128 - p], axis=1).astype(np.float32)
    capmax = 2048 * nseq + 512
    rowc = np.zeros((128, 4, 128), np.float32)
    rowc[:, 0, :32] = np.arange(32) * capmax
    rowc[:, 1, :32] = np.arange(32)
    rowc[:, 2, :17] = np.arange(17) * 512
    rowc[:, 3, :] = np.arange(128)
    ustrict = (np.arange(128)[:, None] < np.arange(128)[None, :]).astype(np.float32).astype(bf)
    return dict(rowc=rowc, ustrict=ustrict, dft=dft, dftc=dftc, bd64=bd, rope=rope, tri=np.ascontiguousarray(tri), pcol=pcol,
                identb=np.eye(128, dtype=np.float32).astype(bf), identf=np.eye(128, dtype=np.float32))


def prep_shared(inp, nseq=2):
    f = lambda a: np.ascontiguousarray(np.asarray(a, dtype=np.float32))
    sh = {}
    sh["mod_w"] = f(inp["mod_w"])
    mod_b = f(inp["mod_b"])
    sh["mod_b"] = mod_b
    sh["mod_bT"] = np.ascontiguousarray(mod_b.reshape(DEPTH, 48, 128).transpose(2, 0, 1))
    nw = f(inp["norm_w"])
    sh["nwT"] = np.ascontiguousarray(nw.reshape(DEPTH, 2, 8, 128).transpose(3, 0, 1, 2))
    sh["nw1_bc"] = np.ascontiguousarray(np.broadcast_to(nw[:, 1, :][None], (128, DEPTH, D)))
    w_in = f(inp["w_in"])
    sh["wfT"] = np.ascontiguousarray(w_in[:, :, :512].transpose(0, 2, 1))
    sh["w_qkvg"] = np.ascontiguousarray(w_in[:, :, 512:])
    sh["w_out"] = f(inp["w_out"])
    sh["rd_bc"] = np.ascontiguousarray(np.broadcast_to(f(inp["ret_decay"]).reshape(1, DEPTH, 16), (128, DEPTH, 16)))
    sh["rnw_bc"] = np.ascontiguousarray(np.broadcast_to(f(inp["ret_norm_w"])[None], (128, DEPTH, 512)))
    sh["router_w"] = f(inp["router_w"])
    sh["rb_bc"] = np.ascontiguousarray(np.broadcast_to(f(inp["router_b"])[None], (128, DEPTH, NE)))
    sh["fnw_bc"] = np.ascontiguousarray(np.broadcast_to(f(inp["final_norm_w"])[None], (128, D)))
    w1 = np.asarray(inp["expert_w1"], dtype=np.float32)
    def wlay(w):
        L, E = w.shape[0], w.shape[1]
        return np.ascontiguousarray(w.reshape(L, E, 8, 128, 1024).transpose(0, 1, 3, 2, 4)).reshape(L, E * 128 * 4, 2048)
    sh["w1g"] = wlay(w1[..., 0::2])
    sh["w1l"] = wlay(w1[..., 1::2])
    sh["w2h"] = wlay(np.asarray(inp["expert_w2"], dtype=np.float32))
    b1 = np.asarray(inp["expert_b1"], dtype=np.float32)
    b1g = b1[..., 0::2].reshape(DEPTH, NE, 8, 128).transpose(0, 1, 3, 2)
    b1l = b1[..., 1::2].reshape(DEPTH, NE, 8, 128).transpose(0, 1, 3, 2)
    sh["b1T"] = np.ascontiguousarray(np.concatenate([b1g, b1l], axis=-1)).reshape(DEPTH, NE * 128, 16)
    sh["b2"] = f(inp["expert_b2"])
    sh.update(make_constants(nseq))
    return sh


def prep_core(inp, sh, core, nseq=2):
    f = lambda a: np.ascontiguousarray(np.asarray(a, dtype=np.float32))
    b0 = core * 2
    m = dict(sh)
    m["x"] = f(inp["x"][b0:b0 + nseq])
    m["ctx"] = f(inp["ctx"][b0:b0 + nseq])
    c = np.asarray(inp["c"], dtype=np.float32)
    cols = [c[b0], c[b0 + 1], np.asarray(inp["c_ctx"], dtype=np.float32)]
    m["cT"] = np.ascontiguousarray(np.stack(cols, axis=1).reshape(8, 128, 3).transpose(1, 0, 2))
    return m


_NC_CACHE = {}


def kernel(**inputs):
    cfg = dict(nseq=2, layers=DEPTH)
    key = "full"
    if key not in _NC_CACHE:
        _NC_CACHE[key] = build_program(cfg)
    nc = _NC_CACHE[key]
    sh = prep_shared(inputs)
    in_maps = [prep_core(inputs, sh, core) for core in range(8)]
    res = run_bass_kernel_spmd(nc, in_maps, core_ids=list(range(8)))
    out = np.concatenate([np.asarray(r["out"], dtype=np.float32) for r in res.results], axis=0)
    return out
```

```python
import numpy as np
import ml_dtypes
from contextlib import ExitStack
import concourse.bass as bass
import concourse.mybir as mybir
from concourse.bass_utils import run_bass_kernel_spmd

F32 = mybir.dt.float32
BF16 = mybir.dt.bfloat16
U32 = mybir.dt.uint32
I32 = mybir.dt.int32
ALU = mybir.AluOpType
AF = mybir.ActivationFunctionType
AX = mybir.AxisListType

D = 1024
T = 4096
TC = 256
NCX = T // 128
NCC = TC // 128
NCS = NCX + NCC
NE = 32
DEPTH = 2
EPS = 1e-6


class Tok:
    __slots__ = ("sem", "val", "key", "owner")

    def __init__(self, sem, val, key, owner):
        self.sem, self.val, self.key, self.owner = sem, val, key, owner


class Buf:
    __slots__ = ("name", "w", "r")

    def __init__(self, name=""):
        self.name = name
        self.w = {}
        self.r = {}


class EngS:
    def __init__(self, name, e, sem):
        self.name, self.e, self.sem = name, e, sem
        self.count = 0
        self.waited = {}


class FW:
    def __init__(self, nc, es, n_dma_sems=12):
        self.nc = nc
        self.engs = {}
        for name, e in (("pe", nc.tensor), ("act", nc.scalar), ("dve", nc.vector),
                        ("pool", nc.gpsimd), ("sp", nc.sync)):
            sem = es.enter_context(nc.semaphore("sem_" + name))
            self.engs[name] = EngS(name, e, sem)
        self.dsems = {}
        for q in ("sp", "pool", "act", "pe"):
            lst = []
            for i in range(n_dma_sems):
                s = es.enter_context(nc.semaphore(f"dsem_{q}{i}"))
                lst.append([s, 0, None])
            self.dsems[q] = [lst, 0]

    def wait(self, E, tok):
        if tok is None:
            return
        if E.waited.get(tok.key, 0) >= tok.val:
            return
        E.e.wait_ge(tok.sem, tok.val)
        E.waited[tok.key] = tok.val

    def _deps(self, reads, writes):
        deps = []
        for b in reads:
            deps.extend(b.w.values())
        for b in writes:
            deps.extend(b.w.values())
            deps.extend(b.r.values())
        return deps

    def op(self, en, fn, reads=(), writes=()):
        E = self.engs[en]
        for tok in self._deps(reads, writes):
            if en == "pe" and tok.owner == "pe":
                continue
            self.wait(E, tok)
        ins = fn(E.e)
        E.count += 1
        ins.then_inc(E.sem, 1)
        tok = Tok(E.sem, E.count, "E" + en, en)
        E.waited[tok.key] = max(E.waited.get(tok.key, 0), 0)
        for b in reads:
            b.r[tok.key] = tok
        for b in writes:
            b.w = {tok.key: tok}
            b.r = {}
        return tok

    def dma(self, q, fn, reads=(), writes=()):
        E = self.engs[q]
        for tok in self._deps(reads, writes):
            self.wait(E, tok)
        lst, idx = self.dsems[q]
        ent = lst[idx % len(lst)]
        self.dsems[q][1] = idx + 1
        if ent[2] is not None:
            self.wait(E, ent[2])
        ins = fn(E.e)
        ent[1] += 16
        ins.then_inc(ent[0], 16)
        tok = Tok(ent[0], ent[1], f"D{q}{idx % len(lst)}", "dma")
        ent[2] = tok
        for b in reads:
            b.r[tok.key] = tok
        for b in writes:
            if all(t.owner == "dma" for t in b.w.values()):
                b.w[tok.key] = tok
            else:
                b.w = {tok.key: tok}
            b.r = {}
        return tok

    def all_toks(self):
        toks = []
        for E in self.engs.values():
            if E.count > 0:
                toks.append(Tok(E.sem, E.count, "E" + E.name, E.name))
        for q, (lst, _) in self.dsems.items():
            for ent in lst:
                if ent[2] is not None:
                    toks.append(ent[2])
        return toks

    def barrier(self, engines=("pe", "act", "dve", "pool", "sp")):
        toks = self.all_toks()
        for en in engines:
            E = self.engs[en]
            for t in toks:
                if t.owner == en:
                    continue
                self.wait(E, t)


_UID = [0]


class Ring:
    def __init__(self, nc, es, name, n, shape, dtype):
        self.tiles = []
        for i in range(n):
            _UID[0] += 1
            self.tiles.append(es.enter_context(nc.sbuf_tensor(f"{name}{i}_u{_UID[0]}", shape, dtype)))
        self.bufs = [Buf(f"{name}{i}") for i in range(n)]
        self.i = 0

    def next(self):
        k = self.i % len(self.tiles)
        self.i += 1
        return self.tiles[k], self.bufs[k]


def bc_mid(ap2, n):
    P, A = ap2.shape
    return ap2.unsqueeze(2).to_broadcast([P, A, n])


def build_program(cfg):
    NSEQ = cfg.get("nseq", 2)
    LAYERS = cfg.get("layers", DEPTH)
    DEBUG = cfg.get("debug", False)
    STOP_AFTER = cfg.get("stop_after", None)
    NTOKC = NSEQ * NCS
    PBS = cfg.get("pb_stop", 99)

    nc = bass.Bass("TRN2", target_bir_lowering=False)
    es = ExitStack()
    fw = FW(nc, es)
    op, dma = fw.op, fw.dma

    def din(name, shape, dt=F32):
        return nc.dram_tensor(name, list(shape), dt, kind="ExternalInput").ap()

    def dscr(name, shape, dt=F32):
        return nc.dram_tensor(name, list(shape), dt, kind="Internal").ap()

    x_in = din("x", [NSEQ, T, D])
    ctx_in = din("ctx", [NSEQ, TC, D])
    cT_d = din("cT", [128, 8, 3])
    modw_d = din("mod_w", [DEPTH, D, 6 * D])
    modbT_d = din("mod_bT", [128, DEPTH, 48])
    modb_d = din("mod_b", [DEPTH, 6 * D])
    nwT_d = din("nwT", [128, DEPTH, 2, 8])
    nw1bc_d = din("nw1_bc", [128, DEPTH, D])
    wfT_d = din("wfT", [DEPTH, 512, D])
    wqkvg_d = din("w_qkvg", [DEPTH, D, 2048])
    wout_d = din("w_out", [DEPTH, D, D])
    rdbc_d = din("rd_bc", [128, DEPTH, 16])
    rnwbc_d = din("rnw_bc", [128, DEPTH, 512])
    rw_d = din("router_w", [DEPTH, D, NE])
    rbbc_d = din("rb_bc", [128, DEPTH, NE])
    fnwbc_d = din("fnw_bc", [128, D])
    dft_d = din("dft", [16, 4, 128, 2 * 8 * 256], BF16)
    dftc_d = din("dftc", [128, 2 * 2 * 256], BF16)
    bd64_d = din("bd64", [128, 2, 128])
    rope_d = din("rope", [128, 2, NCX, 32])
    tri_d = din("tri", [128, 4, 128])
    pcol_d = din("pcol", [128, 4])
    identb_d = din("identb", [128, 128], BF16)
    identf_d = din("identf", [128, 128])

    CAPMAX = 2048 * NSEQ + 512
    CAPB = CAPMAX // 512
    w1g_d = din("w1g", [DEPTH, NE * 128 * 4, 2048])
    w1l_d = din("w1l", [DEPTH, NE * 128 * 4, 2048])
    w2_d = din("w2h", [DEPTH, NE * 128 * 4, 2048])
    b1T_d = din("b1T", [DEPTH, NE * 128, 16])
    b2_d = din("b2", [DEPTH, NE, D])
    ustr_d = din("ustrict", [128, 128], BF16)
    rows_d = din("rowc", [128, 4, 128])
    NSB_MAX = (NTOKC * 128 * 4) // 512 + NE
    hbuf_d = dscr("hbuf", [NSB_MAX * 512, D], BF16)
    obuf_d = dscr("obuf", [NSB_MAX * 512, D])
    h2d_d = dscr("h2d", [NTOKC * 128, D], BF16)
    wbf_d = dscr("wbf", [3 * NE * 128, 8192], BF16)
    Bwbf = Buf("wbf")
    wsrc_all = (w1g_d, w1l_d, w2_d)
    CONV_R = 512

    def conv_pieces(l):
        dst = wbf_d.rearrange("r (q f) -> (r q) f", f=2048)
        for m in range(3):
            for r0 in range(0, NE * 128 * 4, CONV_R):
                yield (lambda m=m, r0=r0: dma("pool", lambda e: e.dma_start(out=dst[m * NE * 128 * 4 + r0:m * NE * 128 * 4 + r0 + CONV_R, :],
                                                                            in_=wsrc_all[m][l, r0:r0 + CONV_R, :]), writes=[Bwbf]))
    out_d = nc.dram_tensor("out", [NSEQ, T, D], F32, kind="ExternalOutput").ap()
    if DEBUG:
        dbg_x = nc.dram_tensor("dbg_x", [NTOKC * 128, D], F32, kind="ExternalOutput").ap()
        dbg_lg = nc.dram_tensor("dbg_lg", [NTOKC * 128, NE], F32, kind="ExternalOutput").ap()
        dbg_x2 = nc.dram_tensor("dbg_x2", [NTOKC * 128, D], F32, kind="ExternalOutput").ap()
        dbg_sched = nc.dram_tensor("dbg_sched", [128, 512], F32, kind="ExternalOutput").ap()

    spill_d = dscr("spill", [NTOKC, 128, 3072], BF16)
    zs_d = dscr("zs", [NSEQ, 128, NCX, 1024], BF16)
    zc_d = dscr("zc", [NSEQ, 128, NCC, 1024], BF16)
    yts_d = dscr("yts", [NTOKC, 128, 512], BF16)
    xmid_d = dscr("xmid", [NTOKC * 128, D])
    xnext_d = dscr("xnext", [NTOKC * 128, D])
    modrow_d = dscr("modrow", [3, 4 * D])

    def sb(name, shape, dt=F32, scope=es):
        _UID[0] += 1
        return scope.enter_context(nc.sbuf_tensor(f"{name}_u{_UID[0]}", list(shape), dt))

    identb = sb("identb", [128, 128], BF16)
    identf = sb("identf", [128, 128])
    tri = sb("tri", [128, 4, 128])
    pcol = sb("pcol", [128, 4])
    bd64 = sb("bd64", [128, 2, 128])
    epst = sb("epst", [128, 1])
    cT = sb("cT", [128, 8, 3])
    B_const = Buf("const")
    dma("sp", lambda e: e.dma_start(out=identb[:], in_=identb_d), writes=[B_const])
    dma("sp", lambda e: e.dma_start(out=identf[:], in_=identf_d), writes=[B_const])
    dma("sp", lambda e: e.dma_start(out=tri[:], in_=tri_d), writes=[B_const])
    dma("sp", lambda e: e.dma_start(out=pcol[:], in_=pcol_d), writes=[B_const])
    dma("sp", lambda e: e.dma_start(out=bd64[:], in_=bd64_d), writes=[B_const])
    dma("sp", lambda e: e.dma_start(out=cT[:], in_=cT_d), writes=[B_const])
    op("dve", lambda e: e.memset(epst[:], EPS), writes=[B_const])
    ustr = sb("ustr", [128, 128], BF16)
    onesb = sb("onesb", [128, 128], BF16)
    rowc = sb("rowc", [128, 4, 128])
    dma("sp", lambda e: e.dma_start(out=ustr[:], in_=ustr_d), writes=[B_const])
    dma("sp", lambda e: e.dma_start(out=rowc[:], in_=rows_d), writes=[B_const])
    op("dve", lambda e: e.memset(onesb[:], 1.0), writes=[B_const])
    siluT = sb("siluT", [128, 8, 3])
    op("act", lambda e: e.activation(out=siluT[:], in_=cT[:], func=AF.Silu), reads=[B_const], writes=[B_const])

    bc_reg = nc.gpsimd.to_reg(3 * NE * 128 - 1)
    ps = es.enter_context(nc.psum_tensor("ps", [128, 8, 512], F32))
    PB = [Buf(f"psum{i}") for i in range(8)]

    def psb16(bank):
        return ps[:, bank, :].bitcast(BF16)

    def chunk_src(l, s, c):
        if l == 0:
            if c < NCC:
                return ctx_in[s, c * 128:(c + 1) * 128, :]
            return x_in[s, (c - NCC) * 128:(c - NCC + 1) * 128, :]
        ci = s * NCS + c
        return xnext_d[ci * 128:(ci + 1) * 128, :]

    for l in range(LAYERS):
        has_ctx_out = l < DEPTH - 1
        lt = ExitStack()
        Gd = sb("Gd", [128, NTOKC, NE], scope=lt)
        destk = sb("destk", [128, NTOKC, 4], I32, scope=lt)
        gatek = sb("gatek", [128, NTOKC, 4], scope=lt)
        basecap = sb("basecap", [128, NE], scope=lt)
        poskf = sb("poskf", [128, NTOKC, 4], scope=lt)
        ekf = sb("ekf", [128, NTOKC, 4], scope=lt)
        Brt_ = Buf("route")
        op("dve", lambda e: e.memset(basecap[:], 0.0), writes=[Brt_])
        lchunks = [(s_, c_) for s_ in range(NSEQ) for c_ in (range(NCS) if has_ctx_out else range(NCC, NCS))]
        NSB = (len(lchunks) * 128 * 4) // 512 + NE
        conv_it = conv_pieces(l)

        def conv_step(n=1):
            for _ in range(n):
                fn_ = next(conv_it, None)
                if fn_ is not None:
                    fn_()
        ls = ExitStack()
        logg = sb("logg", [128, 16], scope=ls)
        rdt = sb("rdt", [128, 16], scope=ls)
        DT8 = sb("DT8", [128, 8, 128], scope=ls)
        wk = sb("wk", [128, 8, 2], scope=ls)
        wq = sb("wq", [128, 8, 2], scope=ls)
        G8 = sb("G8", [128, 8], scope=ls)
        Gbc = sb("Gbc", [128, 8, 64], scope=ls)
        modF = sb("modF", [128, 16, 3], scope=ls)
        A1 = sb("A1", [128, 8, 3], scope=ls)
        wz = sb("wz", [128, 8, 1024], BF16, scope=ls)
        rnwbc = sb("rnwbc", [128, 512], scope=ls)
        nw1bc = sb("nw1bc", [128, D], scope=ls)
        rbbc = sb("rbbc", [128, NE], scope=ls)
        rw = sb("rw", [128, 8, NE], scope=ls)
        nwT = sb("nwT", [128, 2, 8], scope=ls)
        mbT = sb("mbT", [128, 48], scope=ls)
        BL = Buf("layerconst")
        dma("sp", lambda e: e.dma_start(out=rdt[:], in_=rdbc_d[:, l, :]), writes=[BL])
        dma("sp", lambda e: e.dma_start(out=rnwbc[:], in_=rnwbc_d[:, l, :]), writes=[BL])
        dma("sp", lambda e: e.dma_start(out=nw1bc[:], in_=nw1bc_d[:, l, :]), writes=[BL])
        dma("sp", lambda e: e.dma_start(out=rbbc[:], in_=rbbc_d[:, l, :]), writes=[BL])
        dma("sp", lambda e: e.dma_start(out=rw[:], in_=rw_d[l].rearrange("(k p) n -> p k n", p=128)), writes=[BL])
        dma("sp", lambda e: e.dma_start(out=nwT[:], in_=nwT_d[:, l, :, :]), writes=[BL])
        dma("sp", lambda e: e.dma_start(out=mbT[:], in_=modbT_d[:, l, :]), writes=[BL])
        op("act", lambda e: e.activation(out=logg[:], in_=rdt[:], func=AF.Exp, scale=-float(np.log(2.0))), reads=[BL], writes=[BL])
        op("dve", lambda e: e.tensor_scalar(logg[:], logg[:], -1.0, 1.0, op0=ALU.mult, op1=ALU.add), reads=[BL], writes=[BL])
        op("act", lambda e: e.activation(out=logg[:], in_=logg[:], func=AF.Ln), reads=[BL], writes=[BL])
        with ExitStack() as ss:
            tmpa = sb("tmpa", [128, 128], scope=ss)
            tmpb = sb("tmpb", [128, 128], scope=ss)
            Bt = Buf("tmpab")
            for h in range(8):
                op("act", lambda e: e.activation(out=tmpa[:], in_=tri[:, 0, :], func=AF.Exp, scale=logg[:, h:h + 1]), reads=[BL, B_const], writes=[Bt])
                op("dve", lambda e: e.tensor_tensor(out=tmpa[:], in0=tmpa[:], in1=tri[:, 2, :], op=ALU.mult), reads=[Bt], writes=[Bt])
                op("act", lambda e: e.activation(out=tmpb[:], in_=tri[:, 1, :], func=AF.Exp, scale=logg[:, 8 + h:9 + h]), reads=[BL, Bt], writes=[Bt])
                op("dve", lambda e: e.tensor_tensor(out=tmpb[:], in0=tmpb[:], in1=tri[:, 3, :], op=ALU.mult), reads=[Bt], writes=[Bt])
                op("dve", lambda e: e.tensor_tensor(out=DT8[:, h, :], in0=tmpa[:], in1=tmpb[:], op=ALU.add), reads=[Bt], writes=[BL])
            op("act", lambda e: e.activation(out=wk[:, :, 0], in_=logg[:, 0:8], func=AF.Exp, scale=pcol[:, 0:1]), reads=[BL], writes=[BL])
            op("act", lambda e: e.activation(out=wk[:, :, 1], in_=logg[:, 8:16], func=AF.Exp, scale=pcol[:, 2:3]), reads=[BL], writes=[BL])
            op("act", lambda e: e.activation(out=wq[:, :, 0], in_=logg[:, 0:8], func=AF.Exp, scale=pcol[:, 1:2]), reads=[BL], writes=[BL])
            op("act", lambda e: e.activation(out=wq[:, :, 1], in_=logg[:, 8:16], func=AF.Exp, scale=pcol[:, 3:4]), reads=[BL], writes=[BL])
            op("dve", lambda e: e.tensor_scalar(wq[:], wq[:], 0.125, None, op0=ALU.mult), reads=[BL], writes=[BL])
            op("act", lambda e: e.activation(out=G8[0:64, :], in_=logg[0:64, 0:8], func=AF.Exp, scale=128.0), reads=[BL], writes=[BL])
            op("act", lambda e: e.activation(out=G8[64:128, :], in_=logg[64:128, 8:16], func=AF.Exp, scale=128.0), reads=[BL], writes=[BL])
            op("dve", lambda e: e.tensor_copy(out=Gbc[:], in_=bc_mid(G8[:], 64)), reads=[BL], writes=[BL])
            fw.barrier()
        with ExitStack() as ss:
            mwr = Ring(nc, ss, "mw", 2, [128, 8, 512], F32)
            mbrow = sb("mbrow", [3, 4 * D], scope=ss)
            mrow = sb("mrow", [3, 4 * D], scope=ss)
            Bmb = Buf("mbrow")
            for r in range(3):
                dma("sp", lambda e: e.dma_start(out=mbrow[r:r + 1, :], in_=modb_d[l:l + 1, 2 * D:6 * D]), writes=[Bmb])
            psM = ps[:, 0, 0:64].rearrange("p (a b) -> p a b", b=4)
            for cb in range(12):
                m, half = cb // 2, cb % 2
                mw, mwb = mwr.next()
                dma("sp", lambda e: e.dma_start(out=mw[:], in_=modw_d[l, :, cb * 512:(cb + 1) * 512].rearrange("(k p) n -> p k n", p=128)), writes=[mwb])
                if m in (0, 1):
                    for jj in range(4):
                        idx = m * 8 + half * 4 + jj
                        for k in range(8):
                            op("pe", lambda e: e.matmul(psM[:, idx, 0:3], lhsT=mw[:, k, jj * 128:(jj + 1) * 128], rhs=siluT[:, k, :],
                                                        start=(k == 0), stop=(k == 7)), reads=[mwb, B_const], writes=[PB[0]])
                else:
                    for k in range(8):
                        op("pe", lambda e: e.matmul(ps[0:3, 1, :], lhsT=siluT[:, k, :], rhs=mw[:, k, :],
                                                    start=(k == 0), stop=(k == 7)), reads=[mwb, B_const], writes=[PB[1]])
                    c0 = (m - 2) * D + half * 512
                    op("dve", lambda e: e.tensor_tensor(out=mrow[0:3, c0:c0 + 512], in0=ps[0:3, 1, :], in1=mbrow[0:3, c0:c0 + 512], op=ALU.add),
                       reads=[PB[1], Bmb], writes=[Bmb])
            op("dve", lambda e: e.tensor_tensor(out=modF[:], in0=psM[:, :, 0:3], in1=bc_mid(mbT[:, 0:16], 3), op=ALU.add), reads=[PB[0], BL], writes=[BL])
            op("dve", lambda e: e.scalar_tensor_tensor(out=A1[:], in0=modF[:, 8:16, :], scalar=1.0, in1=bc_mid(nwT[:, 0, :], 3), op0=ALU.add, op1=ALU.mult),
               reads=[BL], writes=[BL])
            dma("sp", lambda e: e.dma_start(out=modrow_d[:, :], in_=mrow[0:3, :]), reads=[Bmb], writes=[BL])
            fw.barrier()
        with ExitStack() as ss:
            wft = sb("wft", [128, 4, D], scope=ss)
            Bw = Buf("wft")
            dma("sp", lambda e: e.dma_start(out=wft[:], in_=wfT_d[l].rearrange("(g p) d -> p g d", p=128)), writes=[Bw])
            for dk in range(8):
                for cs in range(2):
                    for gc in range(4):
                        op("pe", lambda e: e.matmul(ps[:, 2 + cs, gc * 128:(gc + 1) * 128], lhsT=wft[:, gc, dk * 128:(dk + 1) * 128], rhs=bd64[:, cs, :],
                                                    start=True, stop=True), reads=[Bw, B_const], writes=[PB[2 + cs]])
                    op("act", lambda e: e.activation(out=wz[:, dk, cs * 512:(cs + 1) * 512], in_=ps[:, 2 + cs, :], func=AF.Copy), reads=[PB[2 + cs]], writes=[BL])
            fw.barrier()

        if STOP_AFTER == "setup":
            ls.close()
            lt.close()
            break
        for s in range(NSEQ):
            sq = ExitStack()
            kvs = sb("kvs", [128, NCS, 512], BF16, scope=sq)
            Bkv = Buf("kvs")
            with ExitStack() as pa:
                wqk = sb("wqk", [128, 8, 2048], BF16, scope=pa)
                Bwqk = Buf("wqk")
                for k in range(8):
                    dma("pool", lambda e: e.dma_start(out=wqk[:, k, :], in_=wqkvg_d[l, k * 128:(k + 1) * 128, :]), writes=[Bwqk])
                ropet = sb("ropet", [128, 2, NCX, 32], scope=pa)
                dma("sp", lambda e: e.dma_start(out=ropet[:], in_=rope_d), writes=[Bwqk])
                xr = Ring(nc, pa, "xa", 2, [128, D], F32)
                junk = sb("junkA", [128, D], BF16, scope=pa)
                Bj = Buf("junkA")
                ssq = sb("ssqA", [128, 4], scope=pa)
                xnr = Ring(nc, pa, "xn", 2, [128, D], BF16)
                hTr = Ring(nc, pa, "hT", 2, [128, 8, 128], BF16)
                qkr = Ring(nc, pa, "qkrot", 2, [128, 16, 64], BF16)
                rtmp = sb("rtmp", [128, 4, 16, 32], scope=pa)
                Brt = Buf("rtmp")
                kwr = Ring(nc, pa, "kw", 2, [128, 8, 2, 64], BF16)
                qcr = Ring(nc, pa, "qc", 2, [128, 8, 2, 64], BF16)
                sgt = sb("sgt", [128, 512], scope=pa)
                Bsg = Buf("sgt")
                recr = Ring(nc, pa, "recA", 2, [128, 3072], BF16)
                zrr = Ring(nc, pa, "zrec", 2, [128, 1024], BF16)
                def mkA(c):
                    st = {}
                    def f0():
                        ci = s * NCS + c
                        is_ctx = c < NCC
                        col = 2 if is_ctx else s
                        xt, xb = xr.next()
                        dma("sp", lambda e: e.dma_start(out=xt[:], in_=chunk_src(l, s, c)), writes=[xb])
                        conv_step(1)
                        op("dve", lambda e: e.scalar_tensor_tensor(out=junk[:], in0=xt[:], scalar=1.0, in1=xt[:], op0=ALU.mult, op1=ALU.mult, accum_out=ssq[:, 0:1]),
                           reads=[xb], writes=[Bj])
                        op("act", lambda e: e.activation(out=ssq[:, 1:2], in_=ssq[:, 0:1], func=AF.Ln, scale=1.0 / D, bias=epst[:, 0:1]), reads=[Bj], writes=[Bj])
                        op("act", lambda e: e.activation(out=ssq[:, 2:3], in_=ssq[:, 1:2], func=AF.Exp, scale=-0.5), reads=[Bj], writes=[Bj])
                        xn, xnb = xnr.next()
                        op("dve", lambda e: e.tensor_scalar(xn[:], xt[:], ssq[:, 2:3], None, op0=ALU.mult), reads=[xb, Bj], writes=[xnb])
                        st.update(xn=xn, xnb=xnb)
                    def f1():
                        ci = s * NCS + c
                        is_ctx = c < NCC
                        col = 2 if is_ctx else s
                        xn, xnb = st["xn"], st["xnb"]
                        pT = psb16(0)
                        for k in range(8):
                            op("pe", lambda e: e.transpose(out=pT[:, k * 128:(k + 1) * 128], in_=xn[:, k * 128:(k + 1) * 128], identity=identb[:]),
                               reads=[xnb, B_const], writes=[PB[0]])
                        hT, hTb = hTr.next()
                        for k in range(8):
                            op("act", lambda e: e.activation(out=hT[:, k, :], in_=pT[:, k * 128:(k + 1) * 128], func=AF.Identity,
                                                             scale=A1[:, k, col:col + 1], bias=modF[:, k, col:col + 1]), reads=[PB[0], BL], writes=[hTb])
                        for grp in range(6):
                            for k in range(8):
                                rhs = wz[:, k, grp * 512:(grp + 1) * 512] if grp < 2 else wqk[:, k, (grp - 2) * 512:(grp - 1) * 512]
                                op("pe", lambda e: e.matmul(ps[:, 1 + grp, :], lhsT=hT[:, k, :], rhs=rhs, start=(k == 0), stop=(k == 7)),
                                   reads=[hTb, BL, Bwqk], writes=[PB[1 + grp]])
                        rec, recb = recr.next()
                        if (not is_ctx) or has_ctx_out:
                            zr, zrb = zrr.next()
                            op("act", lambda e: e.activation(out=zr[:, 0:512], in_=ps[:, 1, :], func=AF.Copy), reads=[PB[1]], writes=[zrb])
                            op("act", lambda e: e.activation(out=zr[:, 512:1024], in_=ps[:, 2, :], func=AF.Copy), reads=[PB[2]], writes=[zrb])
                            if is_ctx:
                                dma("sp", lambda e: e.dma_start(out=zc_d[s, :, c, :], in_=zr[:]), reads=[zrb])
                            else:
                                dma("sp", lambda e: e.dma_start(out=zs_d[s, :, c - NCC, :], in_=zr[:]), reads=[zrb])
                        qk, qkb = qkr.next()
                        psqk = ps[:, 3:5, :].rearrange("p a (h d) -> p (a h) d", d=64)
                        if is_ctx:
                            op("act", lambda e: e.activation(out=qk[:], in_=psqk, func=AF.Copy), reads=[PB[3], PB[4]], writes=[qkb])
                        else:
                            cx = c - NCC
                            cosb = ropet[:, 0, cx, :].unsqueeze(1).to_broadcast([128, 16, 32])
                            sinb = ropet[:, 1, cx, :].unsqueeze(1).to_broadcast([128, 16, 32])
                            t1, t2 = psqk[:, :, 0:32], psqk[:, :, 32:64]
                            op("dve", lambda e: e.tensor_tensor(out=rtmp[:, 0], in0=t1, in1=cosb, op=ALU.mult), reads=[PB[3], PB[4], Bwqk], writes=[Brt])
                            op("dve", lambda e: e.tensor_tensor(out=rtmp[:, 1], in0=t2, in1=sinb, op=ALU.mult), reads=[PB[3], PB[4], Bwqk], writes=[Brt])
                            op("dve", lambda e: e.tensor_tensor(out=rtmp[:, 2], in0=t1, in1=sinb, op=ALU.mult), reads=[PB[3], PB[4], Bwqk], writes=[Brt])
                            op("dve", lambda e: e.tensor_tensor(out=rtmp[:, 3], in0=t2, in1=cosb, op=ALU.mult), reads=[PB[3], PB[4], Bwqk], writes=[Brt])
                            op("dve", lambda e: e.tensor_tensor(out=qk[:, :, 0:32], in0=rtmp[:, 0], in1=rtmp[:, 1], op=ALU.subtract), reads=[Brt], writes=[qkb])
                            op("dve", lambda e: e.tensor_tensor(out=qk[:, :, 32:64], in0=rtmp[:, 2], in1=rtmp[:, 3], op=ALU.add), reads=[Brt], writes=[qkb])
                        kw, kwb = kwr.next()
                        qc, qcb = qcr.next()
                        op("dve", lambda e: e.tensor_tensor(out=kw[:], in0=qk[:, 8:16, :].unsqueeze(2).to_broadcast([128, 8, 2, 64]),
                                                            in1=wk[:].unsqueeze(3).to_broadcast([128, 8, 2, 64]), op=ALU.mult), reads=[qkb, BL], writes=[kwb])
                        op("dve", lambda e: e.tensor_tensor(out=qc[:], in0=qk[:, 0:8, :].unsqueeze(2).to_broadcast([128, 8, 2, 64]),
                                                            in1=wq[:].unsqueeze(3).to_broadcast([128, 8, 2, 64]), op=ALU.mult), reads=[qkb, BL], writes=[qcb])
                        op("act", lambda e: e.activation(out=rec[:, 2048:2560], in_=ps[:, 5, :], func=AF.Copy), reads=[PB[5]], writes=[recb])
                        op("act", lambda e: e.activation(out=sgt[:], in_=ps[:, 6, :], func=AF.Silu), reads=[PB[6]], writes=[Bsg])
                        op("dve", lambda e: e.tensor_tensor(out=rec[:, 2560:3072], in0=sgt[:], in1=rnwbc[:], op=ALU.mult), reads=[Bsg, BL], writes=[recb])
                        for h in range(8):
                            op("pe", lambda e: e.matmul(ps[:, 0, h * 64:(h + 1) * 64], lhsT=kw[:, h].rearrange("p a d -> p (a d)"), rhs=rec[:, 2048 + h * 64:2048 + (h + 1) * 64],
                                                        start=True, stop=True), reads=[kwb, recb], writes=[PB[0]])
                        op("act", lambda e: e.activation(out=kvs[:, c, :], in_=ps[:, 0, :], func=AF.Copy), reads=[PB[0]], writes=[Bkv])
                        p7 = psb16(7)
                        for j in range(8):
                            op("pe", lambda e: e.transpose(out=p7[:, j * 128:(j + 1) * 128], in_=qk[:, 2 * j:2 * j + 2, :].rearrange("p a d -> p (a d)"), identity=identb[:]),
                               reads=[qkb, B_const], writes=[PB[7]])
                        op("act", lambda e: e.activation(out=rec[:, 0:1024], in_=p7[:, :], func=AF.Copy), reads=[PB[7]], writes=[recb])
                        for h in range(8):
                            op("pe", lambda e: e.transpose(out=p7[:, h * 128:(h + 1) * 128], in_=qc[:, h].rearrange("p a d -> p (a d)"), identity=identb[:]),
                               reads=[qcb, B_const], writes=[PB[7]])
                        op("act", lambda e: e.activation(out=rec[:, 1024:2048], in_=p7[:, :], func=AF.Copy), reads=[PB[7]], writes=[recb])
                        dma("sp", lambda e: e.dma_start(out=spill_d[ci], in_=rec[:]), reads=[recb])
                    return f0, f1
                fA = [mkA(c) for c in range(NCS)]
                for i in range(NCS + 1):
                    if i < NCS:
                        fA[i][0]()
                    if i >= 1:
                        fA[i - 1][1]()
                fw.barrier()
            if STOP_AFTER == "passA":
                sq.close()
                break
            with ExitStack() as fo:
                Zt = sb("Zt", [128, NCX, 1024], BF16, scope=fo)
                Bz = Buf("Zt")
                for q4 in range(4):
                    dma("sp", lambda e: e.dma_start(out=Zt[:, q4 * 8:(q4 + 1) * 8, :], in_=zs_d[s, :, q4 * 8:(q4 + 1) * 8, :]), writes=[Bz])
                St = sb("St", [128, 512], scope=fo)
                tmpS = sb("tmpS", [128, 512], scope=fo)
                Bs = Buf("scan")
                op("dve", lambda e: e.memset(St[:], 0.0), writes=[Bs])
                Gf = Gbc[:].rearrange("p h d -> p (h d)")
                order_f = list(range(NCS))
                order_b = [1, 0] + list(range(NCS - 1, NCC - 1, -1))
                for (lo, hi, order) in ((0, 64, order_f), (64, 128, order_b)):
                    for c in order:
                        op("dve", lambda e: e.tensor_copy(out=tmpS[lo:hi, :], in_=kvs[lo:hi, c, :]), reads=[Bkv, Bs], writes=[Bs])
                        op("dve", lambda e: e.tensor_copy(out=kvs[lo:hi, c, :], in_=St[lo:hi, :]), reads=[Bs], writes=[Bkv])
                        op("dve", lambda e: e.tensor_tensor(out=St[lo:hi, :], in0=St[lo:hi, :], in1=Gf[lo:hi, :], op=ALU.mult), reads=[Bs, BL], writes=[Bs])
                        op("dve", lambda e: e.tensor_tensor(out=St[lo:hi, :], in0=St[lo:hi, :], in1=tmpS[lo:hi, :], op=ALU.add), reads=[Bs], writes=[Bs])
                tabr = Ring(nc, fo, "dtab", 4, [128, 2, 8, 256], BF16)
                ystr = Ring(nc, fo, "yst", 2, [128, 2, 4, 128], BF16)
                for kb in range(16):
                    bank0 = 4 * (kb % 2)
                    for tq in range(4):
                        tab, tabb = tabr.next()
                        dma("sp", lambda e: e.dma_start(out=tab[:].rearrange("p a b c -> p (a b c)"), in_=dft_d[kb, tq]), writes=[tabb])
                        for n in range(4):
                            for tcc in range(8):
                                tt = tq * 8 + tcc
                                op("pe", lambda e: e.matmul(ps[:, bank0 + n, 0:256], lhsT=Zt[:, tt, n * 128:(n + 1) * 128], rhs=tab[:, 0, tcc, :],
                                                            start=(tq == 0 and tcc == 0), stop=False), reads=[Bz, tabb], writes=[PB[bank0 + n]])
                                op("pe", lambda e: e.matmul(ps[:, bank0 + n, 0:256], lhsT=Zt[:, tt, 512 + n * 128:512 + (n + 1) * 128], rhs=tab[:, 1, tcc, :],
                                                            start=False, stop=(tq == 3 and tcc == 7)), reads=[Bz, tabb], writes=[PB[bank0 + n]])
                    yst, ystb = ystr.next()
                    for n in range(4):
                        op("act", lambda e: e.activation(out=yst[:, :, n, :], in_=ps[:, bank0 + n, 0:256].rearrange("p (a b) -> p a b", b=128), func=AF.Copy),
                           reads=[PB[bank0 + n]], writes=[ystb])
                    ci0 = s * NCS + NCC + 2 * kb
                    dma("sp", lambda e: e.dma_start(out=yts_d[ci0:ci0 + 2].rearrange("c p f -> p c f"), in_=yst[:].rearrange("p a n t -> p a (n t)")), reads=[ystb])
                if has_ctx_out:
                    Zc = sb("Zc", [128, NCC, 1024], BF16, scope=fo)
                    tabc = sb("tabc", [128, 2, 2, 256], BF16, scope=fo)
                    Bzc = Buf("Zc")
                    dma("sp", lambda e: e.dma_start(out=Zc[:], in_=zc_d[s]), writes=[Bzc])
                    dma("sp", lambda e: e.dma_start(out=tabc[:].rearrange("p a b c -> p (a b c)"), in_=dftc_d), writes=[Bzc])
                    yst, ystb = ystr.next()
                    for n in range(4):
                        for tt in range(2):
                            op("pe", lambda e: e.matmul(ps[:, n, 0:256], lhsT=Zc[:, tt, n * 128:(n + 1) * 128], rhs=tabc[:, 0, tt, :], start=(tt == 0), stop=False),
                               reads=[Bzc], writes=[PB[n]])
                            op("pe", lambda e: e.matmul(ps[:, n, 0:256], lhsT=Zc[:, tt, 512 + n * 128:512 + (n + 1) * 128], rhs=tabc[:, 1, tt, :], start=False, stop=(tt == 1)),
                               reads=[Bzc], writes=[PB[n]])
                        op("act", lambda e: e.activation(out=yst[:, :, n, :], in_=ps[:, n, 0:256].rearrange("p (a b) -> p a b", b=128), func=AF.Copy),
                           reads=[PB[n]], writes=[ystb])
                    ci0 = s * NCS
                    dma("sp", lambda e: e.dma_start(out=yts_d[ci0:ci0 + 2].rearrange("c p f -> p c f"), in_=yst[:].rearrange("p a n t -> p a (n t)")), reads=[ystb])
                fw.barrier()
            if STOP_AFTER == "fourier":
                sq.close()
                break
            with ExitStack() as pb:
                wout = sb("wout", [128, 8, D], BF16, scope=pb)
                Bwo = Buf("wout")
                for k in range(8):
                    dma("pool", lambda e: e.dma_start(out=wout[:, k, :], in_=wout_d[l, k * 128:(k + 1) * 128, :]), writes=[Bwo])
                bcs = {}
                Bbc = Buf("bc")
                for (nm, col) in (("x", s), ("c", 2)):
                    if nm == "c" and not has_ctx_out:
                        continue
                    G1 = sb(f"G1bc{nm}", [128, D], scope=pb)
                    A2 = sb(f"A2bc{nm}", [128, D], scope=pb)
                    B2 = sb(f"B2bc{nm}", [128, D], scope=pb)
                    dma("sp", lambda e: e.dma_start(out=G1[:], in_=modrow_d[col:col + 1, 0:D].partition_broadcast(128)), writes=[Bbc])
                    dma("sp", lambda e: e.dma_start(out=B2[:], in_=modrow_d[col:col + 1, D:2 * D].partition_broadcast(128)), writes=[Bbc])
                    dma("sp", lambda e: e.dma_start(out=A2[:], in_=modrow_d[col:col + 1, 2 * D:3 * D].partition_broadcast(128)), writes=[Bbc])
                    op("dve", lambda e: e.scalar_tensor_tensor(out=A2[:], in0=A2[:], scalar=1.0, in1=nw1bc[:], op0=ALU.add, op1=ALU.mult), reads=[Bbc, BL], writes=[Bbc])
                    bcs[nm] = (G1, A2, B2)
                recr = Ring(nc, pb, "recB", 2, [128, 3072], BF16)
                catr = Ring(nc, pb, "catT", 2, [128, 8, 128], BF16)
                xr = Ring(nc, pb, "xb", 2, [128, D], F32)
                PTr = Ring(nc, pb, "PT", 2, [128, 8, 128], BF16)
                gn = sb("gn", [128, 3, 8, 64], scope=pb)
                gs = sb("gs", [128, 4, 8], scope=pb)
                Bgn = Buf("gn")
                yr = Ring(nc, pb, "yb", 2, [128, 512], BF16)
                x1r = Ring(nc, pb, "x1", 2, [128, D], F32)
                junk = sb("junkB", [128, D], BF16, scope=pb)
                ssq = sb("ssqB", [128, 4], scope=pb)
                Bj = Buf("junkB")
                h2r = Ring(nc, pb, "h2", 2, [128, D], F32)
                h2Tr = Ring(nc, pb, "h2T", 2, [128, 8, 128], F32)
                lgr = Ring(nc, pb, "lg", 2, [128, NE], F32)
                rtr = Ring(nc, pb, "rt", 2, [128, 4, NE], F32)
                smr = Ring(nc, pb, "sm", 2, [128, 16], F32)
                mbr = Ring(nc, pb, "mb", 2, [128, NE], BF16)
                h2br = Ring(nc, pb, "h2b", 2, [128, D], BF16)
                chunks = list(range(NCS)) if has_ctx_out else list(range(NCC, NCS))
                def mkB(c):
                    st = {}
                    def fL():
                        ci = s * NCS + c
                        is_ctx = c < NCC
                        G1, A2, B2 = bcs["c" if is_ctx else "x"]
                        conv_step(1)
                        rec, recb = recr.next()
                        dma("sp", lambda e: e.dma_start(out=rec[:], in_=spill_d[ci]), writes=[recb])
                        cat, catb = catr.next()
                        dma("sp", lambda e: e.dma_start(out=cat[:, 0:4, :].rearrange("p a t -> p (a t)"), in_=yts_d[ci]), writes=[catb])
                        xt, xb = xr.next()
                        dma("sp", lambda e: e.dma_start(out=xt[:], in_=chunk_src(l, s, c)), writes=[xb])
                        st.update(rec=rec, recb=recb, cat=cat, catb=catb, xt=xt, xb=xb)
                    def f1():
                        ci = s * NCS + c
                        is_ctx = c < NCC
                        G1, A2, B2 = bcs["c" if is_ctx else "x"]
                        rec, recb, cat, catb, xt, xb = st["rec"], st["recb"], st["cat"], st["catb"], st["xt"], st["xb"]
                        for h in range(8):
                            hp, hh = h // 2, h % 2
                            op("pe", lambda e: e.matmul(ps[:, hh, hp * 128:(hp + 1) * 128],
                                                        lhsT=rec[hh * 64:(hh + 1) * 64, 512 + hp * 128:512 + (hp + 1) * 128],
                                                        rhs=rec[hh * 64:(hh + 1) * 64, hp * 128:(hp + 1) * 128], start=True, stop=True),
                               reads=[recb], writes=[PB[hh]])
                        PT, PTb = PTr.next()
                        yield
                        for g2 in range(2):
                            op("dve", lambda e: e.tensor_tensor(out=PT[:].rearrange("p (a b) t -> p a b t", b=2)[:, :, g2, :], in0=ps[:, g2, :].rearrange("p (h t) -> p h t", t=128),
                                                                in1=DT8[:].rearrange("p (a b) t -> p a b t", b=2)[:, :, g2, :], op=ALU.mult), reads=[PB[g2], BL], writes=[PTb])
                        for h in range(8):
                            op("pe", lambda e: e.matmul(ps[:, 2, h * 64:(h + 1) * 64], lhsT=PT[:, h, :], rhs=rec[:, 2048 + h * 64:2048 + (h + 1) * 64], start=True, stop=False),
                               reads=[PTb, recb], writes=[PB[2]])
                            op("pe", lambda e: e.matmul(ps[:, 2, h * 64:(h + 1) * 64], lhsT=rec[:, 1024 + h * 128:1024 + (h + 1) * 128], rhs=kvs[:, c, h * 64:(h + 1) * 64], start=False, stop=True),
                               reads=[recb, Bkv], writes=[PB[2]])
                        yield
                        po = ps[:, 2, :].rearrange("p (h d) -> p h d", d=64)
                        op("dve", lambda e: e.tensor_reduce(out=gs[:, 0, :], in_=po, axis=AX.X, op=ALU.add), reads=[PB[2]], writes=[Bgn])
                        op("dve", lambda e: e.tensor_scalar(gs[:, 1, :], gs[:, 0, :], -1.0 / 64, None, op0=ALU.mult), reads=[Bgn], writes=[Bgn])
                        op("dve", lambda e: e.tensor_tensor(out=gn[:, 0], in0=po, in1=bc_mid(gs[:, 1, :], 64), op=ALU.add), reads=[PB[2], Bgn], writes=[Bgn])
                        op("dve", lambda e: e.tensor_tensor(out=gn[:, 1], in0=gn[:, 0], in1=gn[:, 0], op=ALU.mult), reads=[Bgn], writes=[Bgn])
                        op("dve", lambda e: e.tensor_reduce(out=gs[:, 2, :], in_=gn[:, 1], axis=AX.X, op=ALU.add), reads=[Bgn], writes=[Bgn])
                        op("act", lambda e: e.activation(out=gs[:, 3, :], in_=gs[:, 2, :], func=AF.Ln, scale=1.0 / 64, bias=epst[:, 0:1]), reads=[Bgn], writes=[Bgn])
                        yield
                        op("act", lambda e: e.activation(out=gs[:, 2, :], in_=gs[:, 3, :], func=AF.Exp, scale=-0.5), reads=[Bgn], writes=[Bgn])
                        op("dve", lambda e: e.tensor_tensor(out=gn[:, 2], in0=gn[:, 0], in1=bc_mid(gs[:, 2, :], 64), op=ALU.mult), reads=[Bgn], writes=[Bgn])
                        yb_, ybb = yr.next()
                        op("dve", lambda e: e.tensor_tensor(out=yb_[:], in0=gn[:, 2].rearrange("p h d -> p (h d)"), in1=rec[:, 2560:3072], op=ALU.mult), reads=[Bgn, recb], writes=[ybb])
                        p3 = psb16(3)
                        for j in range(4):
                            op("pe", lambda e: e.transpose(out=p3[:, j * 128:(j + 1) * 128], in_=yb_[:, j * 128:(j + 1) * 128], identity=identb[:]), reads=[ybb, B_const], writes=[PB[3]])
                        op("act", lambda e: e.activation(out=cat[:, 4:8, :].rearrange("p a t -> p (a t)"), in_=p3[:, 0:512], func=AF.Copy), reads=[PB[3]], writes=[catb])
                        for hf in range(2):
                            for m in range(8):
                                op("pe", lambda e: e.matmul(ps[:, 4 + hf, :], lhsT=cat[:, m, :], rhs=wout[:, m, hf * 512:(hf + 1) * 512], start=(m == 0), stop=(m == 7)),
                                   reads=[catb, Bwo], writes=[PB[4 + hf]])
                        x1, x1b = x1r.next()
                        yield
                        op("dve", lambda e: e.tensor_tensor(out=x1[:], in0=ps[:, 4:6, :].rearrange("p a n -> p (a n)"), in1=G1[:], op=ALU.mult), reads=[PB[4], PB[5], Bbc], writes=[x1b])
                        op("dve", lambda e: e.tensor_tensor(out=x1[:], in0=x1[:], in1=xt[:], op=ALU.add), reads=[x1b, xb], writes=[x1b])
                        dma("sp", lambda e: e.dma_start(out=xmid_d[ci * 128:(ci + 1) * 128, :], in_=x1[:]), reads=[x1b])
                        if DEBUG:
                            dma("sp", lambda e: e.dma_start(out=dbg_x[ci * 128:(ci + 1) * 128, :], in_=x1[:]), reads=[x1b])
                        st.update(x1=x1, x1b=x1b)
                    def f2():
                        ci = s * NCS + c
                        is_ctx = c < NCC
                        G1, A2, B2 = bcs["c" if is_ctx else "x"]
                        x1, x1b = st["x1"], st["x1b"]
                        op("dve", lambda e: e.scalar_tensor_tensor(out=junk[:], in0=x1[:], scalar=1.0, in1=x1[:], op0=ALU.mult, op1=ALU.mult, accum_out=ssq[:, 0:1]),
                           reads=[x1b], writes=[Bj])
                        op("act", lambda e: e.activation(out=ssq[:, 1:2], in_=ssq[:, 0:1], func=AF.Ln, scale=1.0 / D, bias=epst[:, 0:1]), reads=[Bj], writes=[Bj])
                        op("act", lambda e: e.activation(out=ssq[:, 2:3], in_=ssq[:, 1:2], func=AF.Exp, scale=-0.5), reads=[Bj], writes=[Bj])
                        h2, h2b = h2r.next()
                        yield
                        op("dve", lambda e: e.scalar_tensor_tensor(out=h2[:], in0=x1[:], scalar=ssq[:, 2:3], in1=A2[:], op0=ALU.mult, op1=ALU.mult), reads=[x1b, Bj, Bbc], writes=[h2b])
                        op("dve", lambda e: e.tensor_tensor(out=h2[:], in0=h2[:], in1=B2[:], op=ALU.add), reads=[h2b, Bbc], writes=[h2b])
                        for k in range(8):
                            op("pe", lambda e: e.transpose(out=ps[:, 6 + k // 4, (k % 4) * 128:(k % 4 + 1) * 128], in_=h2[:, k * 128:(k + 1) * 128], identity=identf[:]),
                               reads=[h2b, B_const], writes=[PB[6 + k // 4]])
                        h2T, h2Tb = h2Tr.next()
                        op("act", lambda e: e.activation(out=h2T[:].rearrange("p k t -> p (k t)"), in_=ps[:, 6:8, :].rearrange("p a n -> p (a n)"), func=AF.Copy),
                           reads=[PB[6], PB[7]], writes=[h2Tb])
                        for k in range(8):
                            op("pe", lambda e: e.matmul(ps[:, 6, 0:NE], lhsT=h2T[:, k, :], rhs=rw[:, k, :], start=(k == 0), stop=(k == 7)), reads=[h2Tb, BL], writes=[PB[6]])
                        lg, lgb = lgr.next()
                        yield
                        op("dve", lambda e: e.tensor_tensor(out=lg[:], in0=ps[:, 6, 0:NE], in1=rbbc[:], op=ALU.add), reads=[PB[6], BL], writes=[lgb])
                        if DEBUG:
                            dma("sp", lambda e: e.dma_start(out=dbg_lg[ci * 128:(ci + 1) * 128, :], in_=lg[:]), reads=[lgb])
                        rt, rtb = rtr.next()
                        sm, smb = smr.next()
                        op("dve", lambda e: e.max(out=sm[:, 0:8], in_=lg[:]), reads=[lgb], writes=[smb])
                        op("dve", lambda e: e.tensor_scalar(rt[:, 0, :], lg[:], sm[:, 3:4], None, op0=ALU.is_ge), reads=[lgb, smb], writes=[rtb])
                        op("dve", lambda e: e.tensor_scalar(sm[:, 8:9], sm[:, 0:1], -1.0, None, op0=ALU.mult), reads=[smb], writes=[smb])
                        op("act", lambda e: e.activation(out=rt[:, 1, :], in_=lg[:], func=AF.Exp, bias=sm[:, 8:9], scale=1.0), reads=[lgb, smb], writes=[rtb])
                        yield
                        op("dve", lambda e: e.scalar_tensor_tensor(out=rt[:, 1, :], in0=rt[:, 1, :], scalar=1.0, in1=rt[:, 0, :], op0=ALU.mult, op1=ALU.mult, accum_out=sm[:, 9:10]),
                           reads=[rtb], writes=[rtb, smb])
                        op("dve", lambda e: e.reciprocal(out=sm[:, 10:11], in_=sm[:, 9:10]), reads=[smb], writes=[smb])
                        op("dve", lambda e: e.tensor_scalar(Gd[:, ci, :], rt[:, 1, :], sm[:, 10:11], None, op0=ALU.mult), reads=[rtb, smb], writes=[Brt_])
                        mb_, mbb = mbr.next()
                        op("dve", lambda e: e.tensor_copy(out=mb_[:], in_=rt[:, 0, :]), reads=[rtb], writes=[mbb])
                        op("pe", lambda e: e.matmul(ps[:, 7, 32:64], lhsT=ustr[:], rhs=mb_[:], start=True, stop=True), reads=[mbb, B_const], writes=[PB[7]])
                        op("pe", lambda e: e.matmul(ps[:, 7, 64:96], lhsT=onesb[:], rhs=mb_[:], start=True, stop=True), reads=[mbb, B_const], writes=[PB[7]])
                        yield
                        op("dve", lambda e: e.tensor_tensor(out=rt[:, 2, :], in0=ps[:, 7, 32:64], in1=basecap[:], op=ALU.add), reads=[PB[7], Brt_], writes=[rtb])
                        op("dve", lambda e: e.tensor_tensor(out=basecap[:], in0=ps[:, 7, 64:96], in1=basecap[:], op=ALU.add), reads=[PB[7], Brt_], writes=[Brt_])
                        for k4 in range(4):
                            op("dve", lambda e: e.scalar_tensor_tensor(out=rt[:, 3, :], in0=lg[:], scalar=sm[:, k4:k4 + 1], in1=rt[:, 2, :], op0=ALU.is_equal, op1=ALU.mult,
                                                                       accum_out=poskf[:, ci, k4:k4 + 1]), reads=[lgb, smb, rtb, Brt_], writes=[rtb, Brt_])
                            op("dve", lambda e: e.scalar_tensor_tensor(out=rt[:, 3, :], in0=lg[:], scalar=sm[:, k4:k4 + 1], in1=rowc[:, 1, 0:NE], op0=ALU.is_equal, op1=ALU.mult,
                                                                       accum_out=ekf[:, ci, k4:k4 + 1]), reads=[lgb, smb, rtb, Brt_], writes=[rtb, Brt_])
                            op("dve", lambda e: e.scalar_tensor_tensor(out=rt[:, 3, :], in0=lg[:], scalar=sm[:, k4:k4 + 1], in1=Gd[:, ci, :], op0=ALU.is_equal, op1=ALU.mult,
                                                                       accum_out=gatek[:, ci, k4:k4 + 1]), reads=[lgb, smb, rtb, Brt_], writes=[rtb, Brt_])
                        h2b_, h2bb = h2br.next()
                        op("act", lambda e: e.activation(out=h2b_[:], in_=h2[:], func=AF.Copy), reads=[h2b], writes=[h2bb])
                        dma("sp", lambda e: e.dma_start(out=h2d_d[ci * 128:(ci + 1) * 128, :], in_=h2b_[:]), reads=[h2bb])
                    return fL, f1, f2
                fB = [mkB(c) for c in chunks]
                nB = len(fB)
                for i in range(nB + 2):
                    if i < nB:
                        fB[i][0]()
                    gens = []
                    if 0 <= i - 1 < nB:
                        gens.append(fB[i - 1][1]())
                    if 0 <= i - 2 < nB:
                        gens.append(fB[i - 2][2]())
                    while gens:
                        for g_ in list(gens):
                            if next(g_, "done") == "done":
                                gens.remove(g_)
                fw.barrier()
            sq.close()
        ls.close()
        if STOP_AFTER in ("passA", "scan", "fourier", "mixer0"):
            lt.close()
            break

        conv_step(10000)
        idxW = sb("idxW", [128, NSB, 3], I32, scope=lt)
        idxB = sb("idxB", [128, NSB], I32, scope=lt)
        Bsch = Buf("sched")
        with ExitStack() as sc:
            cnt = sb("cnt", [128, NE], scope=sc)
            nblk = sb("nblk", [128, NE], scope=sc)
            cum = sb("cum", [128, NE], scope=sc)
            cumex = sb("cumex", [128, NE], scope=sc)
            ones32 = sb("ones32", [128, NE], scope=sc)
            cmp17 = sb("cmp17", [128, NE, 17], scope=sc)
            cmpb = sb("cmpb", [128, NSB, NE], scope=sc)
            ebt = sb("ebt", [128, 4, NSB], scope=sc)
            idf = sb("idf", [128, 1, NSB, 4], scope=sc)
            cmpd = sb("cmpd", [128, NTOKC * 4, NE], scope=sc)
            destf = sb("destf", [128, NTOKC * 4], scope=sc)
            bvals = rowc[:, 3, 0:NSB]
            op("dve", lambda e: e.tensor_copy(out=cnt[:], in_=basecap[:]), reads=[Brt_, B_const], writes=[Bsch])
            op("dve", lambda e: e.tensor_tensor(out=cmp17[:], in0=bc_mid(cnt[:], 17), in1=rowc[:, 2, 0:17].unsqueeze(1).to_broadcast([128, NE, 17]), op=ALU.is_gt), reads=[Bsch], writes=[Bsch])
            op("dve", lambda e: e.tensor_reduce(out=nblk[:], in_=cmp17[:], axis=AX.X, op=ALU.add), reads=[Bsch], writes=[Bsch])
            op("dve", lambda e: e.memset(ones32[:], 1.0), writes=[Bsch])
            op("dve", lambda e: e.tensor_tensor_scan(out=cum[:], data0=ones32[:], data1=nblk[:], initial=0.0, op0=ALU.mult, op1=ALU.add), reads=[Bsch], writes=[Bsch])
            op("dve", lambda e: e.tensor_tensor(out=cumex[:], in0=cum[:], in1=nblk[:], op=ALU.subtract), reads=[Bsch], writes=[Bsch])
            op("dve", lambda e: e.tensor_tensor(out=cmpb[:], in0=cum[:].unsqueeze(1).to_broadcast([128, NSB, NE]), in1=bc_mid(bvals, NE), op=ALU.is_le), reads=[Bsch], writes=[Bsch])
            op("dve", lambda e: e.tensor_reduce(out=ebt[:, 0, :], in_=cmpb[:], axis=AX.X, op=ALU.add), reads=[Bsch], writes=[Bsch])
            op("dve", lambda e: e.tensor_scalar(ebt[:, 0, :], ebt[:, 0, :], float(NE - 1), None, op0=ALU.min), reads=[Bsch], writes=[Bsch])
            op("dve", lambda e: e.tensor_scalar(ebt[:, 3, :], ebt[:, 0, :], 128.0, pcol[:, 2:3], op0=ALU.mult, op1=ALU.add), reads=[Bsch], writes=[Bsch])
            op("dve", lambda e: e.memset(ebt[:, 1, :], 0.0), writes=[Bsch])
            op("dve", lambda e: e.tensor_tensor(out=ebt[:, 1, 2:NSB], in0=ebt[:, 0, 2:NSB], in1=ebt[:, 0, 0:NSB - 2], op=ALU.is_equal), reads=[Bsch], writes=[Bsch])
            for m in range(3):
                op("dve", lambda e: e.scalar_tensor_tensor(out=idf[:, 0, :, m], in0=ebt[:, 1, :], scalar=1.0e6, in1=ebt[:, 3, :], op0=ALU.mult, op1=ALU.add), reads=[Bsch], writes=[Bsch])
                if m > 0:
                    op("dve", lambda e: e.tensor_scalar(idf[:, 0, :, m], idf[:, 0, :, m], float(m * NE * 128), None, op0=ALU.add), reads=[Bsch], writes=[Bsch])
            op("dve", lambda e: e.tensor_copy(out=idxW[:], in_=idf[:, 0, :, 0:3]), reads=[Bsch], writes=[Bsch])
            if l > 0:
                op("dve", lambda e: e.tensor_scalar(ebt[:, 3, :], ebt[:, 3, :], float(l * NE * 128), None, op0=ALU.add), reads=[Bsch], writes=[Bsch])
            op("dve", lambda e: e.tensor_copy(out=idxB[:], in_=ebt[:, 3, :]), reads=[Bsch], writes=[Bsch])
            op("dve", lambda e: e.tensor_scalar(cumex[:], cumex[:], 512.0, None, op0=ALU.mult), reads=[Bsch], writes=[Bsch])
            NA = NTOKC * 4
            ekflat = ekf[:].rearrange("p c k -> p (c k)")
            op("dve", lambda e: e.tensor_tensor(out=cmpd[:], in0=rowc[:, 1, 0:NE].unsqueeze(1).to_broadcast([128, NA, NE]), in1=bc_mid(ekflat, NE), op=ALU.is_equal), reads=[Bsch, Brt_], writes=[Bsch])
            op("dve", lambda e: e.tensor_tensor(out=cmpd[:], in0=cmpd[:], in1=cumex[:].unsqueeze(1).to_broadcast([128, NA, NE]), op=ALU.mult), reads=[Bsch], writes=[Bsch])
            op("dve", lambda e: e.tensor_reduce(out=destf[:], in_=cmpd[:], axis=AX.X, op=ALU.add), reads=[Bsch], writes=[Bsch])
            op("dve", lambda e: e.tensor_tensor(out=destf[:], in0=destf[:], in1=poskf[:].rearrange("p c k -> p (c k)"), op=ALU.add), reads=[Bsch, Brt_], writes=[Bsch])
            op("dve", lambda e: e.tensor_copy(out=destk[:].rearrange("p c k -> p (c k)"), in_=destf[:]), reads=[Bsch], writes=[Brt_])
            if DEBUG:
                dma("sp", lambda e: e.dma_start(out=dbg_sched[:, 0:NE], in_=cnt[:]), reads=[Bsch])
                dma("sp", lambda e: e.dma_start(out=dbg_sched[:, NE:NE + NSB], in_=ebt[:, 0, :]), reads=[Bsch])
                dma("sp", lambda e: e.dma_start(out=dbg_sched[:, NE + NSB:NE + NSB + 64], in_=destf[:, 0:64]), reads=[Bsch])
            fw.barrier()
        with ExitStack() as dp:
            hdr = Ring(nc, dp, "h2ld", 3, [128, D], BF16)
            for (s_, c_) in lchunks:
                ci = s_ * NCS + c_
                hd, hdb = hdr.next()
                dma("sp", lambda e: e.dma_start(out=hd[:], in_=h2d_d[ci * 128:(ci + 1) * 128, :]), writes=[hdb])
                for k4 in range(4):
                    dma("pool", lambda e: e.indirect_dma_start(out=hbuf_d[:, :], out_offset=bass.IndirectOffsetOnAxis(ap=destk[:, ci, k4:k4 + 1], axis=0),
                                                               in_=hd[:, :], in_offset=None), reads=[hdb, Brt_])
            fw.barrier()
        IOA = bass.IndirectOffsetOnAxis
        with ExitStack() as mo:
            wr = Ring(nc, mo, "wexp", 2, [128, 3, 4, 2048], BF16)
            b1r = Ring(nc, mo, "b1t", 3, [128, 24], F32)
            hrr = Ring(nc, mo, "hrows", 2, [128, 4, D], BF16)
            hTr = Ring(nc, mo, "hTm", 2, [128, 8, 512], BF16)
            atr = Ring(nc, mo, "actT", 2, [128, 8, 512], BF16)
            xgr = Ring(nc, mo, "xg", 2, [128, 4, 512], F32)
            osr = Ring(nc, mo, "ostage", 3, [128, D], F32)
            wsrc = (w1g_d, w1l_d, w2_d)
            state = {}

            def load_sb(b):
                b1t, b1b = b1r.next()
                dma("pool", lambda e: e.indirect_dma_start(out=b1t[:, 0:16], out_offset=None, in_=b1T_d.rearrange("l r f -> (l r) f"), in_offset=IOA(ap=idxB[:, b:b + 1], axis=0)), reads=[Bsch], writes=[b1b])
                W, Wb = wr.next()
                for m in range(3):
                    dma("pool", lambda e: e.indirect_dma_start(out=W[:, m].rearrange("p q f -> p (q f)"), out_offset=None, in_=wbf_d[:, :], in_offset=IOA(ap=idxW[:, b, m:m + 1], axis=0),
                                                               bounds_check=bc_reg, oob_is_err=False),
                        reads=[Bsch, Bwbf], writes=[Wb])
                hr, hrb = hrr.next()
                dma("sp", lambda e: e.dma_start(out=hr[:], in_=hbuf_d[b * 512:(b + 1) * 512, :].rearrange("(g p) d -> p g d", p=128)), writes=[hrb])
                state[b] = dict(W=W, Wb=Wb, b1t=b1t, b1b=b1b, hr=hr, hrb=hrb)

            def transp_sb(b):
                st = state[b]
                hr, hrb = st["hr"], st["hrb"]
                hT, hTb = hTr.next()
                for g in range(4):
                    pt = psb16(g % 2)
                    for k in range(8):
                        op("pe", lambda e: e.transpose(out=pt[:, k * 128:(k + 1) * 128], in_=hr[:, g, k * 128:(k + 1) * 128], identity=identb[:]), reads=[hrb, B_const], writes=[PB[g % 2]])
                    op("act", lambda e: e.activation(out=hT[:, :, g * 128:(g + 1) * 128], in_=pt.rearrange("p (k t) -> p k t", t=128), func=AF.Copy),
                       reads=[PB[g % 2]], writes=[hTb])
                st.update(hT=hT, hTb=hTb)

            def gemm1_sb(b):
                st = state[b]
                W, Wb, b1t, b1b, hT, hTb = st["W"], st["Wb"], st["b1t"], st["b1b"], st["hT"], st["hTb"]
                aT, aTb = atr.next()
                Wv = W[:].rearrange("p m q (k f) -> p m (q k) f", f=1024)
                op("dve", lambda e: e.tensor_scalar(b1t[:, 16:24], b1t[:, 8:16], 1.0, None, op0=ALU.add), reads=[b1b], writes=[b1b])
                for fc in range(8):
                    bg, bl = 2 + (fc % 2) * 2, 3 + (fc % 2) * 2
                    for k in range(8):
                        op("pe", lambda e: e.matmul(ps[:, bg, :], lhsT=Wv[:, 0, k, fc * 128:(fc + 1) * 128], rhs=hT[:, k, :], start=(k == 0), stop=(k == 7)), reads=[Wb, hTb], writes=[PB[bg]])
                    for k in range(8):
                        op("pe", lambda e: e.matmul(ps[:, bl, :], lhsT=Wv[:, 1, k, fc * 128:(fc + 1) * 128], rhs=hT[:, k, :], start=(k == 0), stop=(k == 7)), reads=[Wb, hTb], writes=[PB[bl]])
                    xg, xgb = xgr.next()
                    op("dve", lambda e: e.tensor_scalar(xg[:, 0, :], ps[:, bg, :], b1t[:, fc:fc + 1], 7.0, op0=ALU.add, op1=ALU.min), reads=[PB[bg], b1b], writes=[xgb])
                    op("act", lambda e: e.activation(out=xg[:, 1, :], in_=xg[:, 0, :], func=AF.Sigmoid, scale=1.702), reads=[xgb], writes=[xgb])
                    op("dve", lambda e: e.tensor_scalar(xg[:, 2, :], ps[:, bl, :], b1t[:, 16 + fc:17 + fc], 8.0, op0=ALU.add, op1=ALU.min), reads=[PB[bl], b1b], writes=[xgb])
                    op("dve", lambda e: e.scalar_tensor_tensor(out=xg[:, 3, :], in0=xg[:, 2, :], scalar=-6.0, in1=xg[:, 0, :], op0=ALU.max, op1=ALU.mult), reads=[xgb], writes=[xgb])
                    op("dve", lambda e: e.tensor_tensor(out=aT[:, fc, :], in0=xg[:, 3, :], in1=xg[:, 1, :], op=ALU.mult), reads=[xgb], writes=[aTb])
                st.update(aT=aT, aTb=aTb, Wv=Wv)

            def gemm2_sb(b):
                st = state[b]
                aT, aTb, Wv, Wb = st["aT"], st["aTb"], st["Wv"], st["Wb"]
                obanks = (6, 2, 4, 6)
                for g in range(4):
                    ob = obanks[g]
                    for hf in range(2):
                        for fc in range(8):
                            op("pe", lambda e: e.matmul(ps[:, ob + hf, :], lhsT=aT[:, fc, g * 128:(g + 1) * 128], rhs=Wv[:, 2, fc, hf * 512:(hf + 1) * 512], start=(fc == 0), stop=(fc == 7)),
                               reads=[aTb, Wb], writes=[PB[ob + hf]])
                    ost, osb = osr.next()
                    op("act", lambda e: e.activation(out=ost[:], in_=ps[:, ob:ob + 2, :].rearrange("p a n -> p (a n)"), func=AF.Copy), reads=[PB[ob], PB[ob + 1]], writes=[osb])
                    dma("sp", lambda e: e.dma_start(out=obuf_d[b * 512 + g * 128:b * 512 + (g + 1) * 128, :], in_=ost[:]), reads=[osb])
                del state[b]

            load_sb(0)
            transp_sb(0)
            for b in range(NSB):
                if b + 1 < NSB:
                    load_sb(b + 1)
                gemm1_sb(b)
                if b + 1 < NSB:
                    transp_sb(b + 1)
                gemm2_sb(b)
            fw.barrier()
        last = (l == DEPTH - 1)
        with ExitStack() as cb:
            b2t = sb("b2t", [NE, D], scope=cb)
            Bc = Buf("cmbconst")
            dma("sp", lambda e: e.dma_start(out=b2t[:], in_=b2_d[l]), writes=[Bc])
            G2 = {}
            for col in list(range(NSEQ)) + ([2] if has_ctx_out else []):
                G2[col] = sb(f"G2bc{col}", [128, D], scope=cb)
                dma("sp", lambda e: e.dma_start(out=G2[col][:], in_=modrow_d[col:col + 1, 3 * D:4 * D].partition_broadcast(128)), writes=[Bc])
            if last:
                fnw = sb("fnw", [128, D], scope=cb)
                dma("sp", lambda e: e.dma_start(out=fnw[:], in_=fnwbc_d), writes=[Bc])
            x1r = Ring(nc, cb, "x1c", 2, [128, D], F32)
            rwr = Ring(nc, cb, "orow", 2, [128, 4, D], F32)
            GTr = Ring(nc, cb, "GT", 2, [NE, 128], F32)
            accr = Ring(nc, cb, "acc", 2, [128, D], F32)
            junk = sb("junkC", [128, D], BF16, scope=cb)
            ssq = sb("ssqC", [128, 4], scope=cb)
            Bj = Buf("junkC")
            for (s, c) in lchunks:
                ci = s * NCS + c
                is_ctx = c < NCC
                col = 2 if is_ctx else s
                x1, x1b = x1r.next()
                dma("sp", lambda e: e.dma_start(out=x1[:], in_=xmid_d[ci * 128:(ci + 1) * 128, :]), writes=[x1b])
                orow, orb = rwr.next()
                for k4 in range(4):
                    dma("pool", lambda e: e.indirect_dma_start(out=orow[:, k4, :], out_offset=None, in_=obuf_d[:, :], in_offset=IOA(ap=destk[:, ci, k4:k4 + 1], axis=0)),
                        reads=[Brt_], writes=[orb])
                op("pe", lambda e: e.transpose(out=ps[0:NE, 0, 0:128], in_=Gd[:, ci, :], identity=identf[:]), reads=[Brt_, B_const], writes=[PB[0]])
                GT, GTb = GTr.next()
                op("act", lambda e: e.activation(out=GT[:], in_=ps[0:NE, 0, 0:128], func=AF.Copy), reads=[PB[0]], writes=[GTb])
                for hf in range(2):
                    op("pe", lambda e: e.matmul(ps[:, 1 + hf, :], lhsT=GT[:], rhs=b2t[:, hf * 512:(hf + 1) * 512], start=True, stop=True), reads=[GTb, Bc], writes=[PB[1 + hf]])
                acc, accb = accr.next()
                op("dve", lambda e: e.scalar_tensor_tensor(out=acc[:], in0=orow[:, 0, :], scalar=gatek[:, ci, 0:1], in1=ps[:, 1:3, :].rearrange("p a n -> p (a n)"), op0=ALU.mult, op1=ALU.add),
                   reads=[orb, Brt_, PB[1], PB[2]], writes=[accb])
                for k4 in range(1, 4):
                    op("dve", lambda e: e.scalar_tensor_tensor(out=acc[:], in0=orow[:, k4, :], scalar=gatek[:, ci, k4:k4 + 1], in1=acc[:], op0=ALU.mult, op1=ALU.add),
                       reads=[orb, Brt_, accb], writes=[accb])
                op("dve", lambda e: e.tensor_tensor(out=acc[:], in0=acc[:], in1=G2[col][:], op=ALU.mult), reads=[accb, Bc], writes=[accb])
                op("dve", lambda e: e.tensor_tensor(out=acc[:], in0=acc[:], in1=x1[:], op=ALU.add), reads=[accb, x1b], writes=[accb])
                if DEBUG:
                    dma("sp", lambda e: e.dma_start(out=dbg_x2[ci * 128:(ci + 1) * 128, :], in_=acc[:]), reads=[accb])
                if last:
                    op("dve", lambda e: e.scalar_tensor_tensor(out=junk[:], in0=acc[:], scalar=1.0, in1=acc[:], op0=ALU.mult, op1=ALU.mult, accum_out=ssq[:, 0:1]), reads=[accb], writes=[Bj])
                    op("act", lambda e: e.activation(out=ssq[:, 1:2], in_=ssq[:, 0:1], func=AF.Ln, scale=1.0 / D, bias=epst[:, 0:1]), reads=[Bj], writes=[Bj])
                    op("act", lambda e: e.activation(out=ssq[:, 2:3], in_=ssq[:, 1:2], func=AF.Exp, scale=-0.5), reads=[Bj], writes=[Bj])
                    op("dve", lambda e: e.scalar_tensor_tensor(out=acc[:], in0=acc[:], scalar=ssq[:, 2:3], in1=fnw[:], op0=ALU.mult, op1=ALU.mult), reads=[accb, Bj, Bc], writes=[accb])
                    dma("sp", lambda e: e.dma_start(out=out_d[s, (c - NCC) * 128:(c - NCC + 1) * 128, :], in_=acc[:]), reads=[accb])
                else:
                    dma("sp", lambda e: e.dma_start(out=xnext_d[ci * 128:(ci + 1) * 128, :], in_=acc[:]), reads=[accb])
            fw.barrier()
        lt.close()
        if STOP_AFTER is not None:
            break

    fw.barrier(engines=("sp",))
    es.close()
    return nc


def make_constants(nseq=2):
    bf = ml_dtypes.bfloat16
    k = np.arange(T, dtype=np.float64)
    ang = 2 * np.pi * np.outer(k, k) / T
    C = (np.cos(ang) / 64.0)
    S = (-np.sin(ang) / 64.0)
    def lay(M):
        M = M.reshape(4, 8, 128, 16, 256)
        return M.transpose(3, 0, 2, 1, 4)
    dft = np.stack([lay(C), lay(S)], axis=3)
    dft = np.ascontiguousarray(dft.reshape(16, 4, 128, 2 * 8 * 256)).astype(bf)
    kc = np.arange(TC, dtype=np.float64)
    angc = 2 * np.pi * np.outer(kc, kc) / TC
    Cc = (np.cos(angc) / 16.0).reshape(2, 128, 256).transpose(1, 0, 2)
    Sc = (-np.sin(angc) / 16.0).reshape(2, 128, 256).transpose(1, 0, 2)
    dftc = np.ascontiguousarray(np.stack([Cc, Sc], axis=1).reshape(128, 2 * 2 * 256)).astype(bf)
    m = np.arange(64, dtype=np.float64)
    a64 = 2 * np.pi * np.outer(m, m) / 64
    bd = np.zeros((128, 2, 128), np.float32)
    for g in range(2):
        bd[g * 64:(g + 1) * 64, 0, g * 64:(g + 1) * 64] = np.cos(a64) / 8.0
        bd[g * 64:(g + 1) * 64, 1, g * 64:(g + 1) * 64] = np.sin(a64) / 8.0
    t = np.arange(T)
    row = (t // 64).astype(np.float32)
    colp = (t % 64).astype(np.float32)
    freqs = (np.float32(10000.0) ** (-np.arange(16, dtype=np.float32) / np.float32(16))).astype(np.float32)
    angr = np.concatenate([row[:, None] * freqs, colp[:, None] * freqs], -1).astype(np.float32)
    cs = np.cos(angr).reshape(NCX, 128, 32).transpose(1, 0, 2)
    sn = np.sin(angr).reshape(NCX, 128, 32).transpose(1, 0, 2)
    rope = np.ascontiguousarray(np.stack([cs, sn], axis=1)).astype(np.float32)
    j = np.arange(128)[:, None]
    i = np.arange(128)[None, :]
    tri = np.stack([np.maximum(i - j, 0), np.maximum(j - i, 0), (i >= j) * 0.125, (j >= i) * 0.125], axis=1).astype(np.float32)
    p = np.arange(128, dtype=np.float32)
    pcol = np.stack([127 - p, p + 1, p, 128 - p], axis=1).astype(np.float32)
    capmax = 2048 * nseq + 512
    rowc = np.zeros((128, 4, 128), np.float32)
    rowc[:, 0, :32] = np.arange(32) * capmax
    rowc[:, 1, :32] = np.arange(32)
    rowc[:, 2, :17] = np.arange(17) * 512
    rowc[:, 3, :] = np.arange(128)
    ustrict = (np.arange(128)[:, None] < np.arange(128)[None, :]).astype(np.float32).astype(bf)
    return dict(rowc=rowc, ustrict=ustrict, dft=dft, dftc=dftc, bd64=bd, rope=rope, tri=np.ascontiguousarray(tri), pcol=pcol,
                identb=np.eye(128, dtype=np.float32).astype(bf), identf=np.eye(128, dtype=np.float32))


def prep_shared(inp, nseq=2):
    f = lambda a: np.ascontiguousarray(np.asarray(a, dtype=np.float32))
    sh = {}
    sh["mod_w"] = f(inp["mod_w"])
    mod_b = f(inp["mod_b"])
    sh["mod_b"] = mod_b
    sh["mod_bT"] = np.ascontiguousarray(mod_b.reshape(DEPTH, 48, 128).transpose(2, 0, 1))
    nw = f(inp["norm_w"])
    sh["nwT"] = np.ascontiguousarray(nw.reshape(DEPTH, 2, 8, 128).transpose(3, 0, 1, 2))
    sh["nw1_bc"] = np.ascontiguousarray(np.broadcast_to(nw[:, 1, :][None], (128, DEPTH, D)))
    w_in = f(inp["w_in"])
    sh["wfT"] = np.ascontiguousarray(w_in[:, :, :512].transpose(0, 2, 1))
    sh["w_qkvg"] = np.ascontiguousarray(w_in[:, :, 512:])
    sh["w_out"] = f(inp["w_out"])
    sh["rd_bc"] = np.ascontiguousarray(np.broadcast_to(f(inp["ret_decay"]).reshape(1, DEPTH, 16), (128, DEPTH, 16)))
    sh["rnw_bc"] = np.ascontiguousarray(np.broadcast_to(f(inp["ret_norm_w"])[None], (128, DEPTH, 512)))
    sh["router_w"] = f(inp["router_w"])
    sh["rb_bc"] = np.ascontiguousarray(np.broadcast_to(f(inp["router_b"])[None], (128, DEPTH, NE)))
    sh["fnw_bc"] = np.ascontiguousarray(np.broadcast_to(f(inp["final_norm_w"])[None], (128, D)))
    w1 = np.asarray(inp["expert_w1"], dtype=np.float32)
    def wlay(w):
        L, E = w.shape[0], w.shape[1]
        return np.ascontiguousarray(w.reshape(L, E, 8, 128, 1024).transpose(0, 1, 3, 2, 4)).reshape(L, E * 128 * 4, 2048)
    sh["w1g"] = wlay(w1[..., 0::2])
    sh["w1l"] = wlay(w1[..., 1::2])
    sh["w2h"] = wlay(np.asarray(inp["expert_w2"], dtype=np.float32))
    b1 = np.asarray(inp["expert_b1"], dtype=np.float32)
    b1g = b1[..., 0::2].reshape(DEPTH, NE, 8, 128).transpose(0, 1, 3, 2)
    b1l = b1[..., 1::2].reshape(DEPTH, NE, 8, 128).transpose(0, 1, 3, 2)
    sh["b1T"] = np.ascontiguousarray(np.concatenate([b1g, b1l], axis=-1)).reshape(DEPTH, NE * 128, 16)
    sh["b2"] = f(inp["expert_b2"])
    sh.update(make_constants(nseq))
    return sh


def prep_core(inp, sh, core, nseq=2):
    f = lambda a: np.ascontiguousarray(np.asarray(a, dtype=np.float32))
    b0 = core * 2
    m = dict(sh)
    m["x"] = f(inp["x"][b0:b0 + nseq])
    m["ctx"] = f(inp["ctx"][b0:b0 + nseq])
    c = np.asarray(inp["c"], dtype=np.float32)
    cols = [c[b0], c[b0 + 1], np.asarray(inp["c_ctx"], dtype=np.float32)]
    m["cT"] = np.ascontiguousarray(np.stack(cols, axis=1).reshape(8, 128, 3).transpose(1, 0, 2))
    return m


_NC_CACHE = {}


def kernel(**inputs):
    cfg = dict(nseq=2, layers=DEPTH)
    key = "full"
    if key not in _NC_CACHE:
        _NC_CACHE[key] = build_program(cfg)
    nc = _NC_CACHE[key]
    sh = prep_shared(inputs)
    in_maps = [prep_core(inputs, sh, core) for core in range(8)]
    res = run_bass_kernel_spmd(nc, in_maps, core_ids=list(range(8)))
    out = np.concatenate([np.asarray(r["out"], dtype=np.float32) for r in res.results], axis=0)
    return out
```

```python
import numpy as np
import ml_dtypes
from contextlib import ExitStack
import concourse.bass as bass
import concourse.mybir as mybir
from concourse.bass_utils import run_bass_kernel_spmd

F32 = mybir.dt.float32
BF16 = mybir.dt.bfloat16
U32 = mybir.dt.uint32
I32 = mybir.dt.int32
ALU = mybir.AluOpType
AF = mybir.ActivationFunctionType
AX = mybir.AxisListType

D = 1024
T = 4096
TC = 256
NCX = T // 128
NCC = TC // 128
NCS = NCX + NCC
NE = 32
DEPTH = 2
EPS = 1e-6


class Tok:
    __slots__ = ("sem", "val", "key", "owner")

    def __init__(self, sem, val, key, owner):
        self.sem, self.val, self.key, self.owner = sem, val, key, owner


class Buf:
    __slots__ = ("name", "w", "r")

    def __init__(self, name=""):
        self.name = name
        self.w = {}
        self.r = {}


class EngS:
    def __init__(self, name, e, sem):
        self.name, self.e, self.sem = name, e, sem
        self.count = 0
        self.waited = {}


class FW:
    def __init__(self, nc, es, n_dma_sems=12):
        self.nc = nc
        self.engs = {}
        for name, e in (("pe", nc.tensor), ("act", nc.scalar), ("dve", nc.vector),
                        ("pool", nc.gpsimd), ("sp", nc.sync)):
            sem = es.enter_context(nc.semaphore("sem_" + name))
            self.engs[name] = EngS(name, e, sem)
        self.dsems = {}
        for q in ("sp", "pool", "act", "pe"):
            lst = []
            for i in range(n_dma_sems):
                s = es.enter_context(nc.semaphore(f"dsem_{q}{i}"))
                lst.append([s, 0, None])
            self.dsems[q] = [lst, 0]

    def wait(self, E, tok):
        if tok is None:
            return
        if E.waited.get(tok.key, 0) >= tok.val:
            return
        E.e.wait_ge(tok.sem, tok.val)
        E.waited[tok.key] = tok.val

    def _deps(self, reads, writes):
        deps = []
        for b in reads:
            deps.extend(b.w.values())
        for b in writes:
            deps.extend(b.w.values())
            deps.extend(b.r.values())
        return deps

    def op(self, en, fn, reads=(), writes=()):
        E = self.engs[en]
        for tok in self._deps(reads, writes):
            if en == "pe" and tok.owner == "pe":
                continue
            self.wait(E, tok)
        ins = fn(E.e)
        E.count += 1
        ins.then_inc(E.sem, 1)
        tok = Tok(E.sem, E.count, "E" + en, en)
        E.waited[tok.key] = max(E.waited.get(tok.key, 0), 0)
        for b in reads:
            b.r[tok.key] = tok
        for b in writes:
            b.w = {tok.key: tok}
            b.r = {}
        return tok

    def dma(self, q, fn, reads=(), writes=()):
        E = self.engs[q]
        for tok in self._deps(reads, writes):
            self.wait(E, tok)
        lst, idx = self.dsems[q]
        ent = lst[idx % len(lst)]
        self.dsems[q][1] = idx + 1
        if ent[2] is not None:
            self.wait(E, ent[2])
        ins = fn(E.e)
        ent[1] += 16
        ins.then_inc(ent[0], 16)
        tok = Tok(ent[0], ent[1], f"D{q}{idx % len(lst)}", "dma")
        ent[2] = tok
        for b in reads:
            b.r[tok.key] = tok
        for b in writes:
            if all(t.owner == "dma" for t in b.w.values()):
                b.w[tok.key] = tok
            else:
                b.w = {tok.key: tok}
            b.r = {}
        return tok

    def all_toks(self):
        toks = []
        for E in self.engs.values():
            if E.count > 0:
                toks.append(Tok(E.sem, E.count, "E" + E.name, E.name))
        for q, (lst, _) in self.dsems.items():
            for ent in lst:
                if ent[2] is not None:
                    toks.append(ent[2])
        return toks

    def barrier(self, engines=("pe", "act", "dve", "pool", "sp")):
        toks = self.all_toks()
        for en in engines:
            E = self.engs[en]
            for t in toks:
                if t.owner == en:
                    continue
                self.wait(E, t)


_UID = [0]


class Ring:
    def __init__(self, nc, es, name, n, shape, dtype):
        self.tiles = []
        for i in range(n):
            _UID[0] += 1
            self.tiles.append(es.enter_context(nc.sbuf_tensor(f"{name}{i}_u{_UID[0]}", shape, dtype)))
        self.bufs = [Buf(f"{name}{i}") for i in range(n)]
        self.i = 0

    def next(self):
        k = self.i % len(self.tiles)
        self.i += 1
        return self.tiles[k], self.bufs[k]


def bc_mid(ap2, n):
    P, A = ap2.shape
    return ap2.unsqueeze(2).to_broadcast([P, A, n])


def build_program(cfg):
    NSEQ = cfg.get("nseq", 2)
    LAYERS = cfg.get("layers", DEPTH)
    DEBUG = cfg.get("debug", False)
    STOP_AFTER = cfg.get("stop_after", None)
    NTOKC = NSEQ * NCS
    PBS = cfg.get("pb_stop", 99)

    nc = bass.Bass("TRN2", target_bir_lowering=False)
    es = ExitStack()
    fw = FW(nc, es)
    op, dma = fw.op, fw.dma

    def din(name, shape, dt=F32):
        return nc.dram_tensor(name, list(shape), dt, kind="ExternalInput").ap()

    def dscr(name, shape, dt=F32):
        return nc.dram_tensor(name, list(shape), dt, kind="Internal").ap()

    x_in = din("x", [NSEQ, T, D])
    ctx_in = din("ctx", [NSEQ, TC, D])
    cT_d = din("cT", [128, 8, 3])
    modw_d = din("mod_w", [DEPTH, D, 6 * D])
    modbT_d = din("mod_bT", [128, DEPTH, 48])
    modb_d = din("mod_b", [DEPTH, 6 * D])
    nwT_d = din("nwT", [128, DEPTH, 2, 8])
    nw1bc_d = din("nw1_bc", [128, DEPTH, D])
    wfT_d = din("wfT", [DEPTH, 512, D])
    wqkvg_d = din("w_qkvg", [DEPTH, D, 2048])
    wout_d = din("w_out", [DEPTH, D, D])
    rdbc_d = din("rd_bc", [128, DEPTH, 16])
    rnwbc_d = din("rnw_bc", [128, DEPTH, 512])
    rw_d = din("router_w", [DEPTH, D, NE])
    rbbc_d = din("rb_bc", [128, DEPTH, NE])
    fnwbc_d = din("fnw_bc", [128, D])
    dft_d = din("dft", [16, 4, 128, 2 * 8 * 256], BF16)
    dftc_d = din("dftc", [128, 2 * 2 * 256], BF16)
    bd64_d = din("bd64", [128, 2, 128])
    rope_d = din("rope", [128, 2, NCX, 32])
    tri_d = din("tri", [128, 4, 128])
    pcol_d = din("pcol", [128, 4])
    identb_d = din("identb", [128, 128], BF16)
    identf_d = din("identf", [128, 128])

    CAPMAX = 2048 * NSEQ + 512
    CAPB = CAPMAX // 512
    w1g_d = din("w1g", [DEPTH, NE * 128 * 4, 2048])
    w1l_d = din("w1l", [DEPTH, NE * 128 * 4, 2048])
    w2_d = din("w2h", [DEPTH, NE * 128 * 4, 2048])
    b1T_d = din("b1T", [DEPTH, NE * 128, 16])
    b2_d = din("b2", [DEPTH, NE, D])
    ustr_d = din("ustrict", [128, 128], BF16)
    rows_d = din("rowc", [128, 4, 256])
    BS = 512
    GB = BS // 128
    NSB_MAX = (NTOKC * 128 * 4) // BS + NE
    NT = (NTOKC * 128) // BS + 1
    hbuf_d = dscr("hbuf", [NSB_MAX * BS, D], BF16)
    obuf_d = dscr("obuf", [NSB_MAX * BS, D])
    h2d_d = dscr("h2d", [NTOKC * 128, D], BF16)
    wbf_d = dscr("wbf", [3 * NE * 128, 8192], BF16)
    Bwbf = Buf("wbf")
    wsrc_all = (w1g_d, w1l_d, w2_d)
    CONV_R = 256

    def conv_pieces(l):
        dst = wbf_d.rearrange("r (q f) -> (r q) f", f=2048)
        for m in range(3):
            for r0 in range(0, NE * 128 * 4, CONV_R):
                yield (lambda m=m, r0=r0: dma("pool", lambda e: e.dma_start(out=dst[m * NE * 128 * 4 + r0:m * NE * 128 * 4 + r0 + CONV_R, :],
                                                                            in_=wsrc_all[m][l, r0:r0 + CONV_R, :]), writes=[Bwbf]))
    out_d = nc.dram_tensor("out", [NSEQ, T, D], F32, kind="ExternalOutput").ap()
    if DEBUG:
        dbg_x = nc.dram_tensor("dbg_x", [NTOKC * 128, D], F32, kind="ExternalOutput").ap()
        dbg_lg = nc.dram_tensor("dbg_lg", [NTOKC * 128, NE], F32, kind="ExternalOutput").ap()
        dbg_x2 = nc.dram_tensor("dbg_x2", [NTOKC * 128, D], F32, kind="ExternalOutput").ap()
        dbg_sched = nc.dram_tensor("dbg_sched", [128, 512], F32, kind="ExternalOutput").ap()

    spill_d = dscr("spill", [NTOKC, 128, 3072], BF16)
    zs_d = dscr("zs", [NSEQ, 128, NCX, 1024], BF16)
    zc_d = dscr("zc", [NSEQ, 128, NCC, 1024], BF16)
    yts_d = dscr("yts", [NTOKC, 128, 512], BF16)
    xmid_d = dscr("xmid", [NTOKC * 128, D])
    xnext_d = dscr("xnext", [NTOKC * 128, D])
    modrow_d = dscr("modrow", [3, 4 * D])

    def sb(name, shape, dt=F32, scope=es):
        _UID[0] += 1
        return scope.enter_context(nc.sbuf_tensor(f"{name}_u{_UID[0]}", list(shape), dt))

    identb = sb("identb", [128, 128], BF16)
    identf = sb("identf", [128, 128])
    tri = sb("tri", [128, 4, 128])
    pcol = sb("pcol", [128, 4])
    bd64 = sb("bd64", [128, 2, 128])
    epst = sb("epst", [128, 1])
    cT = sb("cT", [128, 8, 3])
    B_const = Buf("const")
    dma("sp", lambda e: e.dma_start(out=identb[:], in_=identb_d), writes=[B_const])
    dma("sp", lambda e: e.dma_start(out=identf[:], in_=identf_d), writes=[B_const])
    dma("sp", lambda e: e.dma_start(out=tri[:], in_=tri_d), writes=[B_const])
    dma("sp", lambda e: e.dma_start(out=pcol[:], in_=pcol_d), writes=[B_const])
    dma("sp", lambda e: e.dma_start(out=bd64[:], in_=bd64_d), writes=[B_const])
    dma("sp", lambda e: e.dma_start(out=cT[:], in_=cT_d), writes=[B_const])
    op("dve", lambda e: e.memset(epst[:], EPS), writes=[B_const])
    ustr = sb("ustr", [128, 128], BF16)
    onesb = sb("onesb", [128, 128], BF16)
    rowc = sb("rowc", [128, 4, 256])
    dma("sp", lambda e: e.dma_start(out=ustr[:], in_=ustr_d), writes=[B_const])
    dma("sp", lambda e: e.dma_start(out=rowc[:], in_=rows_d), writes=[B_const])
    op("dve", lambda e: e.memset(onesb[:], 1.0), writes=[B_const])
    siluT = sb("siluT", [128, 8, 3])
    op("act", lambda e: e.activation(out=siluT[:], in_=cT[:], func=AF.Silu), reads=[B_const], writes=[B_const])

    bc_reg = nc.gpsimd.to_reg(3 * NE * 128 - 1)
    ps = es.enter_context(nc.psum_tensor("ps", [128, 8, 512], F32))
    PB = [Buf(f"psum{i}") for i in range(8)]

    def psb16(bank):
        return ps[:, bank, :].bitcast(BF16)

    def chunk_src(l, s, c):
        if l == 0:
            if c < NCC:
                return ctx_in[s, c * 128:(c + 1) * 128, :]
            return x_in[s, (c - NCC) * 128:(c - NCC + 1) * 128, :]
        ci = s * NCS + c
        return xnext_d[ci * 128:(ci + 1) * 128, :]

    for l in range(LAYERS):
        has_ctx_out = l < DEPTH - 1
        lt = ExitStack()
        Gd = sb("Gd", [128, NTOKC, NE], scope=lt)
        destk = sb("destk", [128, NTOKC, 4], I32, scope=lt)
        gatek = sb("gatek", [128, NTOKC, 4], scope=lt)
        basecap = sb("basecap", [128, NE], scope=lt)
        poskf = sb("poskf", [128, NTOKC, 4], scope=lt)
        ekf = sb("ekf", [128, NTOKC, 4], scope=lt)
        Brt_ = Buf("route")
        op("dve", lambda e: e.memset(basecap[:], 0.0), writes=[Brt_])
        lchunks = [(s_, c_) for s_ in range(NSEQ) for c_ in (range(NCS) if has_ctx_out else range(NCC, NCS))]
        NSB = (len(lchunks) * 128 * 4) // BS + NE
        conv_it = conv_pieces(l)

        def conv_step(n=1):
            for _ in range(n):
                fn_ = next(conv_it, None)
                if fn_ is not None:
                    fn_()
        ls = ExitStack()
        logg = sb("logg", [128, 16], scope=ls)
        rdt = sb("rdt", [128, 16], scope=ls)
        DT8 = sb("DT8", [128, 8, 128], scope=ls)
        wk = sb("wk", [128, 8, 2], scope=ls)
        wq = sb("wq", [128, 8, 2], scope=ls)
        G8 = sb("G8", [128, 8], scope=ls)
        Gbc = sb("Gbc", [128, 8, 64], scope=ls)
        modF = sb("modF", [128, 16, 3], scope=ls)
        A1 = sb("A1", [128, 8, 3], scope=ls)
        wz = sb("wz", [128, 8, 1024], BF16, scope=ls)
        rnwbc = sb("rnwbc", [128, 512], scope=ls)
        nw1bc = sb("nw1bc", [128, D], scope=ls)
        rbbc = sb("rbbc", [128, NE], scope=ls)
        rw = sb("rw", [128, 8, NE], scope=ls)
        nwT = sb("nwT", [128, 2, 8], scope=ls)
        mbT = sb("mbT", [128, 48], scope=ls)
        BL = Buf("layerconst")
        dma("sp", lambda e: e.dma_start(out=rdt[:], in_=rdbc_d[:, l, :]), writes=[BL])
        dma("sp", lambda e: e.dma_start(out=rnwbc[:], in_=rnwbc_d[:, l, :]), writes=[BL])
        dma("sp", lambda e: e.dma_start(out=nw1bc[:], in_=nw1bc_d[:, l, :]), writes=[BL])
        dma("sp", lambda e: e.dma_start(out=rbbc[:], in_=rbbc_d[:, l, :]), writes=[BL])
        dma("sp", lambda e: e.dma_start(out=rw[:], in_=rw_d[l].rearrange("(k p) n -> p k n", p=128)), writes=[BL])
        dma("sp", lambda e: e.dma_start(out=nwT[:], in_=nwT_d[:, l, :, :]), writes=[BL])
        dma("sp", lambda e: e.dma_start(out=mbT[:], in_=modbT_d[:, l, :]), writes=[BL])
        op("act", lambda e: e.activation(out=logg[:], in_=rdt[:], func=AF.Exp, scale=-float(np.log(2.0))), reads=[BL], writes=[BL])
        op("dve", lambda e: e.tensor_scalar(logg[:], logg[:], -1.0, 1.0, op0=ALU.mult, op1=ALU.add), reads=[BL], writes=[BL])
        op("act", lambda e: e.activation(out=logg[:], in_=logg[:], func=AF.Ln), reads=[BL], writes=[BL])
        with ExitStack() as ss:
            tmpa = sb("tmpa", [128, 128], scope=ss)
            tmpb = sb("tmpb", [128, 128], scope=ss)
            Bt = Buf("tmpab")
            for h in range(8):
                op("act", lambda e: e.activation(out=tmpa[:], in_=tri[:, 0, :], func=AF.Exp, scale=logg[:, h:h + 1]), reads=[BL, B_const], writes=[Bt])
                op("dve", lambda e: e.tensor_tensor(out=tmpa[:], in0=tmpa[:], in1=tri[:, 2, :], op=ALU.mult), reads=[Bt], writes=[Bt])
                op("act", lambda e: e.activation(out=tmpb[:], in_=tri[:, 1, :], func=AF.Exp, scale=logg[:, 8 + h:9 + h]), reads=[BL, Bt], writes=[Bt])
                op("dve", lambda e: e.tensor_tensor(out=tmpb[:], in0=tmpb[:], in1=tri[:, 3, :], op=ALU.mult), reads=[Bt], writes=[Bt])
                op("dve", lambda e: e.tensor_tensor(out=DT8[:, h, :], in0=tmpa[:], in1=tmpb[:], op=ALU.add), reads=[Bt], writes=[BL])
            op("act", lambda e: e.activation(out=wk[:, :, 0], in_=logg[:, 0:8], func=AF.Exp, scale=pcol[:, 0:1]), reads=[BL], writes=[BL])
            op("act", lambda e: e.activation(out=wk[:, :, 1], in_=logg[:, 8:16], func=AF.Exp, scale=pcol[:, 2:3]), reads=[BL], writes=[BL])
            op("act", lambda e: e.activation(out=wq[:, :, 0], in_=logg[:, 0:8], func=AF.Exp, scale=pcol[:, 1:2]), reads=[BL], writes=[BL])
            op("act", lambda e: e.activation(out=wq[:, :, 1], in_=logg[:, 8:16], func=AF.Exp, scale=pcol[:, 3:4]), reads=[BL], writes=[BL])
            op("dve", lambda e: e.tensor_scalar(wq[:], wq[:], 0.125, None, op0=ALU.mult), reads=[BL], writes=[BL])
            op("act", lambda e: e.activation(out=G8[0:64, :], in_=logg[0:64, 0:8], func=AF.Exp, scale=128.0), reads=[BL], writes=[BL])
            op("act", lambda e: e.activation(out=G8[64:128, :], in_=logg[64:128, 8:16], func=AF.Exp, scale=128.0), reads=[BL], writes=[BL])
            op("dve", lambda e: e.tensor_copy(out=Gbc[:], in_=bc_mid(G8[:], 64)), reads=[BL], writes=[BL])
            fw.barrier()
        with ExitStack() as ss:
            mwr = Ring(nc, ss, "mw", 2, [128, 8, 512], F32)
            mbrow = sb("mbrow", [3, 4 * D], scope=ss)
            mrow = sb("mrow", [3, 4 * D], scope=ss)
            Bmb = Buf("mbrow")
            for r in range(3):
                dma("sp", lambda e: e.dma_start(out=mbrow[r:r + 1, :], in_=modb_d[l:l + 1, 2 * D:6 * D]), writes=[Bmb])
            psM = ps[:, 0, 0:64].rearrange("p (a b) -> p a b", b=4)
            for cb in range(12):
                m, half = cb // 2, cb % 2
                mw, mwb = mwr.next()
                dma("sp", lambda e: e.dma_start(out=mw[:], in_=modw_d[l, :, cb * 512:(cb + 1) * 512].rearrange("(k p) n -> p k n", p=128)), writes=[mwb])
                if m in (0, 1):
                    for jj in range(4):
                        idx = m * 8 + half * 4 + jj
                        for k in range(8):
                            op("pe", lambda e: e.matmul(psM[:, idx, 0:3], lhsT=mw[:, k, jj * 128:(jj + 1) * 128], rhs=siluT[:, k, :],
                                                        start=(k == 0), stop=(k == 7)), reads=[mwb, B_const], writes=[PB[0]])
                else:
                    for k in range(8):
                        op("pe", lambda e: e.matmul(ps[0:3, 1, :], lhsT=siluT[:, k, :], rhs=mw[:, k, :],
                                                    start=(k == 0), stop=(k == 7)), reads=[mwb, B_const], writes=[PB[1]])
                    c0 = (m - 2) * D + half * 512
                    op("dve", lambda e: e.tensor_tensor(out=mrow[0:3, c0:c0 + 512], in0=ps[0:3, 1, :], in1=mbrow[0:3, c0:c0 + 512], op=ALU.add),
                       reads=[PB[1], Bmb], writes=[Bmb])
            op("dve", lambda e: e.tensor_tensor(out=modF[:], in0=psM[:, :, 0:3], in1=bc_mid(mbT[:, 0:16], 3), op=ALU.add), reads=[PB[0], BL], writes=[BL])
            op("dve", lambda e: e.scalar_tensor_tensor(out=A1[:], in0=modF[:, 8:16, :], scalar=1.0, in1=bc_mid(nwT[:, 0, :], 3), op0=ALU.add, op1=ALU.mult),
               reads=[BL], writes=[BL])
            dma("sp", lambda e: e.dma_start(out=modrow_d[:, :], in_=mrow[0:3, :]), reads=[Bmb], writes=[BL])
            fw.barrier()
        with ExitStack() as ss:
            wft = sb("wft", [128, 4, D], scope=ss)
            Bw = Buf("wft")
            dma("sp", lambda e: e.dma_start(out=wft[:], in_=wfT_d[l].rearrange("(g p) d -> p g d", p=128)), writes=[Bw])
            for dk in range(8):
                for cs in range(2):
                    for gc in range(4):
                        op("pe", lambda e: e.matmul(ps[:, 2 + cs, gc * 128:(gc + 1) * 128], lhsT=wft[:, gc, dk * 128:(dk + 1) * 128], rhs=bd64[:, cs, :],
                                                    start=True, stop=True), reads=[Bw, B_const], writes=[PB[2 + cs]])
                    op("act", lambda e: e.activation(out=wz[:, dk, cs * 512:(cs + 1) * 512], in_=ps[:, 2 + cs, :], func=AF.Copy), reads=[PB[2 + cs]], writes=[BL])
            fw.barrier()

        if STOP_AFTER == "setup":
            ls.close()
            lt.close()
            break
        for s in range(NSEQ):
            sq = ExitStack()
            kvs = sb("kvs", [128, NCS, 512], BF16, scope=sq)
            Bkv = Buf("kvs")
            with ExitStack() as pa:
                wqk = sb("wqk", [128, 8, 2048], BF16, scope=pa)
                Bwqk = Buf("wqk")
                for k in range(8):
                    dma("pool", lambda e: e.dma_start(out=wqk[:, k, :], in_=wqkvg_d[l, k * 128:(k + 1) * 128, :]), writes=[Bwqk])
                ropet = sb("ropet", [128, 2, NCX, 32], scope=pa)
                dma("sp", lambda e: e.dma_start(out=ropet[:], in_=rope_d), writes=[Bwqk])
                xr = Ring(nc, pa, "xa", 3, [128, D], F32)
                junk = sb("junkA", [128, D], BF16, scope=pa)
                Bj = Buf("junkA")
                ssq = sb("ssqA", [128, 4], scope=pa)
                xnr = Ring(nc, pa, "xn", 2, [128, D], BF16)
                hTr = Ring(nc, pa, "hT", 2, [128, 8, 128], BF16)
                qkr = Ring(nc, pa, "qkrot", 2, [128, 16, 64], BF16)
                rtmp = sb("rtmp", [128, 4, 16, 32], scope=pa)
                Brt = Buf("rtmp")
                kwr = Ring(nc, pa, "kw", 2, [128, 8, 2, 64], BF16)
                qcr = Ring(nc, pa, "qc", 2, [128, 8, 2, 64], BF16)
                sgt = sb("sgt", [128, 512], scope=pa)
                Bsg = Buf("sgt")
                recr = Ring(nc, pa, "recA", 2, [128, 3072], BF16)
                zrr = Ring(nc, pa, "zrec", 2, [128, 1024], BF16)
                def mkA(c):
                    st = {}
                    def fld():
                        xt, xb = xr.next()
                        dma("sp", lambda e: e.dma_start(out=xt[:], in_=chunk_src(l, s, c)), writes=[xb])
                        conv_step(1)
                        st.update(xt=xt, xb=xb)
                    def f0():
                        ci = s * NCS + c
                        is_ctx = c < NCC
                        col = 2 if is_ctx else s
                        xt, xb = st["xt"], st["xb"]
                        op("dve", lambda e: e.scalar_tensor_tensor(out=junk[:], in0=xt[:], scalar=1.0, in1=xt[:], op0=ALU.mult, op1=ALU.mult, accum_out=ssq[:, 0:1]),
                           reads=[xb], writes=[Bj])
                        op("act", lambda e: e.activation(out=ssq[:, 1:2], in_=ssq[:, 0:1], func=AF.Ln, scale=1.0 / D, bias=epst[:, 0:1]), reads=[Bj], writes=[Bj])
                        op("act", lambda e: e.activation(out=ssq[:, 2:3], in_=ssq[:, 1:2], func=AF.Exp, scale=-0.5), reads=[Bj], writes=[Bj])
                        xn, xnb = xnr.next()
                        op("dve", lambda e: e.tensor_scalar(xn[:], xt[:], ssq[:, 2:3], None, op0=ALU.mult), reads=[xb, Bj], writes=[xnb])
                        st.update(xn=xn, xnb=xnb)
                    def f1():
                        ci = s * NCS + c
                        is_ctx = c < NCC
                        col = 2 if is_ctx else s
                        xn, xnb = st["xn"], st["xnb"]
                        pT = psb16(0)
                        for k in range(8):
                            op("pe", lambda e: e.transpose(out=pT[:, k * 128:(k + 1) * 128], in_=xn[:, k * 128:(k + 1) * 128], identity=identb[:]),
                               reads=[xnb, B_const], writes=[PB[0]])
                        hT, hTb = hTr.next()
                        for k in range(8):
                            op("act", lambda e: e.activation(out=hT[:, k, :], in_=pT[:, k * 128:(k + 1) * 128], func=AF.Identity,
                                                             scale=A1[:, k, col:col + 1], bias=modF[:, k, col:col + 1]), reads=[PB[0], BL], writes=[hTb])
                        for grp in range(6):
                            for k in range(8):
                                rhs = wz[:, k, grp * 512:(grp + 1) * 512] if grp < 2 else wqk[:, k, (grp - 2) * 512:(grp - 1) * 512]
                                op("pe", lambda e: e.matmul(ps[:, 1 + grp, :], lhsT=hT[:, k, :], rhs=rhs, start=(k == 0), stop=(k == 7)),
                                   reads=[hTb, BL, Bwqk], writes=[PB[1 + grp]])
                        rec, recb = recr.next()
                        if (not is_ctx) or has_ctx_out:
                            zr, zrb = zrr.next()
                            op("act", lambda e: e.activation(out=zr[:, 0:512], in_=ps[:, 1, :], func=AF.Copy), reads=[PB[1]], writes=[zrb])
                            op("act", lambda e: e.activation(out=zr[:, 512:1024], in_=ps[:, 2, :], func=AF.Copy), reads=[PB[2]], writes=[zrb])
                            if is_ctx:
                                dma("sp", lambda e: e.dma_start(out=zc_d[s, :, c, :], in_=zr[:]), reads=[zrb])
                            else:
                                dma("sp", lambda e: e.dma_start(out=zs_d[s, :, c - NCC, :], in_=zr[:]), reads=[zrb])
                        qk, qkb = qkr.next()
                        psqk = ps[:, 3:5, :].rearrange("p a (h d) -> p (a h) d", d=64)
                        if is_ctx:
                            op("act", lambda e: e.activation(out=qk[:], in_=psqk, func=AF.Copy), reads=[PB[3], PB[4]], writes=[qkb])
                        else:
                            cx = c - NCC
                            cosb = ropet[:, 0, cx, :].unsqueeze(1).to_broadcast([128, 16, 32])
                            sinb = ropet[:, 1, cx, :].unsqueeze(1).to_broadcast([128, 16, 32])
                            t1, t2 = psqk[:, :, 0:32], psqk[:, :, 32:64]
                            op("dve", lambda e: e.tensor_tensor(out=rtmp[:, 0], in0=t1, in1=cosb, op=ALU.mult), reads=[PB[3], PB[4], Bwqk], writes=[Brt])
                            op("dve", lambda e: e.tensor_tensor(out=rtmp[:, 1], in0=t2, in1=sinb, op=ALU.mult), reads=[PB[3], PB[4], Bwqk], writes=[Brt])
                            op("dve", lambda e: e.tensor_tensor(out=rtmp[:, 2], in0=t1, in1=sinb, op=ALU.mult), reads=[PB[3], PB[4], Bwqk], writes=[Brt])
                            op("dve", lambda e: e.tensor_tensor(out=rtmp[:, 3], in0=t2, in1=cosb, op=ALU.mult), reads=[PB[3], PB[4], Bwqk], writes=[Brt])
                            op("dve", lambda e: e.tensor_tensor(out=qk[:, :, 0:32], in0=rtmp[:, 0], in1=rtmp[:, 1], op=ALU.subtract), reads=[Brt], writes=[qkb])
                            op("dve", lambda e: e.tensor_tensor(out=qk[:, :, 32:64], in0=rtmp[:, 2], in1=rtmp[:, 3], op=ALU.add), reads=[Brt], writes=[qkb])
                        kw, kwb = kwr.next()
                        qc, qcb = qcr.next()
                        op("dve", lambda e: e.tensor_tensor(out=kw[:], in0=qk[:, 8:16, :].unsqueeze(2).to_broadcast([128, 8, 2, 64]),
                                                            in1=wk[:].unsqueeze(3).to_broadcast([128, 8, 2, 64]), op=ALU.mult), reads=[qkb, BL], writes=[kwb])
                        op("dve", lambda e: e.tensor_tensor(out=qc[:], in0=qk[:, 0:8, :].unsqueeze(2).to_broadcast([128, 8, 2, 64]),
                                                            in1=wq[:].unsqueeze(3).to_broadcast([128, 8, 2, 64]), op=ALU.mult), reads=[qkb, BL], writes=[qcb])
                        op("act", lambda e: e.activation(out=rec[:, 2048:2560], in_=ps[:, 5, :], func=AF.Copy), reads=[PB[5]], writes=[recb])
                        op("act", lambda e: e.activation(out=sgt[:], in_=ps[:, 6, :], func=AF.Silu), reads=[PB[6]], writes=[Bsg])
                        op("dve", lambda e: e.tensor_tensor(out=rec[:, 2560:3072], in0=sgt[:], in1=rnwbc[:], op=ALU.mult), reads=[Bsg, BL], writes=[recb])
                        for h in range(8):
                            op("pe", lambda e: e.matmul(ps[:, 0, h * 64:(h + 1) * 64], lhsT=kw[:, h].rearrange("p a d -> p (a d)"), rhs=rec[:, 2048 + h * 64:2048 + (h + 1) * 64],
                                                        start=True, stop=True), reads=[kwb, recb], writes=[PB[0]])
                        op("act", lambda e: e.activation(out=kvs[:, c, :], in_=ps[:, 0, :], func=AF.Copy), reads=[PB[0]], writes=[Bkv])
                        p7 = psb16(7)
                        for j in range(8):
                            op("pe", lambda e: e.transpose(out=p7[:, j * 128:(j + 1) * 128], in_=qk[:, 2 * j:2 * j + 2, :].rearrange("p a d -> p (a d)"), identity=identb[:]),
                               reads=[qkb, B_const], writes=[PB[7]])
                        op("act", lambda e: e.activation(out=rec[:, 0:1024], in_=p7[:, :], func=AF.Copy), reads=[PB[7]], writes=[recb])
                        for h in range(8):
                            op("pe", lambda e: e.transpose(out=p7[:, h * 128:(h + 1) * 128], in_=qc[:, h].rearrange("p a d -> p (a d)"), identity=identb[:]),
                               reads=[qcb, B_const], writes=[PB[7]])
                        op("act", lambda e: e.activation(out=rec[:, 1024:2048], in_=p7[:, :], func=AF.Copy), reads=[PB[7]], writes=[recb])
                        dma("sp", lambda e: e.dma_start(out=spill_d[ci], in_=rec[:]), reads=[recb])
                    return f0, f1, fld
                fA = [mkA(c) for c in range(NCS)]
                fA[0][2]()
                for i in range(NCS + 1):
                    if i + 1 < NCS:
                        fA[i + 1][2]()
                    if i < NCS:
                        fA[i][0]()
                    if i >= 1:
                        fA[i - 1][1]()
                fw.barrier()
            if STOP_AFTER == "passA":
                sq.close()
                break
            with ExitStack() as fo:
                Zt = sb("Zt", [128, NCX, 1024], BF16, scope=fo)
                Bz = Buf("Zt")
                for q4 in range(4):
                    dma("sp", lambda e: e.dma_start(out=Zt[:, q4 * 8:(q4 + 1) * 8, :], in_=zs_d[s, :, q4 * 8:(q4 + 1) * 8, :]), writes=[Bz])
                St = sb("St", [128, 512], scope=fo)
                tmpS = sb("tmpS", [128, 512], scope=fo)
                Bs = Buf("scan")
                op("dve", lambda e: e.memset(St[:], 0.0), writes=[Bs])
                Gf = Gbc[:].rearrange("p h d -> p (h d)")
                order_f = list(range(NCS))
                order_b = [1, 0] + list(range(NCS - 1, NCC - 1, -1))
                for (lo, hi, order) in ((0, 64, order_f), (64, 128, order_b)):
                    for c in order:
                        op("dve", lambda e: e.tensor_copy(out=tmpS[lo:hi, :], in_=kvs[lo:hi, c, :]), reads=[Bkv, Bs], writes=[Bs])
                        op("dve", lambda e: e.tensor_copy(out=kvs[lo:hi, c, :], in_=St[lo:hi, :]), reads=[Bs], writes=[Bkv])
                        op("dve", lambda e: e.tensor_tensor(out=St[lo:hi, :], in0=St[lo:hi, :], in1=Gf[lo:hi, :], op=ALU.mult), reads=[Bs, BL], writes=[Bs])
                        op("dve", lambda e: e.tensor_tensor(out=St[lo:hi, :], in0=St[lo:hi, :], in1=tmpS[lo:hi, :], op=ALU.add), reads=[Bs], writes=[Bs])
                tabr = Ring(nc, fo, "dtab", 4, [128, 2, 8, 256], BF16)
                ystr = Ring(nc, fo, "yst", 2, [128, 2, 4, 128], BF16)
                for kb in range(16):
                    bank0 = 4 * (kb % 2)
                    if kb < 14:
                        conv_step(2)
                    for tq in range(4):
                        tab, tabb = tabr.next()
                        dma("sp", lambda e: e.dma_start(out=tab[:].rearrange("p a b c -> p (a b c)"), in_=dft_d[kb, tq]), writes=[tabb])
                        for n in range(4):
                            for tcc in range(8):
                                tt = tq * 8 + tcc
                                op("pe", lambda e: e.matmul(ps[:, bank0 + n, 0:256], lhsT=Zt[:, tt, n * 128:(n + 1) * 128], rhs=tab[:, 0, tcc, :],
                                                            start=(tq == 0 and tcc == 0), stop=False), reads=[Bz, tabb], writes=[PB[bank0 + n]])
                                op("pe", lambda e: e.matmul(ps[:, bank0 + n, 0:256], lhsT=Zt[:, tt, 512 + n * 128:512 + (n + 1) * 128], rhs=tab[:, 1, tcc, :],
                                                            start=False, stop=(tq == 3 and tcc == 7)), reads=[Bz, tabb], writes=[PB[bank0 + n]])
                    yst, ystb = ystr.next()
                    for n in range(4):
                        op("act", lambda e: e.activation(out=yst[:, :, n, :], in_=ps[:, bank0 + n, 0:256].rearrange("p (a b) -> p a b", b=128), func=AF.Copy),
                           reads=[PB[bank0 + n]], writes=[ystb])
                    ci0 = s * NCS + NCC + 2 * kb
                    dma("sp", lambda e: e.dma_start(out=yts_d[ci0:ci0 + 2].rearrange("c p f -> p c f"), in_=yst[:].rearrange("p a n t -> p a (n t)")), reads=[ystb])
                if has_ctx_out:
                    Zc = sb("Zc", [128, NCC, 1024], BF16, scope=fo)
                    tabc = sb("tabc", [128, 2, 2, 256], BF16, scope=fo)
                    Bzc = Buf("Zc")
                    dma("sp", lambda e: e.dma_start(out=Zc[:], in_=zc_d[s]), writes=[Bzc])
                    dma("sp", lambda e: e.dma_start(out=tabc[:].rearrange("p a b c -> p (a b c)"), in_=dftc_d), writes=[Bzc])
                    yst, ystb = ystr.next()
                    for n in range(4):
                        for tt in range(2):
                            op("pe", lambda e: e.matmul(ps[:, n, 0:256], lhsT=Zc[:, tt, n * 128:(n + 1) * 128], rhs=tabc[:, 0, tt, :], start=(tt == 0), stop=False),
                               reads=[Bzc], writes=[PB[n]])
                            op("pe", lambda e: e.matmul(ps[:, n, 0:256], lhsT=Zc[:, tt, 512 + n * 128:512 + (n + 1) * 128], rhs=tabc[:, 1, tt, :], start=False, stop=(tt == 1)),
                               reads=[Bzc], writes=[PB[n]])
                        op("act", lambda e: e.activation(out=yst[:, :, n, :], in_=ps[:, n, 0:256].rearrange("p (a b) -> p a b", b=128), func=AF.Copy),
                           reads=[PB[n]], writes=[ystb])
                    ci0 = s * NCS
                    dma("sp", lambda e: e.dma_start(out=yts_d[ci0:ci0 + 2].rearrange("c p f -> p c f"), in_=yst[:].rearrange("p a n t -> p a (n t)")), reads=[ystb])
                fw.barrier()
            if STOP_AFTER == "fourier":
                sq.close()
                break
            with ExitStack() as pb:
                wout = sb("wout", [128, 8, D], BF16, scope=pb)
                Bwo = Buf("wout")
                for k in range(8):
                    dma("pool", lambda e: e.dma_start(out=wout[:, k, :], in_=wout_d[l, k * 128:(k + 1) * 128, :]), writes=[Bwo])
                bcs = {}
                Bbc = Buf("bc")
                for (nm, col) in (("x", s), ("c", 2)):
                    if nm == "c" and not has_ctx_out:
                        continue
                    G1 = sb(f"G1bc{nm}", [128, D], scope=pb)
                    A2 = sb(f"A2bc{nm}", [128, D], scope=pb)
                    B2 = sb(f"B2bc{nm}", [128, D], scope=pb)
                    dma("sp", lambda e: e.dma_start(out=G1[:], in_=modrow_d[col:col + 1, 0:D].partition_broadcast(128)), writes=[Bbc])
                    dma("sp", lambda e: e.dma_start(out=B2[:], in_=modrow_d[col:col + 1, D:2 * D].partition_broadcast(128)), writes=[Bbc])
                    dma("sp", lambda e: e.dma_start(out=A2[:], in_=modrow_d[col:col + 1, 2 * D:3 * D].partition_broadcast(128)), writes=[Bbc])
                    op("dve", lambda e: e.scalar_tensor_tensor(out=A2[:], in0=A2[:], scalar=1.0, in1=nw1bc[:], op0=ALU.add, op1=ALU.mult), reads=[Bbc, BL], writes=[Bbc])
                    bcs[nm] = (G1, A2, B2)
                recr = Ring(nc, pb, "recB", 2, [128, 3072], BF16)
                catr = Ring(nc, pb, "catT", 2, [128, 8, 128], BF16)
                xr = Ring(nc, pb, "xb", 2, [128, D], F32)
                PTr = Ring(nc, pb, "PT", 2, [128, 8, 128], BF16)
                gn = sb("gn", [128, 3, 8, 64], scope=pb)
                gs = sb("gs", [128, 4, 8], scope=pb)
                Bgn = Buf("gn")
                yr = Ring(nc, pb, "yb", 2, [128, 512], BF16)
                x1r = Ring(nc, pb, "x1", 2, [128, D], F32)
                junk = sb("junkB", [128, D], BF16, scope=pb)
                ssq = sb("ssqB", [128, 4], scope=pb)
                Bj = Buf("junkB")
                h2r = Ring(nc, pb, "h2", 2, [128, D], F32)
                h2Tr = Ring(nc, pb, "h2T", 2, [128, 8, 128], F32)
                lgr = Ring(nc, pb, "lg", 2, [128, NE], F32)
                rtr = Ring(nc, pb, "rt", 2, [128, 4, NE], F32)
                smr = Ring(nc, pb, "sm", 2, [128, 16], F32)
                mbr = Ring(nc, pb, "mb", 2, [128, NE], BF16)
                h2br = Ring(nc, pb, "h2b", 2, [128, D], BF16)
                chunks = list(range(NCS)) if has_ctx_out else list(range(NCC, NCS))
                def mkB(c):
                    st = {}
                    def fL():
                        ci = s * NCS + c
                        is_ctx = c < NCC
                        G1, A2, B2 = bcs["c" if is_ctx else "x"]
                        conv_step(1)
                        rec, recb = recr.next()
                        dma("sp", lambda e: e.dma_start(out=rec[:], in_=spill_d[ci]), writes=[recb])
                        cat, catb = catr.next()
                        dma("sp", lambda e: e.dma_start(out=cat[:, 0:4, :].rearrange("p a t -> p (a t)"), in_=yts_d[ci]), writes=[catb])
                        xt, xb = xr.next()
                        dma("sp", lambda e: e.dma_start(out=xt[:], in_=chunk_src(l, s, c)), writes=[xb])
                        st.update(rec=rec, recb=recb, cat=cat, catb=catb, xt=xt, xb=xb)
                    def f1():
                        ci = s * NCS + c
                        is_ctx = c < NCC
                        G1, A2, B2 = bcs["c" if is_ctx else "x"]
                        rec, recb, cat, catb, xt, xb = st["rec"], st["recb"], st["cat"], st["catb"], st["xt"], st["xb"]
                        for h in range(8):
                            hp, hh = h // 2, h % 2
                            op("pe", lambda e: e.matmul(ps[:, hh, hp * 128:(hp + 1) * 128],
                                                        lhsT=rec[hh * 64:(hh + 1) * 64, 512 + hp * 128:512 + (hp + 1) * 128],
                                                        rhs=rec[hh * 64:(hh + 1) * 64, hp * 128:(hp + 1) * 128], start=True, stop=True),
                               reads=[recb], writes=[PB[hh]])
                        PT, PTb = PTr.next()
                        yield
                        for g2 in range(2):
                            op("dve", lambda e: e.tensor_tensor(out=PT[:].rearrange("p (a b) t -> p a b t", b=2)[:, :, g2, :], in0=ps[:, g2, :].rearrange("p (h t) -> p h t", t=128),
                                                                in1=DT8[:].rearrange("p (a b) t -> p a b t", b=2)[:, :, g2, :], op=ALU.mult), reads=[PB[g2], BL], writes=[PTb])
                        for h in range(8):
                            op("pe", lambda e: e.matmul(ps[:, 2, h * 64:(h + 1) * 64], lhsT=PT[:, h, :], rhs=rec[:, 2048 + h * 64:2048 + (h + 1) * 64], start=True, stop=False),
                               reads=[PTb, recb], writes=[PB[2]])
                            op("pe", lambda e: e.matmul(ps[:, 2, h * 64:(h + 1) * 64], lhsT=rec[:, 1024 + h * 128:1024 + (h + 1) * 128], rhs=kvs[:, c, h * 64:(h + 1) * 64], start=False, stop=True),
                               reads=[recb, Bkv], writes=[PB[2]])
                        yield
                        po = ps[:, 2, :].rearrange("p (h d) -> p h d", d=64)
                        op("dve", lambda e: e.tensor_reduce(out=gs[:, 0, :], in_=po, axis=AX.X, op=ALU.add), reads=[PB[2]], writes=[Bgn])
                        op("dve", lambda e: e.tensor_scalar(gs[:, 1, :], gs[:, 0, :], -1.0 / 64, None, op0=ALU.mult), reads=[Bgn], writes=[Bgn])
                        op("dve", lambda e: e.tensor_tensor(out=gn[:, 0], in0=po, in1=bc_mid(gs[:, 1, :], 64), op=ALU.add), reads=[PB[2], Bgn], writes=[Bgn])
                        op("dve", lambda e: e.tensor_tensor(out=gn[:, 1], in0=gn[:, 0], in1=gn[:, 0], op=ALU.mult), reads=[Bgn], writes=[Bgn])
                        op("dve", lambda e: e.tensor_reduce(out=gs[:, 2, :], in_=gn[:, 1], axis=AX.X, op=ALU.add), reads=[Bgn], writes=[Bgn])
                        op("act", lambda e: e.activation(out=gs[:, 3, :], in_=gs[:, 2, :], func=AF.Ln, scale=1.0 / 64, bias=epst[:, 0:1]), reads=[Bgn], writes=[Bgn])
                        yield
                        op("act", lambda e: e.activation(out=gs[:, 2, :], in_=gs[:, 3, :], func=AF.Exp, scale=-0.5), reads=[Bgn], writes=[Bgn])
                        op("dve", lambda e: e.tensor_tensor(out=gn[:, 2], in0=gn[:, 0], in1=bc_mid(gs[:, 2, :], 64), op=ALU.mult), reads=[Bgn], writes=[Bgn])
                        yb_, ybb = yr.next()
                        op("dve", lambda e: e.tensor_tensor(out=yb_[:], in0=gn[:, 2].rearrange("p h d -> p (h d)"), in1=rec[:, 2560:3072], op=ALU.mult), reads=[Bgn, recb], writes=[ybb])
                        p3 = psb16(3)
                        for j in range(4):
                            op("pe", lambda e: e.transpose(out=p3[:, j * 128:(j + 1) * 128], in_=yb_[:, j * 128:(j + 1) * 128], identity=identb[:]), reads=[ybb, B_const], writes=[PB[3]])
                        op("act", lambda e: e.activation(out=cat[:, 4:8, :].rearrange("p a t -> p (a t)"), in_=p3[:, 0:512], func=AF.Copy), reads=[PB[3]], writes=[catb])
                        for hf in range(2):
                            for m in range(8):
                                op("pe", lambda e: e.matmul(ps[:, 4 + hf, :], lhsT=cat[:, m, :], rhs=wout[:, m, hf * 512:(hf + 1) * 512], start=(m == 0), stop=(m == 7)),
                                   reads=[catb, Bwo], writes=[PB[4 + hf]])
                        x1, x1b = x1r.next()
                        yield
                        op("dve", lambda e: e.tensor_tensor(out=x1[:], in0=ps[:, 4:6, :].rearrange("p a n -> p (a n)"), in1=G1[:], op=ALU.mult), reads=[PB[4], PB[5], Bbc], writes=[x1b])
                        op("dve", lambda e: e.tensor_tensor(out=x1[:], in0=x1[:], in1=xt[:], op=ALU.add), reads=[x1b, xb], writes=[x1b])
                        dma("sp", lambda e: e.dma_start(out=xmid_d[ci * 128:(ci + 1) * 128, :], in_=x1[:]), reads=[x1b])
                        if DEBUG:
                            dma("sp", lambda e: e.dma_start(out=dbg_x[ci * 128:(ci + 1) * 128, :], in_=x1[:]), reads=[x1b])
                        st.update(x1=x1, x1b=x1b)
                    def f2():
                        ci = s * NCS + c
                        is_ctx = c < NCC
                        G1, A2, B2 = bcs["c" if is_ctx else "x"]
                        x1, x1b = st["x1"], st["x1b"]
                        op("dve", lambda e: e.scalar_tensor_tensor(out=junk[:], in0=x1[:], scalar=1.0, in1=x1[:], op0=ALU.mult, op1=ALU.mult, accum_out=ssq[:, 0:1]),
                           reads=[x1b], writes=[Bj])
                        op("act", lambda e: e.activation(out=ssq[:, 1:2], in_=ssq[:, 0:1], func=AF.Ln, scale=1.0 / D, bias=epst[:, 0:1]), reads=[Bj], writes=[Bj])
                        op("act", lambda e: e.activation(out=ssq[:, 2:3], in_=ssq[:, 1:2], func=AF.Exp, scale=-0.5), reads=[Bj], writes=[Bj])
                        h2, h2b = h2r.next()
                        yield
                        op("dve", lambda e: e.scalar_tensor_tensor(out=h2[:], in0=x1[:], scalar=ssq[:, 2:3], in1=A2[:], op0=ALU.mult, op1=ALU.mult), reads=[x1b, Bj, Bbc], writes=[h2b])
                        op("dve", lambda e: e.tensor_tensor(out=h2[:], in0=h2[:], in1=B2[:], op=ALU.add), reads=[h2b, Bbc], writes=[h2b])
                        for k in range(8):
                            op("pe", lambda e: e.transpose(out=ps[:, 6 + k // 4, (k % 4) * 128:(k % 4 + 1) * 128], in_=h2[:, k * 128:(k + 1) * 128], identity=identf[:]),
                               reads=[h2b, B_const], writes=[PB[6 + k // 4]])
                        h2T, h2Tb = h2Tr.next()
                        op("act", lambda e: e.activation(out=h2T[:].rearrange("p k t -> p (k t)"), in_=ps[:, 6:8, :].rearrange("p a n -> p (a n)"), func=AF.Copy),
                           reads=[PB[6], PB[7]], writes=[h2Tb])
                        for k in range(8):
                            op("pe", lambda e: e.matmul(ps[:, 6, 0:NE], lhsT=h2T[:, k, :], rhs=rw[:, k, :], start=(k == 0), stop=(k == 7)), reads=[h2Tb, BL], writes=[PB[6]])
                        lg, lgb = lgr.next()
                        yield
                        op("dve", lambda e: e.tensor_tensor(out=lg[:], in0=ps[:, 6, 0:NE], in1=rbbc[:], op=ALU.add), reads=[PB[6], BL], writes=[lgb])
                        if DEBUG:
                            dma("sp", lambda e: e.dma_start(out=dbg_lg[ci * 128:(ci + 1) * 128, :], in_=lg[:]), reads=[lgb])
                        rt, rtb = rtr.next()
                        sm, smb = smr.next()
                        op("dve", lambda e: e.max(out=sm[:, 0:8], in_=lg[:]), reads=[lgb], writes=[smb])
                        op("dve", lambda e: e.tensor_scalar(rt[:, 0, :], lg[:], sm[:, 3:4], None, op0=ALU.is_ge), reads=[lgb, smb], writes=[rtb])
                        op("dve", lambda e: e.tensor_scalar(sm[:, 8:9], sm[:, 0:1], -1.0, None, op0=ALU.mult), reads=[smb], writes=[smb])
                        op("act", lambda e: e.activation(out=rt[:, 1, :], in_=lg[:], func=AF.Exp, bias=sm[:, 8:9], scale=1.0), reads=[lgb, smb], writes=[rtb])
                        yield
                        op("dve", lambda e: e.scalar_tensor_tensor(out=rt[:, 1, :], in0=rt[:, 1, :], scalar=1.0, in1=rt[:, 0, :], op0=ALU.mult, op1=ALU.mult, accum_out=sm[:, 9:10]),
                           reads=[rtb], writes=[rtb, smb])
                        op("dve", lambda e: e.reciprocal(out=sm[:, 10:11], in_=sm[:, 9:10]), reads=[smb], writes=[smb])
                        op("dve", lambda e: e.tensor_scalar(Gd[:, ci, :], rt[:, 1, :], sm[:, 10:11], None, op0=ALU.mult), reads=[rtb, smb], writes=[Brt_])
                        mb_, mbb = mbr.next()
                        op("dve", lambda e: e.tensor_copy(out=mb_[:], in_=rt[:, 0, :]), reads=[rtb], writes=[mbb])
                        op("pe", lambda e: e.matmul(ps[:, 7, 32:64], lhsT=ustr[:], rhs=mb_[:], start=True, stop=True), reads=[mbb, B_const], writes=[PB[7]])
                        op("pe", lambda e: e.matmul(ps[:, 7, 64:96], lhsT=onesb[:], rhs=mb_[:], start=True, stop=True), reads=[mbb, B_const], writes=[PB[7]])
                        yield
                        op("dve", lambda e: e.tensor_tensor(out=rt[:, 2, :], in0=ps[:, 7, 32:64], in1=basecap[:], op=ALU.add), reads=[PB[7], Brt_], writes=[rtb])
                        op("dve", lambda e: e.tensor_tensor(out=basecap[:], in0=ps[:, 7, 64:96], in1=basecap[:], op=ALU.add), reads=[PB[7], Brt_], writes=[Brt_])
                        for k4 in range(4):
                            op("dve", lambda e: e.scalar_tensor_tensor(out=rt[:, 3, :], in0=lg[:], scalar=sm[:, k4:k4 + 1], in1=rt[:, 2, :], op0=ALU.is_equal, op1=ALU.mult,
                                                                       accum_out=poskf[:, ci, k4:k4 + 1]), reads=[lgb, smb, rtb, Brt_], writes=[rtb, Brt_])
                            op("dve", lambda e: e.scalar_tensor_tensor(out=rt[:, 3, :], in0=lg[:], scalar=sm[:, k4:k4 + 1], in1=rowc[:, 1, 0:NE], op0=ALU.is_equal, op1=ALU.mult,
                                                                       accum_out=ekf[:, ci, k4:k4 + 1]), reads=[lgb, smb, rtb, Brt_], writes=[rtb, Brt_])
                            op("dve", lambda e: e.scalar_tensor_tensor(out=rt[:, 3, :], in0=lg[:], scalar=sm[:, k4:k4 + 1], in1=Gd[:, ci, :], op0=ALU.is_equal, op1=ALU.mult,
                                                                       accum_out=gatek[:, ci, k4:k4 + 1]), reads=[lgb, smb, rtb, Brt_], writes=[rtb, Brt_])
                        h2b_, h2bb = h2br.next()
                        op("act", lambda e: e.activation(out=h2b_[:], in_=h2[:], func=AF.Copy), reads=[h2b], writes=[h2bb])
                        dma("sp", lambda e: e.dma_start(out=h2d_d[ci * 128:(ci + 1) * 128, :], in_=h2b_[:]), reads=[h2bb])
                    return fL, f1, f2
                fB = [mkB(c) for c in chunks]
                nB = len(fB)
                for i in range(nB + 2):
                    if i < nB:
                        fB[i][0]()
                    gens = []
                    if 0 <= i - 1 < nB:
                        gens.append(fB[i - 1][1]())
                    if 0 <= i - 2 < nB:
                        gens.append(fB[i - 2][2]())
                    while gens:
                        for g_ in list(gens):
                            if next(g_, "done") == "done":
                                gens.remove(g_)
                fw.barrier()
            sq.close()
        ls.close()
        if STOP_AFTER in ("passA", "scan", "fourier", "mixer0"):
            lt.close()
            break

        conv_step(10000)
        idxW = sb("idxW", [128, NSB, 3], I32, scope=lt)
        idxB = sb("idxB", [128, NSB], I32, scope=lt)
        Bsch = Buf("sched")
        with ExitStack() as sc:
            cnt = sb("cnt", [128, NE], scope=sc)
            nblk = sb("nblk", [128, NE], scope=sc)
            cum = sb("cum", [128, NE], scope=sc)
            cumex = sb("cumex", [128, NE], scope=sc)
            ones32 = sb("ones32", [128, NE], scope=sc)
            cmp17 = sb("cmp17", [128, NE, NT], scope=sc)
            cmpb = sb("cmpb", [128, NSB, NE], scope=sc)
            ebt = sb("ebt", [128, 4, NSB], scope=sc)
            idf = sb("idf", [128, 1, NSB, 4], scope=sc)
            cmpd = sb("cmpd", [128, NTOKC * 4, NE], scope=sc)
            destf = sb("destf", [128, NTOKC * 4], scope=sc)
            bvals = rowc[:, 3, 0:NSB]
            op("dve", lambda e: e.tensor_copy(out=cnt[:], in_=basecap[:]), reads=[Brt_, B_const], writes=[Bsch])
            op("dve", lambda e: e.tensor_tensor(out=cmp17[:], in0=bc_mid(cnt[:], NT), in1=rowc[:, 2, 0:NT].unsqueeze(1).to_broadcast([128, NE, NT]), op=ALU.is_gt), reads=[Bsch], writes=[Bsch])
            op("dve", lambda e: e.tensor_reduce(out=nblk[:], in_=cmp17[:], axis=AX.X, op=ALU.add), reads=[Bsch], writes=[Bsch])
            op("dve", lambda e: e.memset(ones32[:], 1.0), writes=[Bsch])
            op("dve", lambda e: e.tensor_tensor_scan(out=cum[:], data0=ones32[:], data1=nblk[:], initial=0.0, op0=ALU.mult, op1=ALU.add), reads=[Bsch], writes=[Bsch])
            op("dve", lambda e: e.tensor_tensor(out=cumex[:], in0=cum[:], in1=nblk[:], op=ALU.subtract), reads=[Bsch], writes=[Bsch])
            op("dve", lambda e: e.tensor_tensor(out=cmpb[:], in0=cum[:].unsqueeze(1).to_broadcast([128, NSB, NE]), in1=bc_mid(bvals, NE), op=ALU.is_le), reads=[Bsch], writes=[Bsch])
            op("dve", lambda e: e.tensor_reduce(out=ebt[:, 0, :], in_=cmpb[:], axis=AX.X, op=ALU.add), reads=[Bsch], writes=[Bsch])
            op("dve", lambda e: e.tensor_scalar(ebt[:, 0, :], ebt[:, 0, :], float(NE - 1), None, op0=ALU.min), reads=[Bsch], writes=[Bsch])
            op("dve", lambda e: e.tensor_scalar(ebt[:, 3, :], ebt[:, 0, :], 128.0, pcol[:, 2:3], op0=ALU.mult, op1=ALU.add), reads=[Bsch], writes=[Bsch])
            op("dve", lambda e: e.memset(ebt[:, 1, :], 0.0), writes=[Bsch])
            op("dve", lambda e: e.tensor_tensor(out=ebt[:, 1, 2:NSB], in0=ebt[:, 0, 2:NSB], in1=ebt[:, 0, 0:NSB - 2], op=ALU.is_equal), reads=[Bsch], writes=[Bsch])
            for m in range(3):
                op("dve", lambda e: e.scalar_tensor_tensor(out=idf[:, 0, :, m], in0=ebt[:, 1, :], scalar=1.0e6, in1=ebt[:, 3, :], op0=ALU.mult, op1=ALU.add), reads=[Bsch], writes=[Bsch])
                if m > 0:
                    op("dve", lambda e: e.tensor_scalar(idf[:, 0, :, m], idf[:, 0, :, m], float(m * NE * 128), None, op0=ALU.add), reads=[Bsch], writes=[Bsch])
            op("dve", lambda e: e.tensor_copy(out=idxW[:], in_=idf[:, 0, :, 0:3]), reads=[Bsch], writes=[Bsch])
            if l > 0:
                op("dve", lambda e: e.tensor_scalar(ebt[:, 3, :], ebt[:, 3, :], float(l * NE * 128), None, op0=ALU.add), reads=[Bsch], writes=[Bsch])
            op("dve", lambda e: e.tensor_copy(out=idxB[:], in_=ebt[:, 3, :]), reads=[Bsch], writes=[Bsch])
            op("dve", lambda e: e.tensor_scalar(cumex[:], cumex[:], float(BS), None, op0=ALU.mult), reads=[Bsch], writes=[Bsch])
            NA = NTOKC * 4
            ekflat = ekf[:].rearrange("p c k -> p (c k)")
            op("dve", lambda e: e.tensor_tensor(out=cmpd[:], in0=rowc[:, 1, 0:NE].unsqueeze(1).to_broadcast([128, NA, NE]), in1=bc_mid(ekflat, NE), op=ALU.is_equal), reads=[Bsch, Brt_], writes=[Bsch])
            op("dve", lambda e: e.tensor_tensor(out=cmpd[:], in0=cmpd[:], in1=cumex[:].unsqueeze(1).to_broadcast([128, NA, NE]), op=ALU.mult), reads=[Bsch], writes=[Bsch])
            op("dve", lambda e: e.tensor_reduce(out=destf[:], in_=cmpd[:], axis=AX.X, op=ALU.add), reads=[Bsch], writes=[Bsch])
            op("dve", lambda e: e.tensor_tensor(out=destf[:], in0=destf[:], in1=poskf[:].rearrange("p c k -> p (c k)"), op=ALU.add), reads=[Bsch, Brt_], writes=[Bsch])
            op("dve", lambda e: e.tensor_copy(out=destk[:].rearrange("p c k -> p (c k)"), in_=destf[:]), reads=[Bsch], writes=[Brt_])
            if DEBUG:
                dma("sp", lambda e: e.dma_start(out=dbg_sched[:, 0:NE], in_=cnt[:]), reads=[Bsch])
                dma("sp", lambda e: e.dma_start(out=dbg_sched[:, NE:NE + NSB], in_=ebt[:, 0, :]), reads=[Bsch])
                dma("sp", lambda e: e.dma_start(out=dbg_sched[:, NE + NSB:NE + NSB + 64], in_=destf[:, 0:64]), reads=[Bsch])
            fw.barrier()
        with ExitStack() as dp:
            hdr = Ring(nc, dp, "h2ld", 3, [128, D], BF16)
            for (s_, c_) in lchunks:
                ci = s_ * NCS + c_
                hd, hdb = hdr.next()
                dma("sp", lambda e: e.dma_start(out=hd[:], in_=h2d_d[ci * 128:(ci + 1) * 128, :]), writes=[hdb])
                for k4 in range(4):
                    dma("pool", lambda e: e.indirect_dma_start(out=hbuf_d[:, :], out_offset=bass.IndirectOffsetOnAxis(ap=destk[:, ci, k4:k4 + 1], axis=0),
                                                               in_=hd[:, :], in_offset=None), reads=[hdb, Brt_])
            fw.barrier()
        IOA = bass.IndirectOffsetOnAxis
        with ExitStack() as mo:
            wr = Ring(nc, mo, "wexp", 2, [128, 3, 4, 2048], BF16)
            b1r = Ring(nc, mo, "b1t", 3, [128, 24], F32)
            hrr = Ring(nc, mo, "hrows", 2, [128, GB, D], BF16)
            hTr = Ring(nc, mo, "hTm", 2, [128, 8, BS], BF16)
            atr = Ring(nc, mo, "actT", 2, [128, 8, BS], BF16)
            xgr = Ring(nc, mo, "xg", 2, [128, 4, BS], F32)
            osr = Ring(nc, mo, "ostage", 3, [128, D], F32)
            wsrc = (w1g_d, w1l_d, w2_d)
            state = {}

            def load_sb(b):
                b1t, b1b = b1r.next()
                dma("pool", lambda e: e.indirect_dma_start(out=b1t[:, 0:16], out_offset=None, in_=b1T_d.rearrange("l r f -> (l r) f"), in_offset=IOA(ap=idxB[:, b:b + 1], axis=0)), reads=[Bsch], writes=[b1b])
                W, Wb = wr.next()
                for m in range(3):
                    dma("pool", lambda e: e.indirect_dma_start(out=W[:, m].rearrange("p q f -> p (q f)"), out_offset=None, in_=wbf_d[:, :], in_offset=IOA(ap=idxW[:, b, m:m + 1], axis=0),
                                                               bounds_check=bc_reg, oob_is_err=False),
                        reads=[Bsch, Bwbf], writes=[Wb])
                hr, hrb = hrr.next()
                dma("sp", lambda e: e.dma_start(out=hr[:], in_=hbuf_d[b * BS:(b + 1) * BS, :].rearrange("(g p) d -> p g d", p=128)), writes=[hrb])
                state[b] = dict(W=W, Wb=Wb, b1t=b1t, b1b=b1b, hr=hr, hrb=hrb)

            def transp_sb(b):
                st = state[b]
                hr, hrb = st["hr"], st["hrb"]
                hT, hTb = hTr.next()
                for g in range(GB):
                    pt = psb16(g % 2)
                    for k in range(8):
                        op("pe", lambda e: e.transpose(out=pt[:, k * 128:(k + 1) * 128], in_=hr[:, g, k * 128:(k + 1) * 128], identity=identb[:]), reads=[hrb, B_const], writes=[PB[g % 2]])
                    op("act", lambda e: e.activation(out=hT[:, :, g * 128:(g + 1) * 128], in_=pt.rearrange("p (k t) -> p k t", t=128), func=AF.Copy),
                       reads=[PB[g % 2]], writes=[hTb])
                st.update(hT=hT, hTb=hTb)

            def gemm1_sb(b):
                st = state[b]
                W, Wb, b1t, b1b, hT, hTb = st["W"], st["Wb"], st["b1t"], st["b1b"], st["hT"], st["hTb"]
                aT, aTb = atr.next()
                Wv = W[:].rearrange("p m q (k f) -> p m (q k) f", f=1024)
                op("dve", lambda e: e.tensor_scalar(b1t[:, 16:24], b1t[:, 8:16], 1.0, None, op0=ALU.add), reads=[b1b], writes=[b1b])
                for fc in range(8):
                    bg, bl = 2 + (fc % 2) * 2, 3 + (fc % 2) * 2
                    for k in range(8):
                        op("pe", lambda e: e.matmul(ps[:, bg, 0:BS], lhsT=Wv[:, 0, k, fc * 128:(fc + 1) * 128], rhs=hT[:, k, :], start=(k == 0), stop=(k == 7)), reads=[Wb, hTb], writes=[PB[bg]])
                    for k in range(8):
                        op("pe", lambda e: e.matmul(ps[:, bl, 0:BS], lhsT=Wv[:, 1, k, fc * 128:(fc + 1) * 128], rhs=hT[:, k, :], start=(k == 0), stop=(k == 7)), reads=[Wb, hTb], writes=[PB[bl]])
                    xg, xgb = xgr.next()
                    op("dve", lambda e: e.tensor_scalar(xg[:, 0, :], ps[:, bg, 0:BS], b1t[:, fc:fc + 1], 7.0, op0=ALU.add, op1=ALU.min), reads=[PB[bg], b1b], writes=[xgb])
                    op("act", lambda e: e.activation(out=xg[:, 1, :], in_=xg[:, 0, :], func=AF.Sigmoid, scale=1.702), reads=[xgb], writes=[xgb])
                    op("dve", lambda e: e.tensor_scalar(xg[:, 2, :], ps[:, bl, 0:BS], b1t[:, 16 + fc:17 + fc], 8.0, op0=ALU.add, op1=ALU.min), reads=[PB[bl], b1b], writes=[xgb])
                    op("dve", lambda e: e.scalar_tensor_tensor(out=xg[:, 3, :], in0=xg[:, 2, :], scalar=-6.0, in1=xg[:, 0, :], op0=ALU.max, op1=ALU.mult), reads=[xgb], writes=[xgb])
                    op("dve", lambda e: e.tensor_tensor(out=aT[:, fc, :], in0=xg[:, 3, :], in1=xg[:, 1, :], op=ALU.mult), reads=[xgb], writes=[aTb])
                st.update(aT=aT, aTb=aTb, Wv=Wv)

            def gemm2_sb(b):
                st = state[b]
                aT, aTb, Wv, Wb = st["aT"], st["aTb"], st["Wv"], st["Wb"]
                obanks = (6, 2, 4, 6)
                for g in range(GB):
                    ob = obanks[g]
                    for hf in range(2):
                        for fc in range(8):
                            op("pe", lambda e: e.matmul(ps[:, ob + hf, :], lhsT=aT[:, fc, g * 128:(g + 1) * 128], rhs=Wv[:, 2, fc, hf * 512:(hf + 1) * 512], start=(fc == 0), stop=(fc == 7)),
                               reads=[aTb, Wb], writes=[PB[ob + hf]])
                    ost, osb = osr.next()
                    op("act", lambda e: e.activation(out=ost[:], in_=ps[:, ob:ob + 2, :].rearrange("p a n -> p (a n)"), func=AF.Copy), reads=[PB[ob], PB[ob + 1]], writes=[osb])
                    dma("sp", lambda e: e.dma_start(out=obuf_d[b * BS + g * 128:b * BS + (g + 1) * 128, :], in_=ost[:]), reads=[osb])
                del state[b]

            load_sb(0)
            transp_sb(0)
            for b in range(NSB):
                if b + 1 < NSB:
                    load_sb(b + 1)
                gemm1_sb(b)
                if b + 1 < NSB:
                    transp_sb(b + 1)
                gemm2_sb(b)
            fw.barrier()
        last = (l == DEPTH - 1)
        with ExitStack() as cb:
            b2t = sb("b2t", [NE, D], scope=cb)
            Bc = Buf("cmbconst")
            dma("sp", lambda e: e.dma_start(out=b2t[:], in_=b2_d[l]), writes=[Bc])
            G2 = {}
            for col in list(range(NSEQ)) + ([2] if has_ctx_out else []):
                G2[col] = sb(f"G2bc{col}", [128, D], scope=cb)
                dma("sp", lambda e: e.dma_start(out=G2[col][:], in_=modrow_d[col:col + 1, 3 * D:4 * D].partition_broadcast(128)), writes=[Bc])
            if last:
                fnw = sb("fnw", [128, D], scope=cb)
                dma("sp", lambda e: e.dma_start(out=fnw[:], in_=fnwbc_d), writes=[Bc])
            x1r = Ring(nc, cb, "x1c", 2, [128, D], F32)
            rwr = Ring(nc, cb, "orow", 2, [128, 4, D], F32)
            GTr = Ring(nc, cb, "GT", 2, [NE, 128], F32)
            accr = Ring(nc, cb, "acc", 2, [128, D], F32)
            junk = sb("junkC", [128, D], BF16, scope=cb)
            ssq = sb("ssqC", [128, 4], scope=cb)
            Bj = Buf("junkC")
            for (s, c) in lchunks:
                ci = s * NCS + c
                is_ctx = c < NCC
                col = 2 if is_ctx else s
                x1, x1b = x1r.next()
                dma("sp", lambda e: e.dma_start(out=x1[:], in_=xmid_d[ci * 128:(ci + 1) * 128, :]), writes=[x1b])
                orow, orb = rwr.next()
                for k4 in range(4):
                    dma("pool", lambda e: e.indirect_dma_start(out=orow[:, k4, :], out_offset=None, in_=obuf_d[:, :], in_offset=IOA(ap=destk[:, ci, k4:k4 + 1], axis=0)),
                        reads=[Brt_], writes=[orb])
                op("pe", lambda e: e.transpose(out=ps[0:NE, 0, 0:128], in_=Gd[:, ci, :], identity=identf[:]), reads=[Brt_, B_const], writes=[PB[0]])
                GT, GTb = GTr.next()
                op("act", lambda e: e.activation(out=GT[:], in_=ps[0:NE, 0, 0:128], func=AF.Copy), reads=[PB[0]], writes=[GTb])
                for hf in range(2):
                    op("pe", lambda e: e.matmul(ps[:, 1 + hf, :], lhsT=GT[:], rhs=b2t[:, hf * 512:(hf + 1) * 512], start=True, stop=True), reads=[GTb, Bc], writes=[PB[1 + hf]])
                acc, accb = accr.next()
                op("dve", lambda e: e.scalar_tensor_tensor(out=acc[:], in0=orow[:, 0, :], scalar=gatek[:, ci, 0:1], in1=ps[:, 1:3, :].rearrange("p a n -> p (a n)"), op0=ALU.mult, op1=ALU.add),
                   reads=[orb, Brt_, PB[1], PB[2]], writes=[accb])
                for k4 in range(1, 4):
                    op("dve", lambda e: e.scalar_tensor_tensor(out=acc[:], in0=orow[:, k4, :], scalar=gatek[:, ci, k4:k4 + 1], in1=acc[:], op0=ALU.mult, op1=ALU.add),
                       reads=[orb, Brt_, accb], writes=[accb])
                op("dve", lambda e: e.tensor_tensor(out=acc[:], in0=acc[:], in1=G2[col][:], op=ALU.mult), reads=[accb, Bc], writes=[accb])
                op("dve", lambda e: e.tensor_tensor(out=acc[:], in0=acc[:], in1=x1[:], op=ALU.add), reads=[accb, x1b], writes=[accb])
                if DEBUG:
                    dma("sp", lambda e: e.dma_start(out=dbg_x2[ci * 128:(ci + 1) * 128, :], in_=acc[:]), reads=[accb])
                if last:
                    op("dve", lambda e: e.scalar_tensor_tensor(out=junk[:], in0=acc[:], scalar=1.0, in1=acc[:], op0=ALU.mult, op1=ALU.mult, accum_out=ssq[:, 0:1]), reads=[accb], writes=[Bj])
                    op("act", lambda e: e.activation(out=ssq[:, 1:2], in_=ssq[:, 0:1], func=AF.Ln, scale=1.0 / D, bias=epst[:, 0:1]), reads=[Bj], writes=[Bj])
                    op("act", lambda e: e.activation(out=ssq[:, 2:3], in_=ssq[:, 1:2], func=AF.Exp, scale=-0.5), reads=[Bj], writes=[Bj])
                    op("dve", lambda e: e.scalar_tensor_tensor(out=acc[:], in0=acc[:], scalar=ssq[:, 2:3], in1=fnw[:], op0=ALU.mult, op1=ALU.mult), reads=[accb, Bj, Bc], writes=[accb])
                    dma("sp", lambda e: e.dma_start(out=out_d[s, (c - NCC) * 128:(c - NCC + 1) * 128, :], in_=acc[:]), reads=[accb])
                else:
                    dma("sp", lambda e: e.dma_start(out=xnext_d[ci * 128:(ci + 1) * 128, :], in_=acc[:]), reads=[accb])
            fw.barrier()
        lt.close()
        if STOP_AFTER is not None:
            break

    fw.barrier(engines=("sp",))
    es.close()
    return nc


def make_constants(nseq=2):
    bf = ml_dtypes.bfloat16
    k = np.arange(T, dtype=np.float64)
    ang = 2 * np.pi * np.outer(k, k) / T
    C = (np.cos(ang) / 64.0)
    S = (-np.sin(ang) / 64.0)
    def lay(M):
        M = M.reshape(4, 8, 128, 16, 256)
        return M.transpose(3, 0, 2, 1, 4)
    dft = np.stack([lay(C), lay(S)], axis=3)
    dft = np.ascontiguousarray(dft.reshape(16, 4, 128, 2 * 8 * 256)).astype(bf)
    kc = np.arange(TC, dtype=np.float64)
    angc = 2 * np.pi * np.outer(kc, kc) / TC
    Cc = (np.cos(angc) / 16.0).reshape(2, 128, 256).transpose(1, 0, 2)
    Sc = (-np.sin(angc) / 16.0).reshape(2, 128, 256).transpose(1, 0, 2)
    dftc = np.ascontiguousarray(np.stack([Cc, Sc], axis=1).reshape(128, 2 * 2 * 256)).astype(bf)
    m = np.arange(64, dtype=np.float64)
    a64 = 2 * np.pi * np.outer(m, m) / 64
    bd = np.zeros((128, 2, 128), np.float32)
    for g in range(2):
        bd[g * 64:(g + 1) * 64, 0, g * 64:(g + 1) * 64] = np.cos(a64) / 8.0
        bd[g * 64:(g + 1) * 64, 1, g * 64:(g + 1) * 64] = np.sin(a64) / 8.0
    t = np.arange(T)
    row = (t // 64).astype(np.float32)
    colp = (t % 64).astype(np.float32)
    freqs = (np.float32(10000.0) ** (-np.arange(16, dtype=np.float32) / np.float32(16))).astype(np.float32)
    angr = np.concatenate([row[:, None] * freqs, colp[:, None] * freqs], -1).astype(np.float32)
    cs = np.cos(angr).reshape(NCX, 128, 32).transpose(1, 0, 2)
    sn = np.sin(angr).reshape(NCX, 128, 32).transpose(1, 0, 2)
    rope = np.ascontiguousarray(np.stack([cs, sn], axis=1)).astype(np.float32)
    j = np.arange(128)[:, None]
    i = np.arange(128)[None, :]
    tri = np.stack([np.maximum(i - j, 0), np.maximum(j - i, 0), (i >= j) * 0.125, (j >= i) * 0.125], axis=1).astype(np.float32)
    p = np.arange(128, dtype=np.float32)
    pcol = np.stack([127 - p, p + 1, p, 128 - p], axis=1).astype(np.float32)
    capmax = 2048 * nseq + 512
    rowc = np.zeros((128, 4, 256), np.float32)
    rowc[:, 0, :32] = np.arange(32) * capmax
    rowc[:, 1, :32] = np.arange(32)
    rowc[:, 2, :] = np.arange(256) * 512
    rowc[:, 3, :] = np.arange(256)
    ustrict = (np.arange(128)[:, None] < np.arange(128)[None, :]).astype(np.float32).astype(bf)
    return dict(rowc=rowc, ustrict=ustrict, dft=dft, dftc=dftc, bd64=bd, rope=rope, tri=np.ascontiguousarray(tri), pcol=pcol,
                identb=np.eye(128, dtype=np.float32).astype(bf), identf=np.eye(128, dtype=np.float32))


def prep_shared(inp, nseq=2):
    f = lambda a: np.ascontiguousarray(np.asarray(a, dtype=np.float32))
    sh = {}
    sh["mod_w"] = f(inp["mod_w"])
    mod_b = f(inp["mod_b"])
    sh["mod_b"] = mod_b
    sh["mod_bT"] = np.ascontiguousarray(mod_b.reshape(DEPTH, 48, 128).transpose(2, 0, 1))
    nw = f(inp["norm_w"])
    sh["nwT"] = np.ascontiguousarray(nw.reshape(DEPTH, 2, 8, 128).transpose(3, 0, 1, 2))
    sh["nw1_bc"] = np.ascontiguousarray(np.broadcast_to(nw[:, 1, :][None], (128, DEPTH, D)))
    w_in = f(inp["w_in"])
    sh["wfT"] = np.ascontiguousarray(w_in[:, :, :512].transpose(0, 2, 1))
    sh["w_qkvg"] = np.ascontiguousarray(w_in[:, :, 512:])
    sh["w_out"] = f(inp["w_out"])
    sh["rd_bc"] = np.ascontiguousarray(np.broadcast_to(f(inp["ret_decay"]).reshape(1, DEPTH, 16), (128, DEPTH, 16)))
    sh["rnw_bc"] = np.ascontiguousarray(np.broadcast_to(f(inp["ret_norm_w"])[None], (128, DEPTH, 512)))
    sh["router_w"] = f(inp["router_w"])
    sh["rb_bc"] = np.ascontiguousarray(np.broadcast_to(f(inp["router_b"])[None], (128, DEPTH, NE)))
    sh["fnw_bc"] = np.ascontiguousarray(np.broadcast_to(f(inp["final_norm_w"])[None], (128, D)))
    w1 = np.asarray(inp["expert_w1"], dtype=np.float32)
    def wlay(w):
        L, E = w.shape[0], w.shape[1]
        return np.ascontiguousarray(w.reshape(L, E, 8, 128, 1024).transpose(0, 1, 3, 2, 4)).reshape(L, E * 128 * 4, 2048)
    sh["w1g"] = wlay(w1[..., 0::2])
    sh["w1l"] = wlay(w1[..., 1::2])
    sh["w2h"] = wlay(np.asarray(inp["expert_w2"], dtype=np.float32))
    b1 = np.asarray(inp["expert_b1"], dtype=np.float32)
    b1g = b1[..., 0::2].reshape(DEPTH, NE, 8, 128).transpose(0, 1, 3, 2)
    b1l = b1[..., 1::2].reshape(DEPTH, NE, 8, 128).transpose(0, 1, 3, 2)
    sh["b1T"] = np.ascontiguousarray(np.concatenate([b1g, b1l], axis=-1)).reshape(DEPTH, NE * 128, 16)
    sh["b2"] = f(inp["expert_b2"])
    sh.update(make_constants(nseq))
    return sh


def prep_core(inp, sh, core, nseq=2):
    f = lambda a: np.ascontiguousarray(np.asarray(a, dtype=np.float32))
    b0 = core * 2
    m = dict(sh)
    m["x"] = f(inp["x"][b0:b0 + nseq])
    m["ctx"] = f(inp["ctx"][b0:b0 + nseq])
    c = np.asarray(inp["c"], dtype=np.float32)
    cols = [c[b0], c[b0 + 1], np.asarray(inp["c_ctx"], dtype=np.float32)]
    m["cT"] = np.ascontiguousarray(np.stack(cols, axis=1).reshape(8, 128, 3).transpose(1, 0, 2))
    return m


_NC_CACHE = {}


def kernel(**inputs):
    cfg = dict(nseq=2, layers=DEPTH)
    key = "full"
    if key not in _NC_CACHE:
        _NC_CACHE[key] = build_program(cfg)
    nc = _NC_CACHE[key]
    sh = prep_shared(inputs)
    in_maps = [prep_core(inputs, sh, core) for core in range(8)]
    res = run_bass_kernel_spmd(nc, in_maps, core_ids=list(range(8)))
    out = np.concatenate([np.asarray(r["out"], dtype=np.float32) for r in res.results], axis=0)
    return out
```

```python
import numpy as np
import ml_dtypes
from contextlib import ExitStack
import concourse.bass as bass
import concourse.mybir as mybir
from concourse.bass_utils import run_bass_kernel_spmd

F32 = mybir.dt.float32
BF16 = mybir.dt.bfloat16
U32 = mybir.dt.uint32
I32 = mybir.dt.int32
ALU = mybir.AluOpType
AF = mybir.ActivationFunctionType
AX = mybir.AxisListType

D = 1024
T = 4096
TC = 256
NCX = T // 128
NCC = TC // 128
NCS = NCX + NCC
NE = 32
DEPTH = 2
EPS = 1e-6


class Tok:
    __slots__ = ("sem", "val", "key", "owner")

    def __init__(self, sem, val, key, owner):
        self.sem, self.val, self.key, self.owner = sem, val, key, owner


class Buf:
    __slots__ = ("name", "w", "r")

    def __init__(self, name=""):
        self.name = name
        self.w = {}
        self.r = {}


class EngS:
    def __init__(self, name, e, sem):
        self.name, self.e, self.sem = name, e, sem
        self.count = 0
        self.waited = {}


class FW:
    def __init__(self, nc, es, n_dma_sems=12):
        self.nc = nc
        self.engs = {}
        for name, e in (("pe", nc.tensor), ("act", nc.scalar), ("dve", nc.vector),
                        ("pool", nc.gpsimd), ("sp", nc.sync)):
            sem = es.enter_context(nc.semaphore("sem_" + name))
            self.engs[name] = EngS(name, e, sem)
        self.dsems = {}
        for q in ("sp", "pool", "act", "pe"):
            lst = []
            for i in range(n_dma_sems):
                s = es.enter_context(nc.semaphore(f"dsem_{q}{i}"))
                lst.append([s, 0, None])
            self.dsems[q] = [lst, 0]

    def wait(self, E, tok):
        if tok is None:
            return
        if E.waited.get(tok.key, 0) >= tok.val:
            return
        E.e.wait_ge(tok.sem, tok.val)
        E.waited[tok.key] = tok.val

    def _deps(self, reads, writes):
        deps = []
        for b in reads:
            deps.extend(b.w.values())
        for b in writes:
            deps.extend(b.w.values())
            deps.extend(b.r.values())
        return deps

    def op(self, en, fn, reads=(), writes=()):
        E = self.engs[en]
        for tok in self._deps(reads, writes):
            if en == "pe" and tok.owner == "pe":
                continue
            self.wait(E, tok)
        ins = fn(E.e)
        E.count += 1
        ins.then_inc(E.sem, 1)
        tok = Tok(E.sem, E.count, "E" + en, en)
        E.waited[tok.key] = max(E.waited.get(tok.key, 0), 0)
        for b in reads:
            b.r[tok.key] = tok
        for b in writes:
            b.w = {tok.key: tok}
            b.r = {}
        return tok

    def dma(self, q, fn, reads=(), writes=()):
        E = self.engs[q]
        for tok in self._deps(reads, writes):
            self.wait(E, tok)
        lst, idx = self.dsems[q]
        ent = lst[idx % len(lst)]
        self.dsems[q][1] = idx + 1
        if ent[2] is not None:
            self.wait(E, ent[2])
        ins = fn(E.e)
        ent[1] += 16
        ins.then_inc(ent[0], 16)
        tok = Tok(ent[0], ent[1], f"D{q}{idx % len(lst)}", "dma")
        ent[2] = tok
        for b in reads:
            b.r[tok.key] = tok
        for b in writes:
            if all(t.owner == "dma" for t in b.w.values()):
                b.w[tok.key] = tok
            else:
                b.w = {tok.key: tok}
            b.r = {}
        return tok

    def all_toks(self):
        toks = []
        for E in self.engs.values():
            if E.count > 0:
                toks.append(Tok(E.sem, E.count, "E" + E.name, E.name))
        for q, (lst, _) in self.dsems.items():
            for ent in lst:
                if ent[2] is not None:
                    toks.append(ent[2])
        return toks

    def barrier(self, engines=("pe", "act", "dve", "pool", "sp")):
        toks = self.all_toks()
        for en in engines:
            E = self.engs[en]
            for t in toks:
                if t.owner == en:
                    continue
                self.wait(E, t)


_UID = [0]


class Ring:
    def __init__(self, nc, es, name, n, shape, dtype):
        self.tiles = []
        for i in range(n):
            _UID[0] += 1
            self.tiles.append(es.enter_context(nc.sbuf_tensor(f"{name}{i}_u{_UID[0]}", shape, dtype)))
        self.bufs = [Buf(f"{name}{i}") for i in range(n)]
        self.i = 0

    def next(self):
        k = self.i % len(self.tiles)
        self.i += 1
        return self.tiles[k], self.bufs[k]


def bc_mid(ap2, n):
    P, A = ap2.shape
    return ap2.unsqueeze(2).to_broadcast([P, A, n])


def build_program(cfg):
    NSEQ = cfg.get("nseq", 2)
    LAYERS = cfg.get("layers", DEPTH)
    DEBUG = cfg.get("debug", False)
    STOP_AFTER = cfg.get("stop_after", None)
    NTOKC = NSEQ * NCS
    PBS = cfg.get("pb_stop", 99)

    nc = bass.Bass("TRN2", target_bir_lowering=False)
    es = ExitStack()
    fw = FW(nc, es)
    op, dma = fw.op, fw.dma

    def din(name, shape, dt=F32):
        return nc.dram_tensor(name, list(shape), dt, kind="ExternalInput").ap()

    def dscr(name, shape, dt=F32):
        return nc.dram_tensor(name, list(shape), dt, kind="Internal").ap()

    x_in = din("x", [NSEQ, T, D])
    ctx_in = din("ctx", [NSEQ, TC, D])
    cT_d = din("cT", [128, 8, 3])
    modw_d = din("mod_w", [DEPTH, D, 6 * D])
    modbT_d = din("mod_bT", [128, DEPTH, 48])
    modb_d = din("mod_b", [DEPTH, 6 * D])
    nwT_d = din("nwT", [128, DEPTH, 2, 8])
    nw1bc_d = din("nw1_bc", [128, DEPTH, D])
    wfT_d = din("wfT", [DEPTH, 512, D])
    wqkvg_d = din("w_qkvg", [DEPTH, D, 2048])
    wout_d = din("w_out", [DEPTH, D, D])
    rdbc_d = din("rd_bc", [128, DEPTH, 16])
    rnwbc_d = din("rnw_bc", [128, DEPTH, 512])
    rw_d = din("router_w", [DEPTH, D, NE])
    rbbc_d = din("rb_bc", [128, DEPTH, NE])
    fnwbc_d = din("fnw_bc", [128, D])
    dft_d = din("dft", [16, 4, 128, 2 * 8 * 256], BF16)
    dftc_d = din("dftc", [128, 2 * 2 * 256], BF16)
    bd64_d = din("bd64", [128, 2, 128])
    rope_d = din("rope", [128, 2, NCX, 32])
    tri_d = din("tri", [128, 4, 128])
    pcol_d = din("pcol", [128, 4])
    identb_d = din("identb", [128, 128], BF16)
    identf_d = din("identf", [128, 128])

    CAPMAX = 2048 * NSEQ + 512
    CAPB = CAPMAX // 512
    w1g_d = din("w1g", [DEPTH, NE * 128 * 4, 2048])
    w1l_d = din("w1l", [DEPTH, NE * 128 * 4, 2048])
    w2_d = din("w2h", [DEPTH, NE * 128 * 4, 2048])
    b1T_d = din("b1T", [DEPTH, NE * 128, 16])
    b2_d = din("b2", [DEPTH, NE, D])
    ustr_d = din("ustrict", [128, 128], BF16)
    rows_d = din("rowc", [128, 4, 256])
    BS = 512
    GB = BS // 128
    NSB_MAX = (NTOKC * 128 * 4) // BS + NE
    NT = (NTOKC * 128) // BS + 1
    hbuf_d = dscr("hbuf", [NSB_MAX * BS, D], BF16)
    obuf_d = dscr("obuf", [NSB_MAX * BS, D])
    h2d_d = dscr("h2d", [NTOKC * 128, D], BF16)
    wbf_d = dscr("wbf", [3 * NE * 128, 8192], BF16)
    Bwbf = Buf("wbf")
    wsrc_all = (w1g_d, w1l_d, w2_d)
    CONV_R = 256

    def conv_pieces(l):
        dst = wbf_d.rearrange("r (q f) -> (r q) f", f=2048)
        for m in range(3):
            for r0 in range(0, NE * 128 * 4, CONV_R):
                yield (lambda m=m, r0=r0: dma("pool", lambda e: e.dma_start(out=dst[m * NE * 128 * 4 + r0:m * NE * 128 * 4 + r0 + CONV_R, :],
                                                                            in_=wsrc_all[m][l, r0:r0 + CONV_R, :]), writes=[Bwbf]))
    out_d = nc.dram_tensor("out", [NSEQ, T, D], F32, kind="ExternalOutput").ap()
    if DEBUG:
        dbg_x = nc.dram_tensor("dbg_x", [NTOKC * 128, D], F32, kind="ExternalOutput").ap()
        dbg_lg = nc.dram_tensor("dbg_lg", [NTOKC * 128, NE], F32, kind="ExternalOutput").ap()
        dbg_x2 = nc.dram_tensor("dbg_x2", [NTOKC * 128, D], F32, kind="ExternalOutput").ap()
        dbg_sched = nc.dram_tensor("dbg_sched", [128, 512], F32, kind="ExternalOutput").ap()

    spill_d = dscr("spill", [NTOKC, 128, 3072], BF16)
    zs_d = dscr("zs", [NSEQ, 128, NCX, 1024], BF16)
    zc_d = dscr("zc", [NSEQ, 128, NCC, 1024], BF16)
    yts_d = dscr("yts", [NTOKC, 128, 512], BF16)
    xmid_d = dscr("xmid", [NTOKC * 128, D])
    xnext_d = dscr("xnext", [NTOKC * 128, D])
    modrow_d = dscr("modrow", [3, 4 * D])

    def sb(name, shape, dt=F32, scope=es):
        _UID[0] += 1
        return scope.enter_context(nc.sbuf_tensor(f"{name}_u{_UID[0]}", list(shape), dt))

    identb = sb("identb", [128, 128], BF16)
    identf = sb("identf", [128, 128])
    tri = sb("tri", [128, 4, 128])
    pcol = sb("pcol", [128, 4])
    bd64 = sb("bd64", [128, 2, 128])
    epst = sb("epst", [128, 1])
    cT = sb("cT", [128, 8, 3])
    B_const = Buf("const")
    dma("sp", lambda e: e.dma_start(out=identb[:], in_=identb_d), writes=[B_const])
    dma("sp", lambda e: e.dma_start(out=identf[:], in_=identf_d), writes=[B_const])
    dma("sp", lambda e: e.dma_start(out=tri[:], in_=tri_d), writes=[B_const])
    dma("sp", lambda e: e.dma_start(out=pcol[:], in_=pcol_d), writes=[B_const])
    dma("sp", lambda e: e.dma_start(out=bd64[:], in_=bd64_d), writes=[B_const])
    dma("sp", lambda e: e.dma_start(out=cT[:], in_=cT_d), writes=[B_const])
    op("dve", lambda e: e.memset(epst[:], EPS), writes=[B_const])
    ustr = sb("ustr", [128, 128], BF16)
    onesb = sb("onesb", [128, 128], BF16)
    rowc = sb("rowc", [128, 4, 256])
    dma("sp", lambda e: e.dma_start(out=ustr[:], in_=ustr_d), writes=[B_const])
    dma("sp", lambda e: e.dma_start(out=rowc[:], in_=rows_d), writes=[B_const])
    op("dve", lambda e: e.memset(onesb[:], 1.0), writes=[B_const])
    siluT = sb("siluT", [128, 8, 3])
    op("act", lambda e: e.activation(out=siluT[:], in_=cT[:], func=AF.Silu), reads=[B_const], writes=[B_const])

    bc_reg = nc.gpsimd.to_reg(3 * NE * 128 - 1)
    ps = es.enter_context(nc.psum_tensor("ps", [128, 8, 512], F32))
    PB = [Buf(f"psum{i}") for i in range(8)]

    def psb16(bank):
        return ps[:, bank, :].bitcast(BF16)

    def chunk_src(l, s, c):
        if l == 0:
            if c < NCC:
                return ctx_in[s, c * 128:(c + 1) * 128, :]
            return x_in[s, (c - NCC) * 128:(c - NCC + 1) * 128, :]
        ci = s * NCS + c
        return xnext_d[ci * 128:(ci + 1) * 128, :]

    for l in range(LAYERS):
        has_ctx_out = l < DEPTH - 1
        lt = ExitStack()
        Gd = sb("Gd", [128, NTOKC, NE], scope=lt)
        destk = sb("destk", [128, NTOKC, 4], I32, scope=lt)
        gatek = sb("gatek", [128, NTOKC, 4], scope=lt)
        basecap = sb("basecap", [128, NE], scope=lt)
        poskf = sb("poskf", [128, NTOKC, 4], scope=lt)
        ekf = sb("ekf", [128, NTOKC, 4], scope=lt)
        Brt_ = Buf("route")
        op("dve", lambda e: e.memset(basecap[:], 0.0), writes=[Brt_])
        lchunks = [(s_, c_) for s_ in range(NSEQ) for c_ in (range(NCS) if has_ctx_out else range(NCC, NCS))]
        NSB = (len(lchunks) * 128 * 4) // BS + NE
        conv_it = conv_pieces(l)

        def conv_step(n=1):
            for _ in range(n):
                fn_ = next(conv_it, None)
                if fn_ is not None:
                    fn_()
        ls = ExitStack()
        logg = sb("logg", [128, 16], scope=ls)
        rdt = sb("rdt", [128, 16], scope=ls)
        DT8 = sb("DT8", [128, 8, 128], scope=ls)
        wk = sb("wk", [128, 8, 2], scope=ls)
        wq = sb("wq", [128, 8, 2], scope=ls)
        G8 = sb("G8", [128, 8], scope=ls)
        Gbc = sb("Gbc", [128, 8, 64], scope=ls)
        modF = sb("modF", [128, 16, 3], scope=ls)
        A1 = sb("A1", [128, 8, 3], scope=ls)
        wz = sb("wz", [128, 8, 1024], BF16, scope=ls)
        rnwbc = sb("rnwbc", [128, 512], scope=ls)
        nw1bc = sb("nw1bc", [128, D], scope=ls)
        rbbc = sb("rbbc", [128, NE], scope=ls)
        rw = sb("rw", [128, 8, NE], scope=ls)
        nwT = sb("nwT", [128, 2, 8], scope=ls)
        mbT = sb("mbT", [128, 48], scope=ls)
        BL = Buf("layerconst")
        dma("sp", lambda e: e.dma_start(out=rdt[:], in_=rdbc_d[:, l, :]), writes=[BL])
        dma("sp", lambda e: e.dma_start(out=rnwbc[:], in_=rnwbc_d[:, l, :]), writes=[BL])
        dma("sp", lambda e: e.dma_start(out=nw1bc[:], in_=nw1bc_d[:, l, :]), writes=[BL])
        dma("sp", lambda e: e.dma_start(out=rbbc[:], in_=rbbc_d[:, l, :]), writes=[BL])
        dma("sp", lambda e: e.dma_start(out=rw[:], in_=rw_d[l].rearrange("(k p) n -> p k n", p=128)), writes=[BL])
        dma("sp", lambda e: e.dma_start(out=nwT[:], in_=nwT_d[:, l, :, :]), writes=[BL])
        dma("sp", lambda e: e.dma_start(out=mbT[:], in_=modbT_d[:, l, :]), writes=[BL])
        op("act", lambda e: e.activation(out=logg[:], in_=rdt[:], func=AF.Exp, scale=-float(np.log(2.0))), reads=[BL], writes=[BL])
        op("dve", lambda e: e.tensor_scalar(logg[:], logg[:], -1.0, 1.0, op0=ALU.mult, op1=ALU.add), reads=[BL], writes=[BL])
        op("act", lambda e: e.activation(out=logg[:], in_=logg[:], func=AF.Ln), reads=[BL], writes=[BL])
        with ExitStack() as ss:
            tmpa = sb("tmpa", [128, 128], scope=ss)
            tmpb = sb("tmpb", [128, 128], scope=ss)
            Bt = Buf("tmpab")
            for h in range(8):
                op("act", lambda e: e.activation(out=tmpa[:], in_=tri[:, 0, :], func=AF.Exp, scale=logg[:, h:h + 1]), reads=[BL, B_const], writes=[Bt])
                op("dve", lambda e: e.tensor_tensor(out=tmpa[:], in0=tmpa[:], in1=tri[:, 2, :], op=ALU.mult), reads=[Bt], writes=[Bt])
                op("act", lambda e: e.activation(out=tmpb[:], in_=tri[:, 1, :], func=AF.Exp, scale=logg[:, 8 + h:9 + h]), reads=[BL, Bt], writes=[Bt])
                op("dve", lambda e: e.tensor_tensor(out=tmpb[:], in0=tmpb[:], in1=tri[:, 3, :], op=ALU.mult), reads=[Bt], writes=[Bt])
                op("dve", lambda e: e.tensor_tensor(out=DT8[:, h, :], in0=tmpa[:], in1=tmpb[:], op=ALU.add), reads=[Bt], writes=[BL])
            op("act", lambda e: e.activation(out=wk[:, :, 0], in_=logg[:, 0:8], func=AF.Exp, scale=pcol[:, 0:1]), reads=[BL], writes=[BL])
            op("act", lambda e: e.activation(out=wk[:, :, 1], in_=logg[:, 8:16], func=AF.Exp, scale=pcol[:, 2:3]), reads=[BL], writes=[BL])
            op("act", lambda e: e.activation(out=wq[:, :, 0], in_=logg[:, 0:8], func=AF.Exp, scale=pcol[:, 1:2]), reads=[BL], writes=[BL])
            op("act", lambda e: e.activation(out=wq[:, :, 1], in_=logg[:, 8:16], func=AF.Exp, scale=pcol[:, 3:4]), reads=[BL], writes=[BL])
            op("dve", lambda e: e.tensor_scalar(wq[:], wq[:], 0.125, None, op0=ALU.mult), reads=[BL], writes=[BL])
            op("act", lambda e: e.activation(out=G8[0:64, :], in_=logg[0:64, 0:8], func=AF.Exp, scale=128.0), reads=[BL], writes=[BL])
            op("act", lambda e: e.activation(out=G8[64:128, :], in_=logg[64:128, 8:16], func=AF.Exp, scale=128.0), reads=[BL], writes=[BL])
            op("dve", lambda e: e.tensor_copy(out=Gbc[:], in_=bc_mid(G8[:], 64)), reads=[BL], writes=[BL])
            fw.barrier()
        with ExitStack() as ss:
            mwr = Ring(nc, ss, "mw", 2, [128, 8, 512], F32)
            mbrow = sb("mbrow", [3, 4 * D], scope=ss)
            mrow = sb("mrow", [3, 4 * D], scope=ss)
            Bmb = Buf("mbrow")
            for r in range(3):
                dma("sp", lambda e: e.dma_start(out=mbrow[r:r + 1, :], in_=modb_d[l:l + 1, 2 * D:6 * D]), writes=[Bmb])
            psM = ps[:, 0, 0:64].rearrange("p (a b) -> p a b", b=4)
            for cb in range(12):
                m, half = cb // 2, cb % 2
                mw, mwb = mwr.next()
                dma("sp", lambda e: e.dma_start(out=mw[:], in_=modw_d[l, :, cb * 512:(cb + 1) * 512].rearrange("(k p) n -> p k n", p=128)), writes=[mwb])
                if m in (0, 1):
                    for jj in range(4):
                        idx = m * 8 + half * 4 + jj
                        for k in range(8):
                            op("pe", lambda e: e.matmul(psM[:, idx, 0:3], lhsT=mw[:, k, jj * 128:(jj + 1) * 128], rhs=siluT[:, k, :],
                                                        start=(k == 0), stop=(k == 7)), reads=[mwb, B_const], writes=[PB[0]])
                else:
                    for k in range(8):
                        op("pe", lambda e: e.matmul(ps[0:3, 1, :], lhsT=siluT[:, k, :], rhs=mw[:, k, :],
                                                    start=(k == 0), stop=(k == 7)), reads=[mwb, B_const], writes=[PB[1]])
                    c0 = (m - 2) * D + half * 512
                    op("dve", lambda e: e.tensor_tensor(out=mrow[0:3, c0:c0 + 512], in0=ps[0:3, 1, :], in1=mbrow[0:3, c0:c0 + 512], op=ALU.add),
                       reads=[PB[1], Bmb], writes=[Bmb])
            op("dve", lambda e: e.tensor_tensor(out=modF[:], in0=psM[:, :, 0:3], in1=bc_mid(mbT[:, 0:16], 3), op=ALU.add), reads=[PB[0], BL], writes=[BL])
            op("dve", lambda e: e.scalar_tensor_tensor(out=A1[:], in0=modF[:, 8:16, :], scalar=1.0, in1=bc_mid(nwT[:, 0, :], 3), op0=ALU.add, op1=ALU.mult),
               reads=[BL], writes=[BL])
            dma("sp", lambda e: e.dma_start(out=modrow_d[:, :], in_=mrow[0:3, :]), reads=[Bmb], writes=[BL])
            fw.barrier()
        with ExitStack() as ss:
            wft = sb("wft", [128, 4, D], scope=ss)
            Bw = Buf("wft")
            dma("sp", lambda e: e.dma_start(out=wft[:], in_=wfT_d[l].rearrange("(g p) d -> p g d", p=128)), writes=[Bw])
            for dk in range(8):
                for cs in range(2):
                    for gc in range(4):
                        op("pe", lambda e: e.matmul(ps[:, 2 + cs, gc * 128:(gc + 1) * 128], lhsT=wft[:, gc, dk * 128:(dk + 1) * 128], rhs=bd64[:, cs, :],
                                                    start=True, stop=True), reads=[Bw, B_const], writes=[PB[2 + cs]])
                    op("act", lambda e: e.activation(out=wz[:, dk, cs * 512:(cs + 1) * 512], in_=ps[:, 2 + cs, :], func=AF.Copy), reads=[PB[2 + cs]], writes=[BL])
            fw.barrier()

        if STOP_AFTER == "setup":
            ls.close()
            lt.close()
            break
        for s in range(NSEQ):
            sq = ExitStack()
            kvs = sb("kvs", [128, NCS, 512], BF16, scope=sq)
            Bkv = Buf("kvs")
            with ExitStack() as pa:
                wqk = sb("wqk", [128, 8, 2048], BF16, scope=pa)
                Bwqk = Buf("wqk")
                for k in range(8):
                    dma("pool", lambda e: e.dma_start(out=wqk[:, k, :], in_=wqkvg_d[l, k * 128:(k + 1) * 128, :]), writes=[Bwqk])
                ropet = sb("ropet", [128, 2, NCX, 32], scope=pa)
                dma("sp", lambda e: e.dma_start(out=ropet[:], in_=rope_d), writes=[Bwqk])
                xr = Ring(nc, pa, "xa", 3, [128, D], F32)
                junk = sb("junkA", [128, D], BF16, scope=pa)
                Bj = Buf("junkA")
                ssq = sb("ssqA", [128, 4], scope=pa)
                xnr = Ring(nc, pa, "xn", 2, [128, D], BF16)
                hTr = Ring(nc, pa, "hT", 2, [128, 8, 128], BF16)
                qkr = Ring(nc, pa, "qkrot", 2, [128, 16, 64], BF16)
                rtmp = sb("rtmp", [128, 4, 16, 32], scope=pa)
                Brt = Buf("rtmp")
                kwr = Ring(nc, pa, "kw", 2, [128, 8, 2, 64], BF16)
                qcr = Ring(nc, pa, "qc", 2, [128, 8, 2, 64], BF16)
                sgt = sb("sgt", [128, 512], scope=pa)
                Bsg = Buf("sgt")
                recr = Ring(nc, pa, "recA", 2, [128, 3072], BF16)
                zrr = Ring(nc, pa, "zrec", 2, [128, 1024], BF16)
                def mkA(c):
                    st = {}
                    def fld():
                        xt, xb = xr.next()
                        dma("sp", lambda e: e.dma_start(out=xt[:], in_=chunk_src(l, s, c)), writes=[xb])
                        conv_step(1)
                        st.update(xt=xt, xb=xb)
                    def f0():
                        ci = s * NCS + c
                        is_ctx = c < NCC
                        col = 2 if is_ctx else s
                        xt, xb = st["xt"], st["xb"]
                        op("dve", lambda e: e.scalar_tensor_tensor(out=junk[:], in0=xt[:], scalar=1.0, in1=xt[:], op0=ALU.mult, op1=ALU.mult, accum_out=ssq[:, 0:1]),
                           reads=[xb], writes=[Bj])
                        op("act", lambda e: e.activation(out=ssq[:, 1:2], in_=ssq[:, 0:1], func=AF.Ln, scale=1.0 / D, bias=epst[:, 0:1]), reads=[Bj], writes=[Bj])
                        op("act", lambda e: e.activation(out=ssq[:, 2:3], in_=ssq[:, 1:2], func=AF.Exp, scale=-0.5), reads=[Bj], writes=[Bj])
                        xn, xnb = xnr.next()
                        op("dve", lambda e: e.tensor_scalar(xn[:], xt[:], ssq[:, 2:3], None, op0=ALU.mult), reads=[xb, Bj], writes=[xnb])
                        st.update(xn=xn, xnb=xnb)
                    def f1():
                        ci = s * NCS + c
                        is_ctx = c < NCC
                        col = 2 if is_ctx else s
                        xn, xnb = st["xn"], st["xnb"]
                        pT = psb16(0)
                        for k in range(8):
                            op("pe", lambda e: e.transpose(out=pT[:, k * 128:(k + 1) * 128], in_=xn[:, k * 128:(k + 1) * 128], identity=identb[:]),
                               reads=[xnb, B_const], writes=[PB[0]])
                        hT, hTb = hTr.next()
                        for k in range(8):
                            op("act", lambda e: e.activation(out=hT[:, k, :], in_=pT[:, k * 128:(k + 1) * 128], func=AF.Identity,
                                                             scale=A1[:, k, col:col + 1], bias=modF[:, k, col:col + 1]), reads=[PB[0], BL], writes=[hTb])
                        for grp in range(6):
                            for k in range(8):
                                rhs = wz[:, k, grp * 512:(grp + 1) * 512] if grp < 2 else wqk[:, k, (grp - 2) * 512:(grp - 1) * 512]
                                op("pe", lambda e: e.matmul(ps[:, 1 + grp, :], lhsT=hT[:, k, :], rhs=rhs, start=(k == 0), stop=(k == 7)),
                                   reads=[hTb, BL, Bwqk], writes=[PB[1 + grp]])
                        rec, recb = recr.next()
                        if (not is_ctx) or has_ctx_out:
                            zr, zrb = zrr.next()
                            op("act", lambda e: e.activation(out=zr[:, 0:512], in_=ps[:, 1, :], func=AF.Copy), reads=[PB[1]], writes=[zrb])
                            op("act", lambda e: e.activation(out=zr[:, 512:1024], in_=ps[:, 2, :], func=AF.Copy), reads=[PB[2]], writes=[zrb])
                            if is_ctx:
                                dma("sp", lambda e: e.dma_start(out=zc_d[s, :, c, :], in_=zr[:]), reads=[zrb])
                            else:
                                dma("sp", lambda e: e.dma_start(out=zs_d[s, :, c - NCC, :], in_=zr[:]), reads=[zrb])
                        qk, qkb = qkr.next()
                        psqk = ps[:, 3:5, :].rearrange("p a (h d) -> p (a h) d", d=64)
                        if is_ctx:
                            op("act", lambda e: e.activation(out=qk[:], in_=psqk, func=AF.Copy), reads=[PB[3], PB[4]], writes=[qkb])
                        else:
                            cx = c - NCC
                            cosb = ropet[:, 0, cx, :].unsqueeze(1).to_broadcast([128, 16, 32])
                            sinb = ropet[:, 1, cx, :].unsqueeze(1).to_broadcast([128, 16, 32])
                            t1, t2 = psqk[:, :, 0:32], psqk[:, :, 32:64]
                            op("dve", lambda e: e.tensor_tensor(out=rtmp[:, 0], in0=t1, in1=cosb, op=ALU.mult), reads=[PB[3], PB[4], Bwqk], writes=[Brt])
                            op("dve", lambda e: e.tensor_tensor(out=rtmp[:, 1], in0=t2, in1=sinb, op=ALU.mult), reads=[PB[3], PB[4], Bwqk], writes=[Brt])
                            op("dve", lambda e: e.tensor_tensor(out=rtmp[:, 2], in0=t1, in1=sinb, op=ALU.mult), reads=[PB[3], PB[4], Bwqk], writes=[Brt])
                            op("dve", lambda e: e.tensor_tensor(out=rtmp[:, 3], in0=t2, in1=cosb, op=ALU.mult), reads=[PB[3], PB[4], Bwqk], writes=[Brt])
                            op("dve", lambda e: e.tensor_tensor(out=qk[:, :, 0:32], in0=rtmp[:, 0], in1=rtmp[:, 1], op=ALU.subtract), reads=[Brt], writes=[qkb])
                            op("dve", lambda e: e.tensor_tensor(out=qk[:, :, 32:64], in0=rtmp[:, 2], in1=rtmp[:, 3], op=ALU.add), reads=[Brt], writes=[qkb])
                        kw, kwb = kwr.next()
                        qc, qcb = qcr.next()
                        op("dve", lambda e: e.tensor_tensor(out=kw[:], in0=qk[:, 8:16, :].unsqueeze(2).to_broadcast([128, 8, 2, 64]),
                                                            in1=wk[:].unsqueeze(3).to_broadcast([128, 8, 2, 64]), op=ALU.mult), reads=[qkb, BL], writes=[kwb])
                        op("dve", lambda e: e.tensor_tensor(out=qc[:], in0=qk[:, 0:8, :].unsqueeze(2).to_broadcast([128, 8, 2, 64]),
                                                            in1=wq[:].unsqueeze(3).to_broadcast([128, 8, 2, 64]), op=ALU.mult), reads=[qkb, BL], writes=[qcb])
                        op("act", lambda e: e.activation(out=rec[:, 2048:2560], in_=ps[:, 5, :], func=AF.Copy), reads=[PB[5]], writes=[recb])
                        op("act", lambda e: e.activation(out=sgt[:], in_=ps[:, 6, :], func=AF.Silu), reads=[PB[6]], writes=[Bsg])
                        op("dve", lambda e: e.tensor_tensor(out=rec[:, 2560:3072], in0=sgt[:], in1=rnwbc[:], op=ALU.mult), reads=[Bsg, BL], writes=[recb])
                        for h in range(8):
                            op("pe", lambda e: e.matmul(ps[:, 0, h * 64:(h + 1) * 64], lhsT=kw[:, h].rearrange("p a d -> p (a d)"), rhs=rec[:, 2048 + h * 64:2048 + (h + 1) * 64],
                                                        start=True, stop=True), reads=[kwb, recb], writes=[PB[0]])
                        op("act", lambda e: e.activation(out=kvs[:, c, :], in_=ps[:, 0, :], func=AF.Copy), reads=[PB[0]], writes=[Bkv])
                        p7 = psb16(7)
                        for j in range(8):
                            op("pe", lambda e: e.transpose(out=p7[:, j * 128:(j + 1) * 128], in_=qk[:, 2 * j:2 * j + 2, :].rearrange("p a d -> p (a d)"), identity=identb[:]),
                               reads=[qkb, B_const], writes=[PB[7]])
                        op("act", lambda e: e.activation(out=rec[:, 0:1024], in_=p7[:, :], func=AF.Copy), reads=[PB[7]], writes=[recb])
                        for h in range(8):
                            op("pe", lambda e: e.transpose(out=p7[:, h * 128:(h + 1) * 128], in_=qc[:, h].rearrange("p a d -> p (a d)"), identity=identb[:]),
                               reads=[qcb, B_const], writes=[PB[7]])
                        op("act", lambda e: e.activation(out=rec[:, 1024:2048], in_=p7[:, :], func=AF.Copy), reads=[PB[7]], writes=[recb])
                        dma("sp", lambda e: e.dma_start(out=spill_d[ci], in_=rec[:]), reads=[recb])
                    return f0, f1, fld
                fA = [mkA(c) for c in range(NCS)]
                fA[0][2]()
                for i in range(NCS + 1):
                    if i + 1 < NCS:
                        fA[i + 1][2]()
                    if i < NCS:
                        fA[i][0]()
                    if i >= 1:
                        fA[i - 1][1]()
                fw.barrier()
            if STOP_AFTER == "passA":
                sq.close()
                break
            with ExitStack() as fo:
                Zt = sb("Zt", [128, NCX, 1024], BF16, scope=fo)
                Bz = Buf("Zt")
                for q4 in range(4):
                    dma("sp", lambda e: e.dma_start(out=Zt[:, q4 * 8:(q4 + 1) * 8, :], in_=zs_d[s, :, q4 * 8:(q4 + 1) * 8, :]), writes=[Bz])
                St = sb("St", [128, 512], scope=fo)
                tmpS = sb("tmpS", [128, 512], scope=fo)
                Bs = Buf("scan")
                op("dve", lambda e: e.memset(St[:], 0.0), writes=[Bs])
                Gf = Gbc[:].rearrange("p h d -> p (h d)")
                order_f = list(range(NCS))
                order_b = [1, 0] + list(range(NCS - 1, NCC - 1, -1))
                for (lo, hi, order) in ((0, 64, order_f), (64, 128, order_b)):
                    for c in order:
                        op("dve", lambda e: e.tensor_copy(out=tmpS[lo:hi, :], in_=kvs[lo:hi, c, :]), reads=[Bkv, Bs], writes=[Bs])
                        op("dve", lambda e: e.tensor_copy(out=kvs[lo:hi, c, :], in_=St[lo:hi, :]), reads=[Bs], writes=[Bkv])
                        op("dve", lambda e: e.tensor_tensor(out=St[lo:hi, :], in0=St[lo:hi, :], in1=Gf[lo:hi, :], op=ALU.mult), reads=[Bs, BL], writes=[Bs])
                        op("dve", lambda e: e.tensor_tensor(out=St[lo:hi, :], in0=St[lo:hi, :], in1=tmpS[lo:hi, :], op=ALU.add), reads=[Bs], writes=[Bs])
                tabr = Ring(nc, fo, "dtab", 2, [128, 2, 8, 256], BF16)
                ystr = Ring(nc, fo, "yst", 2, [128, 2, 4, 128], BF16)
                ystage = sb("ystage", [128, 4, T // 2], BF16, scope=fo)
                Bys = Buf("ystage")
                vsr = Ring(nc, fo, "vsb", 2, [128, 256], F32)
                for kb in range(9):
                    bank0 = 4 * (kb % 2)
                    NW = 256 if kb < 8 else 2
                    conv_step(3)
                    for tq in range(4):
                        tab, tabb = tabr.next()
                        dma("sp", lambda e: e.dma_start(out=tab[:].rearrange("p a b c -> p (a b c)"), in_=dft_d[kb, tq]), writes=[tabb])
                        for n in range(4):
                            for tcc in range(8):
                                tt = tq * 8 + tcc
                                op("pe", lambda e: e.matmul(ps[:, bank0 + n, 0:NW], lhsT=Zt[:, tt, n * 128:(n + 1) * 128], rhs=tab[:, 0, tcc, 0:NW],
                                                            start=(tq == 0 and tcc == 0), stop=False), reads=[Bz, tabb], writes=[PB[bank0 + n]])
                                op("pe", lambda e: e.matmul(ps[:, bank0 + n, 256:256 + NW], lhsT=Zt[:, tt, 512 + n * 128:512 + (n + 1) * 128], rhs=tab[:, 1, tcc, 0:NW],
                                                            start=False, stop=(tq == 3 and tcc == 7)), reads=[Bz, tabb], writes=[PB[bank0 + n]])
                    if kb < 8:
                        yst, ystb = ystr.next()
                    for n in range(4):
                        U = ps[:, bank0 + n, 0:256]
                        if kb == 8:
                            op("act", lambda e: e.activation(out=ystage[:, n, 0:1], in_=U[:, 0:1], func=AF.Copy), reads=[PB[bank0 + n]], writes=[Bys])
                            continue
                        vs, vsb_ = vsr.next()
                        op("act", lambda e: e.activation(out=vs[:], in_=ps[:, bank0 + n, 256:512], func=AF.Copy), reads=[PB[bank0 + n]], writes=[vsb_])
                        op("dve", lambda e: e.tensor_tensor(out=yst[:, :, n, :], in0=U.rearrange("p (a b) -> p a b", b=128), in1=vs[:].rearrange("p (a b) -> p a b", b=128), op=ALU.add),
                           reads=[PB[bank0 + n], vsb_], writes=[ystb])
                        j0 = 1 if kb == 0 else 0
                        hi = T // 2 - 256 * kb - j0
                        lo = T // 2 - 256 * kb - 256
                        op("dve", lambda e: e.tensor_tensor(out=ystage[:, n, hi:lo:-1], in0=U[:, j0:256], in1=vs[:, j0:256], op=ALU.subtract), reads=[PB[bank0 + n], vsb_], writes=[Bys])
                    if kb < 8:
                        ci0 = s * NCS + NCC + 2 * kb
                        dma("sp", lambda e: e.dma_start(out=yts_d[ci0:ci0 + 2].rearrange("c p f -> p c f"), in_=yst[:].rearrange("p a n t -> p a (n t)")), reads=[ystb])
                for c4 in range(4):
                    ci0 = s * NCS + NCC + 16 + 4 * c4
                    for n in range(4):
                        dma("sp", lambda e: e.dma_start(out=yts_d[ci0:ci0 + 4, :, n * 128:(n + 1) * 128].rearrange("c p t -> p c t"),
                                                        in_=ystage[:, n, c4 * 512:(c4 + 1) * 512].rearrange("p (c t) -> p c t", t=128)), reads=[Bys])
                if has_ctx_out:
                    Zc = sb("Zc", [128, NCC, 1024], BF16, scope=fo)
                    tabc = sb("tabc", [128, 2, 2, 256], BF16, scope=fo)
                    Bzc = Buf("Zc")
                    dma("sp", lambda e: e.dma_start(out=Zc[:], in_=zc_d[s]), writes=[Bzc])
                    dma("sp", lambda e: e.dma_start(out=tabc[:].rearrange("p a b c -> p (a b c)"), in_=dftc_d), writes=[Bzc])
                    yst, ystb = ystr.next()
                    for n in range(4):
                        for tt in range(2):
                            op("pe", lambda e: e.matmul(ps[:, n, 0:256], lhsT=Zc[:, tt, n * 128:(n + 1) * 128], rhs=tabc[:, 0, tt, :], start=(tt == 0), stop=False),
                               reads=[Bzc], writes=[PB[n]])
                            op("pe", lambda e: e.matmul(ps[:, n, 0:256], lhsT=Zc[:, tt, 512 + n * 128:512 + (n + 1) * 128], rhs=tabc[:, 1, tt, :], start=False, stop=(tt == 1)),
                               reads=[Bzc], writes=[PB[n]])
                        op("act", lambda e: e.activation(out=yst[:, :, n, :], in_=ps[:, n, 0:256].rearrange("p (a b) -> p a b", b=128), func=AF.Copy),
                           reads=[PB[n]], writes=[ystb])
                    ci0 = s * NCS
                    dma("sp", lambda e: e.dma_start(out=yts_d[ci0:ci0 + 2].rearrange("c p f -> p c f"), in_=yst[:].rearrange("p a n t -> p a (n t)")), reads=[ystb])
                fw.barrier()
            if STOP_AFTER == "fourier":
                sq.close()
                break
            with ExitStack() as pb:
                wout = sb("wout", [128, 8, D], BF16, scope=pb)
                Bwo = Buf("wout")
                for k in range(8):
                    dma("pool", lambda e: e.dma_start(out=wout[:, k, :], in_=wout_d[l, k * 128:(k + 1) * 128, :]), writes=[Bwo])
                bcs = {}
                Bbc = Buf("bc")
                for (nm, col) in (("x", s), ("c", 2)):
                    if nm == "c" and not has_ctx_out:
                        continue
                    G1 = sb(f"G1bc{nm}", [128, D], scope=pb)
                    A2 = sb(f"A2bc{nm}", [128, D], scope=pb)
                    B2 = sb(f"B2bc{nm}", [128, D], scope=pb)
                    dma("sp", lambda e: e.dma_start(out=G1[:], in_=modrow_d[col:col + 1, 0:D].partition_broadcast(128)), writes=[Bbc])
                    dma("sp", lambda e: e.dma_start(out=B2[:], in_=modrow_d[col:col + 1, D:2 * D].partition_broadcast(128)), writes=[Bbc])
                    dma("sp", lambda e: e.dma_start(out=A2[:], in_=modrow_d[col:col + 1, 2 * D:3 * D].partition_broadcast(128)), writes=[Bbc])
                    op("dve", lambda e: e.scalar_tensor_tensor(out=A2[:], in0=A2[:], scalar=1.0, in1=nw1bc[:], op0=ALU.add, op1=ALU.mult), reads=[Bbc, BL], writes=[Bbc])
                    bcs[nm] = (G1, A2, B2)
                recr = Ring(nc, pb, "recB", 2, [128, 3072], BF16)
                catr = Ring(nc, pb, "catT", 2, [128, 8, 128], BF16)
                xr = Ring(nc, pb, "xb", 2, [128, D], F32)
                PTr = Ring(nc, pb, "PT", 2, [128, 8, 128], BF16)
                gn = sb("gn", [128, 3, 8, 64], scope=pb)
                gs = sb("gs", [128, 4, 8], scope=pb)
                Bgn = Buf("gn")
                yr = Ring(nc, pb, "yb", 2, [128, 512], BF16)
                x1r = Ring(nc, pb, "x1", 2, [128, D], F32)
                junk = sb("junkB", [128, D], BF16, scope=pb)
                ssq = sb("ssqB", [128, 4], scope=pb)
                Bj = Buf("junkB")
                h2r = Ring(nc, pb, "h2", 2, [128, D], F32)
                h2Tr = Ring(nc, pb, "h2T", 2, [128, 8, 128], F32)
                lgr = Ring(nc, pb, "lg", 2, [128, NE], F32)
                rtr = Ring(nc, pb, "rt", 2, [128, 4, NE], F32)
                smr = Ring(nc, pb, "sm", 2, [128, 16], F32)
                mbr = Ring(nc, pb, "mb", 2, [128, NE], BF16)
                h2br = Ring(nc, pb, "h2b", 2, [128, D], BF16)
                chunks = list(range(NCS)) if has_ctx_out else list(range(NCC, NCS))
                def mkB(c):
                    st = {}
                    def fL():
                        ci = s * NCS + c
                        is_ctx = c < NCC
                        G1, A2, B2 = bcs["c" if is_ctx else "x"]
                        conv_step(1)
                        rec, recb = recr.next()
                        dma("sp", lambda e: e.dma_start(out=rec[:], in_=spill_d[ci]), writes=[recb])
                        cat, catb = catr.next()
                        dma("sp", lambda e: e.dma_start(out=cat[:, 0:4, :].rearrange("p a t -> p (a t)"), in_=yts_d[ci]), writes=[catb])
                        xt, xb = xr.next()
                        dma("sp", lambda e: e.dma_start(out=xt[:], in_=chunk_src(l, s, c)), writes=[xb])
                        st.update(rec=rec, recb=recb, cat=cat, catb=catb, xt=xt, xb=xb)
                    def f1():
                        ci = s * NCS + c
                        is_ctx = c < NCC
                        G1, A2, B2 = bcs["c" if is_ctx else "x"]
                        rec, recb, cat, catb, xt, xb = st["rec"], st["recb"], st["cat"], st["catb"], st["xt"], st["xb"]
                        for h in range(8):
                            hp, hh = h // 2, h % 2
                            op("pe", lambda e: e.matmul(ps[:, hh, hp * 128:(hp + 1) * 128],
                                                        lhsT=rec[hh * 64:(hh + 1) * 64, 512 + hp * 128:512 + (hp + 1) * 128],
                                                        rhs=rec[hh * 64:(hh + 1) * 64, hp * 128:(hp + 1) * 128], start=True, stop=True),
                               reads=[recb], writes=[PB[hh]])
                        PT, PTb = PTr.next()
                        yield
                        for g2 in range(2):
                            op("dve", lambda e: e.tensor_tensor(out=PT[:].rearrange("p (a b) t -> p a b t", b=2)[:, :, g2, :], in0=ps[:, g2, :].rearrange("p (h t) -> p h t", t=128),
                                                                in1=DT8[:].rearrange("p (a b) t -> p a b t", b=2)[:, :, g2, :], op=ALU.mult), reads=[PB[g2], BL], writes=[PTb])
                        for h in range(8):
                            op("pe", lambda e: e.matmul(ps[:, 2, h * 64:(h + 1) * 64], lhsT=PT[:, h, :], rhs=rec[:, 2048 + h * 64:2048 + (h + 1) * 64], start=True, stop=False),
                               reads=[PTb, recb], writes=[PB[2]])
                            op("pe", lambda e: e.matmul(ps[:, 2, h * 64:(h + 1) * 64], lhsT=rec[:, 1024 + h * 128:1024 + (h + 1) * 128], rhs=kvs[:, c, h * 64:(h + 1) * 64], start=False, stop=True),
                               reads=[recb, Bkv], writes=[PB[2]])
                        yield
                        po = ps[:, 2, :].rearrange("p (h d) -> p h d", d=64)
                        op("dve", lambda e: e.tensor_reduce(out=gs[:, 0, :], in_=po, axis=AX.X, op=ALU.add), reads=[PB[2]], writes=[Bgn])
                        op("dve", lambda e: e.tensor_scalar(gs[:, 1, :], gs[:, 0, :], -1.0 / 64, None, op0=ALU.mult), reads=[Bgn], writes=[Bgn])
                        op("dve", lambda e: e.tensor_tensor(out=gn[:, 0], in0=po, in1=bc_mid(gs[:, 1, :], 64), op=ALU.add), reads=[PB[2], Bgn], writes=[Bgn])
                        op("dve", lambda e: e.tensor_tensor(out=gn[:, 1], in0=gn[:, 0], in1=gn[:, 0], op=ALU.mult), reads=[Bgn], writes=[Bgn])
                        op("dve", lambda e: e.tensor_reduce(out=gs[:, 2, :], in_=gn[:, 1], axis=AX.X, op=ALU.add), reads=[Bgn], writes=[Bgn])
                        op("act", lambda e: e.activation(out=gs[:, 3, :], in_=gs[:, 2, :], func=AF.Ln, scale=1.0 / 64, bias=epst[:, 0:1]), reads=[Bgn], writes=[Bgn])
                        yield
                        op("act", lambda e: e.activation(out=gs[:, 2, :], in_=gs[:, 3, :], func=AF.Exp, scale=-0.5), reads=[Bgn], writes=[Bgn])
                        op("dve", lambda e: e.tensor_tensor(out=gn[:, 2], in0=gn[:, 0], in1=bc_mid(gs[:, 2, :], 64), op=ALU.mult), reads=[Bgn], writes=[Bgn])
                        yb_, ybb = yr.next()
                        op("dve", lambda e: e.tensor_tensor(out=yb_[:], in0=gn[:, 2].rearrange("p h d -> p (h d)"), in1=rec[:, 2560:3072], op=ALU.mult), reads=[Bgn, recb], writes=[ybb])
                        p3 = psb16(3)
                        for j in range(4):
                            op("pe", lambda e: e.transpose(out=p3[:, j * 128:(j + 1) * 128], in_=yb_[:, j * 128:(j + 1) * 128], identity=identb[:]), reads=[ybb, B_const], writes=[PB[3]])
                        op("act", lambda e: e.activation(out=cat[:, 4:8, :].rearrange("p a t -> p (a t)"), in_=p3[:, 0:512], func=AF.Copy), reads=[PB[3]], writes=[catb])
                        for hf in range(2):
                            for m in range(8):
                                op("pe", lambda e: e.matmul(ps[:, 4 + hf, :], lhsT=cat[:, m, :], rhs=wout[:, m, hf * 512:(hf + 1) * 512], start=(m == 0), stop=(m == 7)),
                                   reads=[catb, Bwo], writes=[PB[4 + hf]])
                        x1, x1b = x1r.next()
                        yield
                        op("dve", lambda e: e.tensor_tensor(out=x1[:], in0=ps[:, 4:6, :].rearrange("p a n -> p (a n)"), in1=G1[:], op=ALU.mult), reads=[PB[4], PB[5], Bbc], writes=[x1b])
                        op("dve", lambda e: e.tensor_tensor(out=x1[:], in0=x1[:], in1=xt[:], op=ALU.add), reads=[x1b, xb], writes=[x1b])
                        dma("sp", lambda e: e.dma_start(out=xmid_d[ci * 128:(ci + 1) * 128, :], in_=x1[:]), reads=[x1b])
                        if DEBUG:
                            dma("sp", lambda e: e.dma_start(out=dbg_x[ci * 128:(ci + 1) * 128, :], in_=x1[:]), reads=[x1b])
                        st.update(x1=x1, x1b=x1b)
                    def f2():
                        ci = s * NCS + c
                        is_ctx = c < NCC
                        G1, A2, B2 = bcs["c" if is_ctx else "x"]
                        x1, x1b = st["x1"], st["x1b"]
                        op("dve", lambda e: e.scalar_tensor_tensor(out=junk[:], in0=x1[:], scalar=1.0, in1=x1[:], op0=ALU.mult, op1=ALU.mult, accum_out=ssq[:, 0:1]),
                           reads=[x1b], writes=[Bj])
                        op("act", lambda e: e.activation(out=ssq[:, 1:2], in_=ssq[:, 0:1], func=AF.Ln, scale=1.0 / D, bias=epst[:, 0:1]), reads=[Bj], writes=[Bj])
                        op("act", lambda e: e.activation(out=ssq[:, 2:3], in_=ssq[:, 1:2], func=AF.Exp, scale=-0.5), reads=[Bj], writes=[Bj])
                        h2, h2b = h2r.next()
                        yield
                        op("dve", lambda e: e.scalar_tensor_tensor(out=h2[:], in0=x1[:], scalar=ssq[:, 2:3], in1=A2[:], op0=ALU.mult, op1=ALU.mult), reads=[x1b, Bj, Bbc], writes=[h2b])
                        op("dve", lambda e: e.tensor_tensor(out=h2[:], in0=h2[:], in1=B2[:], op=ALU.add), reads=[h2b, Bbc], writes=[h2b])
                        for k in range(8):
                            op("pe", lambda e: e.transpose(out=ps[:, 6 + k // 4, (k % 4) * 128:(k % 4 + 1) * 128], in_=h2[:, k * 128:(k + 1) * 128], identity=identf[:]),
                               reads=[h2b, B_const], writes=[PB[6 + k // 4]])
                        h2T, h2Tb = h2Tr.next()
                        op("act", lambda e: e.activation(out=h2T[:].rearrange("p k t -> p (k t)"), in_=ps[:, 6:8, :].rearrange("p a n -> p (a n)"), func=AF.Copy),
                           reads=[PB[6], PB[7]], writes=[h2Tb])
                        for k in range(8):
                            op("pe", lambda e: e.matmul(ps[:, 6, 0:NE], lhsT=h2T[:, k, :], rhs=rw[:, k, :], start=(k == 0), stop=(k == 7)), reads=[h2Tb, BL], writes=[PB[6]])
                        lg, lgb = lgr.next()
                        yield
                        op("dve", lambda e: e.tensor_tensor(out=lg[:], in0=ps[:, 6, 0:NE], in1=rbbc[:], op=ALU.add), reads=[PB[6], BL], writes=[lgb])
                        if DEBUG:
                            dma("sp", lambda e: e.dma_start(out=dbg_lg[ci * 128:(ci + 1) * 128, :], in_=lg[:]), reads=[lgb])
                        rt, rtb = rtr.next()
                        sm, smb = smr.next()
                        op("dve", lambda e: e.max(out=sm[:, 0:8], in_=lg[:]), reads=[lgb], writes=[smb])
                        op("dve", lambda e: e.tensor_scalar(rt[:, 0, :], lg[:], sm[:, 3:4], None, op0=ALU.is_ge), reads=[lgb, smb], writes=[rtb])
                        op("dve", lambda e: e.tensor_scalar(sm[:, 8:9], sm[:, 0:1], -1.0, None, op0=ALU.mult), reads=[smb], writes=[smb])
                        op("act", lambda e: e.activation(out=rt[:, 1, :], in_=lg[:], func=AF.Exp, bias=sm[:, 8:9], scale=1.0), reads=[lgb, smb], writes=[rtb])
                        yield
                        op("dve", lambda e: e.scalar_tensor_tensor(out=rt[:, 1, :], in0=rt[:, 1, :], scalar=1.0, in1=rt[:, 0, :], op0=ALU.mult, op1=ALU.mult, accum_out=sm[:, 9:10]),
                           reads=[rtb], writes=[rtb, smb])
                        op("dve", lambda e: e.reciprocal(out=sm[:, 10:11], in_=sm[:, 9:10]), reads=[smb], writes=[smb])
                        op("dve", lambda e: e.tensor_scalar(Gd[:, ci, :], rt[:, 1, :], sm[:, 10:11], None, op0=ALU.mult), reads=[rtb, smb], writes=[Brt_])
                        mb_, mbb = mbr.next()
                        op("dve", lambda e: e.tensor_copy(out=mb_[:], in_=rt[:, 0, :]), reads=[rtb], writes=[mbb])
                        op("pe", lambda e: e.matmul(ps[:, 7, 32:64], lhsT=ustr[:], rhs=mb_[:], start=True, stop=True), reads=[mbb, B_const], writes=[PB[7]])
                        op("pe", lambda e: e.matmul(ps[:, 7, 64:96], lhsT=onesb[:], rhs=mb_[:], start=True, stop=True), reads=[mbb, B_const], writes=[PB[7]])
                        yield
                        op("dve", lambda e: e.tensor_tensor(out=rt[:, 2, :], in0=ps[:, 7, 32:64], in1=basecap[:], op=ALU.add), reads=[PB[7], Brt_], writes=[rtb])
                        op("dve", lambda e: e.tensor_tensor(out=basecap[:], in0=ps[:, 7, 64:96], in1=basecap[:], op=ALU.add), reads=[PB[7], Brt_], writes=[Brt_])
                        for k4 in range(4):
                            op("dve", lambda e: e.scalar_tensor_tensor(out=rt[:, 3, :], in0=lg[:], scalar=sm[:, k4:k4 + 1], in1=rt[:, 2, :], op0=ALU.is_equal, op1=ALU.mult,
                                                                       accum_out=poskf[:, ci, k4:k4 + 1]), reads=[lgb, smb, rtb, Brt_], writes=[rtb, Brt_])
                            op("dve", lambda e: e.scalar_tensor_tensor(out=rt[:, 3, :], in0=lg[:], scalar=sm[:, k4:k4 + 1], in1=rowc[:, 1, 0:NE], op0=ALU.is_equal, op1=ALU.mult,
                                                                       accum_out=ekf[:, ci, k4:k4 + 1]), reads=[lgb, smb, rtb, Brt_], writes=[rtb, Brt_])
                            op("dve", lambda e: e.scalar_tensor_tensor(out=rt[:, 3, :], in0=lg[:], scalar=sm[:, k4:k4 + 1], in1=Gd[:, ci, :], op0=ALU.is_equal, op1=ALU.mult,
                                                                       accum_out=gatek[:, ci, k4:k4 + 1]), reads=[lgb, smb, rtb, Brt_], writes=[rtb, Brt_])
                        h2b_, h2bb = h2br.next()
                        op("act", lambda e: e.activation(out=h2b_[:], in_=h2[:], func=AF.Copy), reads=[h2b], writes=[h2bb])
                        dma("sp", lambda e: e.dma_start(out=h2d_d[ci * 128:(ci + 1) * 128, :], in_=h2b_[:]), reads=[h2bb])
                    return fL, f1, f2
                fB = [mkB(c) for c in chunks]
                nB = len(fB)
                for i in range(nB + 2):
                    if i < nB:
                        fB[i][0]()
                    gens = []
                    if 0 <= i - 1 < nB:
                        gens.append(fB[i - 1][1]())
                    if 0 <= i - 2 < nB:
                        gens.append(fB[i - 2][2]())
                    while gens:
                        for g_ in list(gens):
                            if next(g_, "done") == "done":
                                gens.remove(g_)
                fw.barrier()
            sq.close()
        ls.close()
        if STOP_AFTER in ("passA", "scan", "fourier", "mixer0"):
            lt.close()
            break

        conv_step(10000)
        idxW = sb("idxW", [128, NSB, 3], I32, scope=lt)
        idxB = sb("idxB", [128, NSB], I32, scope=lt)
        Bsch = Buf("sched")
        with ExitStack() as sc:
            cnt = sb("cnt", [128, NE], scope=sc)
            nblk = sb("nblk", [128, NE], scope=sc)
            cum = sb("cum", [128, NE], scope=sc)
            cumex = sb("cumex", [128, NE], scope=sc)
            ones32 = sb("ones32", [128, NE], scope=sc)
            cmp17 = sb("cmp17", [128, NE, NT], scope=sc)
            cmpb = sb("cmpb", [128, NSB, NE], scope=sc)
            ebt = sb("ebt", [128, 4, NSB], scope=sc)
            idf = sb("idf", [128, 1, NSB, 4], scope=sc)
            cmpd = sb("cmpd", [128, NTOKC * 4, NE], scope=sc)
            destf = sb("destf", [128, NTOKC * 4], scope=sc)
            bvals = rowc[:, 3, 0:NSB]
            op("dve", lambda e: e.tensor_copy(out=cnt[:], in_=basecap[:]), reads=[Brt_, B_const], writes=[Bsch])
            op("dve", lambda e: e.tensor_tensor(out=cmp17[:], in0=bc_mid(cnt[:], NT), in1=rowc[:, 2, 0:NT].unsqueeze(1).to_broadcast([128, NE, NT]), op=ALU.is_gt), reads=[Bsch], writes=[Bsch])
            op("dve", lambda e: e.tensor_reduce(out=nblk[:], in_=cmp17[:], axis=AX.X, op=ALU.add), reads=[Bsch], writes=[Bsch])
            op("dve", lambda e: e.memset(ones32[:], 1.0), writes=[Bsch])
            op("dve", lambda e: e.tensor_tensor_scan(out=cum[:], data0=ones32[:], data1=nblk[:], initial=0.0, op0=ALU.mult, op1=ALU.add), reads=[Bsch], writes=[Bsch])
            op("dve", lambda e: e.tensor_tensor(out=cumex[:], in0=cum[:], in1=nblk[:], op=ALU.subtract), reads=[Bsch], writes=[Bsch])
            op("dve", lambda e: e.tensor_tensor(out=cmpb[:], in0=cum[:].unsqueeze(1).to_broadcast([128, NSB, NE]), in1=bc_mid(bvals, NE), op=ALU.is_le), reads=[Bsch], writes=[Bsch])
            op("dve", lambda e: e.tensor_reduce(out=ebt[:, 0, :], in_=cmpb[:], axis=AX.X, op=ALU.add), reads=[Bsch], writes=[Bsch])
            op("dve", lambda e: e.tensor_scalar(ebt[:, 0, :], ebt[:, 0, :], float(NE - 1), None, op0=ALU.min), reads=[Bsch], writes=[Bsch])
            op("dve", lambda e: e.tensor_scalar(ebt[:, 3, :], ebt[:, 0, :], 128.0, pcol[:, 2:3], op0=ALU.mult, op1=ALU.add), reads=[Bsch], writes=[Bsch])
            op("dve", lambda e: e.memset(ebt[:, 1, :], 0.0), writes=[Bsch])
            op("dve", lambda e: e.tensor_tensor(out=ebt[:, 1, 2:NSB], in0=ebt[:, 0, 2:NSB], in1=ebt[:, 0, 0:NSB - 2], op=ALU.is_equal), reads=[Bsch], writes=[Bsch])
            for m in range(3):
                op("dve", lambda e: e.scalar_tensor_tensor(out=idf[:, 0, :, m], in0=ebt[:, 1, :], scalar=1.0e6, in1=ebt[:, 3, :], op0=ALU.mult, op1=ALU.add), reads=[Bsch], writes=[Bsch])
                if m > 0:
                    op("dve", lambda e: e.tensor_scalar(idf[:, 0, :, m], idf[:, 0, :, m], float(m * NE * 128), None, op0=ALU.add), reads=[Bsch], writes=[Bsch])
            op("dve", lambda e: e.tensor_copy(out=idxW[:], in_=idf[:, 0, :, 0:3]), reads=[Bsch], writes=[Bsch])
            if l > 0:
                op("dve", lambda e: e.tensor_scalar(ebt[:, 3, :], ebt[:, 3, :], float(l * NE * 128), None, op0=ALU.add), reads=[Bsch], writes=[Bsch])
            op("dve", lambda e: e.tensor_copy(out=idxB[:], in_=ebt[:, 3, :]), reads=[Bsch], writes=[Bsch])
            op("dve", lambda e: e.tensor_scalar(cumex[:], cumex[:], float(BS), None, op0=ALU.mult), reads=[Bsch], writes=[Bsch])
            NA = NTOKC * 4
            ekflat = ekf[:].rearrange("p c k -> p (c k)")
            op("dve", lambda e: e.tensor_tensor(out=cmpd[:], in0=rowc[:, 1, 0:NE].unsqueeze(1).to_broadcast([128, NA, NE]), in1=bc_mid(ekflat, NE), op=ALU.is_equal), reads=[Bsch, Brt_], writes=[Bsch])
            op("dve", lambda e: e.tensor_tensor(out=cmpd[:], in0=cmpd[:], in1=cumex[:].unsqueeze(1).to_broadcast([128, NA, NE]), op=ALU.mult), reads=[Bsch], writes=[Bsch])
            op("dve", lambda e: e.tensor_reduce(out=destf[:], in_=cmpd[:], axis=AX.X, op=ALU.add), reads=[Bsch], writes=[Bsch])
            op("dve", lambda e: e.tensor_tensor(out=destf[:], in0=destf[:], in1=poskf[:].rearrange("p c k -> p (c k)"), op=ALU.add), reads=[Bsch, Brt_], writes=[Bsch])
            op("dve", lambda e: e.tensor_copy(out=destk[:].rearrange("p c k -> p (c k)"), in_=destf[:]), reads=[Bsch], writes=[Brt_])
            if DEBUG:
                dma("sp", lambda e: e.dma_start(out=dbg_sched[:, 0:NE], in_=cnt[:]), reads=[Bsch])
                dma("sp", lambda e: e.dma_start(out=dbg_sched[:, NE:NE + NSB], in_=ebt[:, 0, :]), reads=[Bsch])
                dma("sp", lambda e: e.dma_start(out=dbg_sched[:, NE + NSB:NE + NSB + 64], in_=destf[:, 0:64]), reads=[Bsch])
            fw.barrier()
        with ExitStack() as dp:
            hdr = Ring(nc, dp, "h2ld", 3, [128, D], BF16)
            for (s_, c_) in lchunks:
                ci = s_ * NCS + c_
                hd, hdb = hdr.next()
                dma("sp", lambda e: e.dma_start(out=hd[:], in_=h2d_d[ci * 128:(ci + 1) * 128, :]), writes=[hdb])
                for k4 in range(4):
                    dma("pool", lambda e: e.indirect_dma_start(out=hbuf_d[:, :], out_offset=bass.IndirectOffsetOnAxis(ap=destk[:, ci, k4:k4 + 1], axis=0),
                                                               in_=hd[:, :], in_offset=None), reads=[hdb, Brt_])
            fw.barrier()
        IOA = bass.IndirectOffsetOnAxis
        with ExitStack() as mo:
            wr = Ring(nc, mo, "wexp", 2, [128, 3, 4, 2048], BF16)
            b1r = Ring(nc, mo, "b1t", 3, [128, 24], F32)
            hrr = Ring(nc, mo, "hrows", 2, [128, GB, D], BF16)
            hTr = Ring(nc, mo, "hTm", 2, [128, 8, BS], BF16)
            atr = Ring(nc, mo, "actT", 2, [128, 8, BS], BF16)
            xgr = Ring(nc, mo, "xg", 2, [128, 4, BS], F32)
            osr = Ring(nc, mo, "ostage", 3, [128, D], F32)
            wsrc = (w1g_d, w1l_d, w2_d)
            state = {}

            def load_sb(b):
                b1t, b1b = b1r.next()
                dma("pool", lambda e: e.indirect_dma_start(out=b1t[:, 0:16], out_offset=None, in_=b1T_d.rearrange("l r f -> (l r) f"), in_offset=IOA(ap=idxB[:, b:b + 1], axis=0)), reads=[Bsch], writes=[b1b])
                W, Wb = wr.next()
                for m in range(3):
                    dma("pool", lambda e: e.indirect_dma_start(out=W[:, m].rearrange("p q f -> p (q f)"), out_offset=None, in_=wbf_d[:, :], in_offset=IOA(ap=idxW[:, b, m:m + 1], axis=0),
                                                               bounds_check=bc_reg, oob_is_err=False),
                        reads=[Bsch, Bwbf], writes=[Wb])
                hr, hrb = hrr.next()
                dma("sp", lambda e: e.dma_start(out=hr[:], in_=hbuf_d[b * BS:(b + 1) * BS, :].rearrange("(g p) d -> p g d", p=128)), writes=[hrb])
                state[b] = dict(W=W, Wb=Wb, b1t=b1t, b1b=b1b, hr=hr, hrb=hrb)

            def transp_sb(b):
                st = state[b]
                hr, hrb = st["hr"], st["hrb"]
                hT, hTb = hTr.next()
                for g in range(GB):
                    pt = psb16(g % 2)
                    for k in range(8):
                        op("pe", lambda e: e.transpose(out=pt[:, k * 128:(k + 1) * 128], in_=hr[:, g, k * 128:(k + 1) * 128], identity=identb[:]), reads=[hrb, B_const], writes=[PB[g % 2]])
                    op("act", lambda e: e.activation(out=hT[:, :, g * 128:(g + 1) * 128], in_=pt.rearrange("p (k t) -> p k t", t=128), func=AF.Copy),
                       reads=[PB[g % 2]], writes=[hTb])
                st.update(hT=hT, hTb=hTb)

            def gemm1_sb(b):
                st = state[b]
                W, Wb, b1t, b1b, hT, hTb = st["W"], st["Wb"], st["b1t"], st["b1b"], st["hT"], st["hTb"]
                aT, aTb = atr.next()
                Wv = W[:].rearrange("p m q (k f) -> p m (q k) f", f=1024)
                op("dve", lambda e: e.tensor_scalar(b1t[:, 16:24], b1t[:, 8:16], 1.0, None, op0=ALU.add), reads=[b1b], writes=[b1b])
                for fc in range(8):
                    bg, bl = 2 + (fc % 2) * 2, 3 + (fc % 2) * 2
                    for k in range(8):
                        op("pe", lambda e: e.matmul(ps[:, bg, 0:BS], lhsT=Wv[:, 0, k, fc * 128:(fc + 1) * 128], rhs=hT[:, k, :], start=(k == 0), stop=(k == 7)), reads=[Wb, hTb], writes=[PB[bg]])
                    for k in range(8):
                        op("pe", lambda e: e.matmul(ps[:, bl, 0:BS], lhsT=Wv[:, 1, k, fc * 128:(fc + 1) * 128], rhs=hT[:, k, :], start=(k == 0), stop=(k == 7)), reads=[Wb, hTb], writes=[PB[bl]])
                    xg, xgb = xgr.next()
                    op("dve", lambda e: e.tensor_scalar(xg[:, 0, :], ps[:, bg, 0:BS], b1t[:, fc:fc + 1], 7.0, op0=ALU.add, op1=ALU.min), reads=[PB[bg], b1b], writes=[xgb])
                    op("act", lambda e: e.activation(out=xg[:, 1, :], in_=xg[:, 0, :], func=AF.Sigmoid, scale=1.702), reads=[xgb], writes=[xgb])
                    op("dve", lambda e: e.tensor_scalar(xg[:, 2, :], ps[:, bl, 0:BS], b1t[:, 16 + fc:17 + fc], 8.0, op0=ALU.add, op1=ALU.min), reads=[PB[bl], b1b], writes=[xgb])
                    op("dve", lambda e: e.scalar_tensor_tensor(out=xg[:, 3, :], in0=xg[:, 2, :], scalar=-6.0, in1=xg[:, 0, :], op0=ALU.max, op1=ALU.mult), reads=[xgb], writes=[xgb])
                    op("dve", lambda e: e.tensor_tensor(out=aT[:, fc, :], in0=xg[:, 3, :], in1=xg[:, 1, :], op=ALU.mult), reads=[xgb], writes=[aTb])
                st.update(aT=aT, aTb=aTb, Wv=Wv)

            def gemm2_sb(b):
                st = state[b]
                aT, aTb, Wv, Wb = st["aT"], st["aTb"], st["Wv"], st["Wb"]
                obanks = (6, 2, 4, 6)
                for g in range(GB):
                    ob = obanks[g]
                    for hf in range(2):
                        for fc in range(8):
                            op("pe", lambda e: e.matmul(ps[:, ob + hf, :], lhsT=aT[:, fc, g * 128:(g + 1) * 128], rhs=Wv[:, 2, fc, hf * 512:(hf + 1) * 512], start=(fc == 0), stop=(fc == 7)),
                               reads=[aTb, Wb], writes=[PB[ob + hf]])
                    ost, osb = osr.next()
                    op("act", lambda e: e.activation(out=ost[:], in_=ps[:, ob:ob + 2, :].rearrange("p a n -> p (a n)"), func=AF.Copy), reads=[PB[ob], PB[ob + 1]], writes=[osb])
                    dma("sp", lambda e: e.dma_start(out=obuf_d[b * BS + g * 128:b * BS + (g + 1) * 128, :], in_=ost[:]), reads=[osb])
                del state[b]

            load_sb(0)
            transp_sb(0)
            for b in range(NSB):
                if b + 1 < NSB:
                    load_sb(b + 1)
                gemm1_sb(b)
                if b + 1 < NSB:
                    transp_sb(b + 1)
                gemm2_sb(b)
            fw.barrier()
        last = (l == DEPTH - 1)
        with ExitStack() as cb:
            b2t = sb("b2t", [NE, D], scope=cb)
            Bc = Buf("cmbconst")
            dma("sp", lambda e: e.dma_start(out=b2t[:], in_=b2_d[l]), writes=[Bc])
            G2 = {}
            for col in list(range(NSEQ)) + ([2] if has_ctx_out else []):
                G2[col] = sb(f"G2bc{col}", [128, D], scope=cb)
                dma("sp", lambda e: e.dma_start(out=G2[col][:], in_=modrow_d[col:col + 1, 3 * D:4 * D].partition_broadcast(128)), writes=[Bc])
            if last:
                fnw = sb("fnw", [128, D], scope=cb)
                dma("sp", lambda e: e.dma_start(out=fnw[:], in_=fnwbc_d), writes=[Bc])
            x1r = Ring(nc, cb, "x1c", 2, [128, D], F32)
            rwr = Ring(nc, cb, "orow", 2, [128, 4, D], F32)
            GTr = Ring(nc, cb, "GT", 2, [NE, 128], F32)
            accr = Ring(nc, cb, "acc", 2, [128, D], F32)
            junk = sb("junkC", [128, D], BF16, scope=cb)
            ssq = sb("ssqC", [128, 4], scope=cb)
            Bj = Buf("junkC")
            for (s, c) in lchunks:
                ci = s * NCS + c
                is_ctx = c < NCC
                col = 2 if is_ctx else s
                x1, x1b = x1r.next()
                dma("sp", lambda e: e.dma_start(out=x1[:], in_=xmid_d[ci * 128:(ci + 1) * 128, :]), writes=[x1b])
                orow, orb = rwr.next()
                for k4 in range(4):
                    dma("pool", lambda e: e.indirect_dma_start(out=orow[:, k4, :], out_offset=None, in_=obuf_d[:, :], in_offset=IOA(ap=destk[:, ci, k4:k4 + 1], axis=0)),
                        reads=[Brt_], writes=[orb])
                op("pe", lambda e: e.transpose(out=ps[0:NE, 0, 0:128], in_=Gd[:, ci, :], identity=identf[:]), reads=[Brt_, B_const], writes=[PB[0]])
                GT, GTb = GTr.next()
                op("act", lambda e: e.activation(out=GT[:], in_=ps[0:NE, 0, 0:128], func=AF.Copy), reads=[PB[0]], writes=[GTb])
                for hf in range(2):
                    op("pe", lambda e: e.matmul(ps[:, 1 + hf, :], lhsT=GT[:], rhs=b2t[:, hf * 512:(hf + 1) * 512], start=True, stop=True), reads=[GTb, Bc], writes=[PB[1 + hf]])
                acc, accb = accr.next()
                op("dve", lambda e: e.scalar_tensor_tensor(out=acc[:], in0=orow[:, 0, :], scalar=gatek[:, ci, 0:1], in1=ps[:, 1:3, :].rearrange("p a n -> p (a n)"), op0=ALU.mult, op1=ALU.add),
                   reads=[orb, Brt_, PB[1], PB[2]], writes=[accb])
                for k4 in range(1, 4):
                    op("dve", lambda e: e.scalar_tensor_tensor(out=acc[:], in0=orow[:, k4, :], scalar=gatek[:, ci, k4:k4 + 1], in1=acc[:], op0=ALU.mult, op1=ALU.add),
                       reads=[orb, Brt_, accb], writes=[accb])
                op("dve", lambda e: e.tensor_tensor(out=acc[:], in0=acc[:], in1=G2[col][:], op=ALU.mult), reads=[accb, Bc], writes=[accb])
                op("dve", lambda e: e.tensor_tensor(out=acc[:], in0=acc[:], in1=x1[:], op=ALU.add), reads=[accb, x1b], writes=[accb])
                if DEBUG:
                    dma("sp", lambda e: e.dma_start(out=dbg_x2[ci * 128:(ci + 1) * 128, :], in_=acc[:]), reads=[accb])
                if last:
                    op("dve", lambda e: e.scalar_tensor_tensor(out=junk[:], in0=acc[:], scalar=1.0, in1=acc[:], op0=ALU.mult, op1=ALU.mult, accum_out=ssq[:, 0:1]), reads=[accb], writes=[Bj])
                    op("act", lambda e: e.activation(out=ssq[:, 1:2], in_=ssq[:, 0:1], func=AF.Ln, scale=1.0 / D, bias=epst[:, 0:1]), reads=[Bj], writes=[Bj])
                    op("act", lambda e: e.activation(out=ssq[:, 2:3], in_=ssq[:, 1:2], func=AF.Exp, scale=-0.5), reads=[Bj], writes=[Bj])
                    op("dve", lambda e: e.scalar_tensor_tensor(out=acc[:], in0=acc[:], scalar=ssq[:, 2:3], in1=fnw[:], op0=ALU.mult, op1=ALU.mult), reads=[accb, Bj, Bc], writes=[accb])
                    dma("sp", lambda e: e.dma_start(out=out_d[s, (c - NCC) * 128:(c - NCC + 1) * 128, :], in_=acc[:]), reads=[accb])
                else:
                    dma("sp", lambda e: e.dma_start(out=xnext_d[ci * 128:(ci + 1) * 128, :], in_=acc[:]), reads=[accb])
            fw.barrier()
        lt.close()
        if STOP_AFTER is not None:
            break

    fw.barrier(engines=("sp",))
    es.close()
    return nc


def make_constants(nseq=2):
    bf = ml_dtypes.bfloat16
    k = np.arange(T, dtype=np.float64)
    ang = 2 * np.pi * np.outer(k, k) / T
    C = (np.cos(ang) / 64.0)
    S = (-np.sin(ang) / 64.0)
    def lay(M):
        M = M.reshape(4, 8, 128, 16, 256)
        return M.transpose(3, 0, 2, 1, 4)
    dft = np.stack([lay(C), lay(S)], axis=3)
    dft = np.ascontiguousarray(dft.reshape(16, 4, 128, 2 * 8 * 256)).astype(bf)
    kc = np.arange(TC, dtype=np.float64)
    angc = 2 * np.pi * np.outer(kc, kc) / TC
    Cc = (np.cos(angc) / 16.0).reshape(2, 128, 256).transpose(1, 0, 2)
    Sc = (-np.sin(angc) / 16.0).reshape(2, 128, 256).transpose(1, 0, 2)
    dftc = np.ascontiguousarray(np.stack([Cc, Sc], axis=1).reshape(128, 2 * 2 * 256)).astype(bf)
    m = np.arange(64, dtype=np.float64)
    a64 = 2 * np.pi * np.outer(m, m) / 64
    bd = np.zeros((128, 2, 128), np.float32)
    for g in range(2):
        bd[g * 64:(g + 1) * 64, 0, g * 64:(g + 1) * 64] = np.cos(a64) / 8.0
        bd[g * 64:(g + 1) * 64, 1, g * 64:(g + 1) * 64] = np.sin(a64) / 8.0
    t = np.arange(T)
    row = (t // 64).astype(np.float32)
    colp = (t % 64).astype(np.float32)
    freqs = (np.float32(10000.0) ** (-np.arange(16, dtype=np.float32) / np.float32(16))).astype(np.float32)
    angr = np.concatenate([row[:, None] * freqs, colp[:, None] * freqs], -1).astype(np.float32)
    cs = np.cos(angr).reshape(NCX, 128, 32).transpose(1, 0, 2)
    sn = np.sin(angr).reshape(NCX, 128, 32).transpose(1, 0, 2)
    rope = np.ascontiguousarray(np.stack([cs, sn], axis=1)).astype(np.float32)
    j = np.arange(128)[:, None]
    i = np.arange(128)[None, :]
    tri = np.stack([np.maximum(i - j, 0), np.maximum(j - i, 0), (i >= j) * 0.125, (j >= i) * 0.125], axis=1).astype(np.float32)
    p = np.arange(128, dtype=np.float32)
    pcol = np.stack([127 - p, p + 1, p, 128 - p], axis=1).astype(np.float32)
    capmax = 2048 * nseq + 512
    rowc = np.zeros((128, 4, 256), np.float32)
    rowc[:, 0, :32] = np.arange(32) * capmax
    rowc[:, 1, :32] = np.arange(32)
    rowc[:, 2, :] = np.arange(256) * 512
    rowc[:, 3, :] = np.arange(256)
    ustrict = (np.arange(128)[:, None] < np.arange(128)[None, :]).astype(np.float32).astype(bf)
    return dict(rowc=rowc, ustrict=ustrict, dft=dft, dftc=dftc, bd64=bd, rope=rope, tri=np.ascontiguousarray(tri), pcol=pcol,
                identb=np.eye(128, dtype=np.float32).astype(bf), identf=np.eye(128, dtype=np.float32))


def prep_shared(inp, nseq=2):
    f = lambda a: np.ascontiguousarray(np.asarray(a, dtype=np.float32))
    sh = {}
    sh["mod_w"] = f(inp["mod_w"])
    mod_b = f(inp["mod_b"])
    sh["mod_b"] = mod_b
    sh["mod_bT"] = np.ascontiguousarray(mod_b.reshape(DEPTH, 48, 128).transpose(2, 0, 1))
    nw = f(inp["norm_w"])
    sh["nwT"] = np.ascontiguousarray(nw.reshape(DEPTH, 2, 8, 128).transpose(3, 0, 1, 2))
    sh["nw1_bc"] = np.ascontiguousarray(np.broadcast_to(nw[:, 1, :][None], (128, DEPTH, D)))
    w_in = f(inp["w_in"])
    sh["wfT"] = np.ascontiguousarray(w_in[:, :, :512].transpose(0, 2, 1))
    sh["w_qkvg"] = np.ascontiguousarray(w_in[:, :, 512:])
    sh["w_out"] = f(inp["w_out"])
    sh["rd_bc"] = np.ascontiguousarray(np.broadcast_to(f(inp["ret_decay"]).reshape(1, DEPTH, 16), (128, DEPTH, 16)))
    sh["rnw_bc"] = np.ascontiguousarray(np.broadcast_to(f(inp["ret_norm_w"])[None], (128, DEPTH, 512)))
    sh["router_w"] = f(inp["router_w"])
    sh["rb_bc"] = np.ascontiguousarray(np.broadcast_to(f(inp["router_b"])[None], (128, DEPTH, NE)))
    sh["fnw_bc"] = np.ascontiguousarray(np.broadcast_to(f(inp["final_norm_w"])[None], (128, D)))
    w1 = np.asarray(inp["expert_w1"], dtype=np.float32)
    def wlay(w):
        L, E = w.shape[0], w.shape[1]
        return np.ascontiguousarray(w.reshape(L, E, 8, 128, 1024).transpose(0, 1, 3, 2, 4)).reshape(L, E * 128 * 4, 2048)
    sh["w1g"] = wlay(w1[..., 0::2])
    sh["w1l"] = wlay(w1[..., 1::2])
    sh["w2h"] = wlay(np.asarray(inp["expert_w2"], dtype=np.float32))
    b1 = np.asarray(inp["expert_b1"], dtype=np.float32)
    b1g = b1[..., 0::2].reshape(DEPTH, NE, 8, 128).transpose(0, 1, 3, 2)
    b1l = b1[..., 1::2].reshape(DEPTH, NE, 8, 128).transpose(0, 1, 3, 2)
    sh["b1T"] = np.ascontiguousarray(np.concatenate([b1g, b1l], axis=-1)).reshape(DEPTH, NE * 128, 16)
    sh["b2"] = f(inp["expert_b2"])
    sh.update(make_constants(nseq))
    return sh


def prep_core(inp, sh, core, nseq=2):
    f = lambda a: np.ascontiguousarray(np.asarray(a, dtype=np.float32))
    b0 = core * 2
    m = dict(sh)
    m["x"] = f(inp["x"][b0:b0 + nseq])
    m["ctx"] = f(inp["ctx"][b0:b0 + nseq])
    c = np.asarray(inp["c"], dtype=np.float32)
    cols = [c[b0], c[b0 + 1], np.asarray(inp["c_ctx"], dtype=np.float32)]
    m["cT"] = np.ascontiguousarray(np.stack(cols, axis=1).reshape(8, 128, 3).transpose(1, 0, 2))
    return m


_NC_CACHE = {}


def kernel(**inputs):
    cfg = dict(nseq=2, layers=DEPTH)
    key = "full"
    if key not in _NC_CACHE:
        _NC_CACHE[key] = build_program(cfg)
    nc = _NC_CACHE[key]
    sh = prep_shared(inputs)
    in_maps = [prep_core(inputs, sh, core) for core in range(8)]
    res = run_bass_kernel_spmd(nc, in_maps, core_ids=list(range(8)))
    out = np.concatenate([np.asarray(r["out"], dtype=np.float32) for r in res.results], axis=0)
    return out
```
